# Optimizing a Trainium2 kernel written in Bass

```python
import jax
import jax.numpy as jnp
from jax import lax
import numpy as np

D_MODEL = 1024
BATCH = 8
SEQ = 4096
DEPTH = 1

CHUNK = 64
D_MLSTM = D_MODEL // 2
D_POOL = D_MODEL - D_MLSTM
N_HEADS = 4
HEAD_DIM = D_MLSTM // N_HEADS
CONV_WIDTH = 4
POOL_WINDOWS = (2, 4, 8, 16)
N_POOL_GROUPS = len(POOL_WINDOWS)
POOL_GROUP_DIM = D_POOL // N_POOL_GROUPS
N_EXPERT_GROUPS = 4
EXPERTS_PER_GROUP = 8
N_EXPERTS = N_EXPERT_GROUPS * EXPERTS_PER_GROUP
TOP_K = 2
D_EXPERT = D_MODEL // 2
MOE_BLOCK = 128
EPS = 1e-6

COL_U = 0
COL_V = COL_U + D_MLSTM
COL_O = COL_V + D_MLSTM
COL_I = COL_O + D_MLSTM
COL_F = COL_I + N_HEADS
COL_P = COL_F + N_HEADS
IN_COLS = COL_P + D_POOL

kernel_name = "hybrid_mlstm_pool_hmoe_block"


def rms_norm(x, g):
    xf = x.astype(jnp.float32)
    y = xf * lax.rsqrt(jnp.mean(xf * xf, axis=-1, keepdims=True) + EPS)
    return (y * g.astype(jnp.float32)).astype(x.dtype)


def causal_conv(x, w, b):
    k_w, ch = w.shape
    y = lax.conv_general_dilated(x, w[:, None, :], window_strides=(1,), padding=[(k_w - 1, 0)],
                                 dimension_numbers=('NWC', 'WIO', 'NWC'), feature_group_count=ch)
    return y + b


def mlstm_chunkwise(q, k, v, i_pre, f_pre):
    bsz, nh, seq, dh = q.shape
    nc = seq // CHUNK
    q = q.astype(jnp.float32) * (dh ** -0.5)
    k = k.astype(jnp.float32)
    v = v.astype(jnp.float32)
    log_i = i_pre.astype(jnp.float32)
    log_f = jax.nn.log_sigmoid(f_pre.astype(jnp.float32))

    def to_chunks(a):
        return jnp.moveaxis(a.reshape(bsz, nh, nc, CHUNK, *a.shape[3:]), 2, 0)

    qc, kc, vc, ic = to_chunks(q), to_chunks(k), to_chunks(v), to_chunks(log_i)
    bc = jnp.cumsum(to_chunks(log_f), axis=-1)
    causal = jnp.tril(jnp.ones((CHUNK, CHUNK), dtype=bool))

    def step(carry, inp):
        c_st, n_st, m_st = carry
        q_, k_, v_, ig, b_ = inp
        d_log = b_[..., :, None] - b_[..., None, :] + ig[..., None, :]
        d_log = jnp.where(causal, d_log, -jnp.inf)
        inter = b_ + m_st[..., None]
        m_t = jnp.maximum(inter, jnp.max(d_log, axis=-1))
        w_intra = jnp.exp(d_log - m_t[..., None])
        a_inter = jnp.exp(inter - m_t)
        s = jnp.einsum('bhtd,bhsd->bhts', q_, k_) * w_intra
        num = (a_inter[..., None] * jnp.einsum('bhed,bhtd->bhte', c_st, q_)
               + jnp.einsum('bhts,bhse->bhte', s, v_))
        den = a_inter * jnp.einsum('bhd,bhtd->bht', n_st, q_) + jnp.sum(s, axis=-1)
        h = num / jnp.maximum(jnp.abs(den), jnp.exp(-m_t))[..., None]
        b_last = b_[..., -1]
        w_log = b_last[..., None] - b_ + ig
        m_new = jnp.maximum(b_last + m_st, jnp.max(w_log, axis=-1))
        a_prev = jnp.exp(b_last + m_st - m_new)
        w_s = jnp.exp(w_log - m_new[..., None])
        c_new = a_prev[..., None, None] * c_st + jnp.einsum('bhs,bhse,bhsd->bhed', w_s, v_, k_)
        n_new = a_prev[..., None] * n_st + jnp.einsum('bhs,bhsd->bhd', w_s, k_)
        return (c_new, n_new, m_new), h

    init = (jnp.zeros((bsz, nh, dh, dh), jnp.float32), jnp.zeros((bsz, nh, dh), jnp.float32),
            jnp.zeros((bsz, nh), jnp.float32))
    _, hs = lax.scan(step, init, (qc, kc, vc, ic, bc))
    return jnp.moveaxis(hs, 0, 2).reshape(bsz, nh, seq, dh)


def mlstm_group(u, v, o_pre, i_pre, f_pre, conv_w, conv_b, w_q, w_k, b_i, b_f, norm_g, skip):
    bsz, seq, _ = u.shape
    uc = jax.nn.silu(causal_conv(u, conv_w, conv_b))
    uh = uc.reshape(bsz, seq, N_HEADS, HEAD_DIM)
    q = jnp.einsum('bshd,hde->bhse', uh, w_q)
    k = jnp.einsum('bshd,hde->bhse', uh, w_k)
    vh = v.reshape(bsz, seq, N_HEADS, HEAD_DIM).transpose(0, 2, 1, 3)
    ig = (i_pre + b_i).transpose(0, 2, 1)
    fg = (f_pre + b_f).transpose(0, 2, 1)
    h = mlstm_chunkwise(q, k, vh, ig, fg)
    h = h * lax.rsqrt(jnp.mean(h * h, axis=-1, keepdims=True) + EPS)
    h = h * norm_g.astype(jnp.float32).reshape(N_HEADS, 1, HEAD_DIM)
    h = h.transpose(0, 2, 1, 3).reshape(bsz, seq, D_MLSTM)
    out = jax.nn.sigmoid(o_pre.astype(jnp.float32)) * (h + skip.astype(jnp.float32) * uc.astype(jnp.float32))
    return out.astype(u.dtype)


def pool_group(p, w_pool, b_pool, pool_scale):
    bsz, seq, _ = p.shape
    pf = p.astype(jnp.float32).reshape(bsz, seq, N_POOL_GROUPS, POOL_GROUP_DIM)
    cs = jnp.pad(jnp.cumsum(pf, axis=1), ((0, 0), (1, 0), (0, 0), (0, 0)))
    t = jnp.arange(seq, dtype=jnp.int32)
    win = jnp.array(POOL_WINDOWS, dtype=jnp.int32)
    start = jnp.maximum(t[:, None] + 1 - win[None, :], 0)
    lo = cs[:, start, jnp.arange(N_POOL_GROUPS)[None, :]]
    count = jnp.minimum(t[:, None] + 1, win[None, :]).astype(jnp.float32)
    pooled = (cs[:, 1:] - lo) / count[None, :, :, None] - pf
    y = jnp.einsum('bsgc,gcd->bsgd', pooled, w_pool.astype(jnp.float32)).reshape(bsz, seq, D_POOL)
    y = (y + b_pool.astype(jnp.float32)) * pool_scale.astype(jnp.float32)
    return y.astype(p.dtype)


def hierarchical_moe(h, w_rg, b_rg, w_re, b_re, w_gate, w_up, w_down):
    bsz, seq, dm = h.shape
    n_tok = bsz * seq
    hf = h.reshape(n_tok, dm)
    g_logits = (hf @ w_rg + b_rg).astype(jnp.float32)
    g_prob = jax.nn.softmax(g_logits, axis=-1)
    g_idx = jnp.argmax(g_logits, axis=-1).astype(jnp.int32)
    g_gate = jnp.take_along_axis(g_prob, g_idx[:, None], axis=1)[:, 0]
    e_logits = (hf @ w_re + b_re).astype(jnp.float32).reshape(n_tok, N_EXPERT_GROUPS, EXPERTS_PER_GROUP)
    e_logits = jnp.take_along_axis(e_logits, g_idx[:, None, None], axis=1)[:, 0]
    top_val, top_loc = lax.top_k(e_logits, TOP_K)
    gates = jax.nn.softmax(top_val, axis=-1) * g_gate[:, None]
    expert = g_idx[:, None] * EXPERTS_PER_GROUP + top_loc.astype(jnp.int32)
    n_asg = n_tok * TOP_K
    flat_e = expert.reshape(n_asg)
    flat_tok = jnp.repeat(jnp.arange(n_tok, dtype=jnp.int32), TOP_K)
    flat_gate = gates.reshape(n_asg)
    order = jnp.argsort(flat_e)
    sorted_e = flat_e[order]
    counts = jnp.bincount(flat_e, length=N_EXPERTS).astype(jnp.int32)
    starts = jnp.cumsum(counts) - counts
    padded = (counts + MOE_BLOCK - 1) // MOE_BLOCK * MOE_BLOCK
    pad_ends = jnp.cumsum(padded)
    pad_starts = pad_ends - padded
    rank = jnp.arange(n_asg, dtype=jnp.int32) - starts[sorted_e]
    dest = pad_starts[sorted_e] + rank
    n_blocks = (n_asg + MOE_BLOCK - 1) // MOE_BLOCK + N_EXPERTS
    n_slots = n_blocks * MOE_BLOCK
    slot_tok = jnp.full((n_slots,), n_tok, jnp.int32).at[dest].set(flat_tok[order])
    slot_gate = jnp.zeros((n_slots,), jnp.float32).at[dest].set(flat_gate[order])
    block_pos = jnp.arange(n_blocks, dtype=jnp.int32) * MOE_BLOCK
    block_expert = jnp.minimum(jnp.searchsorted(pad_ends, block_pos, side='right'), N_EXPERTS - 1)

    def run_block(args):
        tok, e = args
        xb = hf[jnp.minimum(tok, n_tok - 1)]
        a = xb @ w_gate[e]
        b = xb @ w_up[e]
        return (jax.nn.silu(a) * b) @ w_down[e]

    out = lax.map(run_block, (slot_tok.reshape(n_blocks, MOE_BLOCK), block_expert))
    out = (out.reshape(n_slots, dm).astype(jnp.float32) * slot_gate[:, None]).astype(h.dtype)
    y = jnp.zeros((n_tok, dm), h.dtype).at[slot_tok].add(out, mode='drop')
    return y.reshape(bsz, seq, dm)


def setup_inputs(seed: int = 0) -> dict:
    key = jax.random.key(seed)
    ks = jax.random.split(key, 28)
    L = DEPTH

    def nrm(k, shape, scale):
        return jax.random.normal(k, shape, jnp.float32) * scale

    return {
        'x': nrm(ks[0], (BATCH, SEQ, D_MODEL), 1.0),
        'c': nrm(ks[1], (BATCH, D_MODEL), 1.0),
        'ada_w': nrm(ks[2], (L, D_MODEL, 6 * D_MODEL), D_MODEL ** -0.5),
        'ada_b': nrm(ks[3], (L, 6 * D_MODEL), 0.02),
        'norm1_g': 1.0 + nrm(ks[4], (L, D_MODEL), 0.02),
        'w_in': nrm(ks[5], (L, D_MODEL, IN_COLS), D_MODEL ** -0.5),
        'conv_w': nrm(ks[6], (L, CONV_WIDTH, D_MLSTM), CONV_WIDTH ** -0.5),
        'conv_b': nrm(ks[7], (L, D_MLSTM), 0.02),
        'w_q': nrm(ks[8], (L, N_HEADS, HEAD_DIM, HEAD_DIM), HEAD_DIM ** -0.5),
        'w_k': nrm(ks[9], (L, N_HEADS, HEAD_DIM, HEAD_DIM), HEAD_DIM ** -0.5),
        'b_igate': nrm(ks[10], (L, N_HEADS), 0.1),
        'b_fgate': jnp.linspace(3.0, 6.0, N_HEADS, dtype=jnp.float32)[None, :] + nrm(ks[11], (L, N_HEADS), 0.1),
        'mlstm_norm_g': 1.0 + nrm(ks[12], (L, D_MLSTM), 0.02),
        'mlstm_skip': 1.0 + nrm(ks[13], (L, D_MLSTM), 0.1),
        'w_pool': nrm(ks[14], (L, N_POOL_GROUPS, POOL_GROUP_DIM, POOL_GROUP_DIM), POOL_GROUP_DIM ** -0.5),
        'b_pool': nrm(ks[15], (L, D_POOL), 0.02),
        'pool_scale': 1.0 + nrm(ks[16], (L, D_POOL), 0.1),
        'w_out': nrm(ks[17], (L, D_MODEL, D_MODEL), D_MODEL ** -0.5),
        'norm2_g': 1.0 + nrm(ks[18], (L, D_MODEL), 0.02),
        'w_router_group': nrm(ks[19], (L, D_MODEL, N_EXPERT_GROUPS), D_MODEL ** -0.5),
        'b_router_group': nrm(ks[20], (L, N_EXPERT_GROUPS), 0.01),
        'w_router_expert': nrm(ks[21], (L, D_MODEL, N_EXPERTS), D_MODEL ** -0.5),
        'b_router_expert': nrm(ks[22], (L, N_EXPERTS), 0.01),
        'w_expert_gate': nrm(ks[23], (L, N_EXPERTS, D_MODEL, D_EXPERT), D_MODEL ** -0.5),
        'w_expert_up': nrm(ks[24], (L, N_EXPERTS, D_MODEL, D_EXPERT), D_MODEL ** -0.5),
        'w_expert_down': nrm(ks[25], (L, N_EXPERTS, D_EXPERT, D_MODEL), D_EXPERT ** -0.5),
        'final_g': 1.0 + nrm(ks[26], (D_MODEL,), 0.02),
    }


def reference(x, c, ada_w, ada_b, norm1_g, w_in, conv_w, conv_b, w_q, w_k, b_igate, b_fgate,
              mlstm_norm_g, mlstm_skip, w_pool, b_pool, pool_scale, w_out, norm2_g,
              w_router_group, b_router_group, w_router_expert, b_router_expert,
              w_expert_gate, w_expert_up, w_expert_down, final_g):
    bsz, seq, dm = x.shape
    for l in range(DEPTH):
        mod = (jax.nn.silu(c) @ ada_w[l] + ada_b[l]).reshape(bsz, 6, dm)
        shift_a, scale_a, gate_a = mod[:, 0, None, :], mod[:, 1, None, :], mod[:, 2, None, :]
        shift_f, scale_f, gate_f = mod[:, 3, None, :], mod[:, 4, None, :], mod[:, 5, None, :]
        h = rms_norm(x, norm1_g[l]) * (1.0 + scale_a) + shift_a
        proj = h @ w_in[l]
        y_m = mlstm_group(proj[..., COL_U:COL_V], proj[..., COL_V:COL_O], proj[..., COL_O:COL_I],
                          proj[..., COL_I:COL_F], proj[..., COL_F:COL_P], conv_w[l], conv_b[l],
                          w_q[l], w_k[l], b_igate[l], b_fgate[l], mlstm_norm_g[l], mlstm_skip[l])
        y_p = pool_group(proj[..., COL_P:IN_COLS], w_pool[l], b_pool[l], pool_scale[l])
        mix = jnp.concatenate([y_m, y_p], axis=-1) @ w_out[l]
        x = x + gate_a * mix
        h = rms_norm(x, norm2_g[l]) * (1.0 + scale_f) + shift_f
        x = x + gate_f * hierarchical_moe(h, w_router_group[l], b_router_group[l], w_router_expert[l],
                                          b_router_expert[l], w_expert_gate[l], w_expert_up[l],
                                          w_expert_down[l])
    return rms_norm(x, final_g)
```

```python
import numpy as np
from contextlib import ExitStack
import concourse.bass as bass
import concourse.mybir as mybir
from concourse.bass_utils import run_bass_kernel_spmd

F32 = mybir.dt.float32
BF16 = mybir.dt.bfloat16
I32 = mybir.dt.int32
AF = mybir.ActivationFunctionType
ALU = mybir.AluOpType
AX = mybir.AxisListType

T = 4096
D = 1024
NT = T // 128
SEG = 512
NSEG = T // SEG
TPS = SEG // 128
INC = 2056
COL_U, COL_V, COL_O, COL_I, COL_F, COL_P = 0, 512, 1024, 1536, 1540, 1544
NE = 32
B = 384
BT = B // 128
NB = (2 * T) // B + NE
NSLOT = NB * B
JMAX = (T + B - 1) // B + 1
EPS = 1e-6
BIG = 30000.0

ENGS = ("pe", "act", "dve", "pool", "sp")
DMAQ = ("sp", "act", "pool")
NDSEM = 8


class Op:
    __slots__ = ("eng", "fn", "reads", "writes", "dma", "deps", "sig", "sem", "val", "name")

    def __init__(self, eng, fn, reads, writes, dma, name):
        self.eng, self.fn, self.reads, self.writes, self.dma, self.name = eng, fn, reads, writes, dma, name
        self.deps = []
        self.sig = False
        self.sem = None
        self.val = 0


class Prog:
    def __init__(self, nc):
        self.nc = nc
        self.ops = []
        self.last_w = {}
        self.readers = {}

    def op(self, eng, fn, reads=(), writes=(), dma=False, name=""):
        o = Op(eng, fn, tuple(reads), tuple(writes), dma, name)
        deps = set()
        for r in o.reads:
            w = self.last_w.get(r)
            if w is not None:
                deps.add(w)
        for w_ in o.writes:
            w = self.last_w.get(w_)
            if w is not None:
                deps.add(w)
            for rd in self.readers.get(w_, ()):
                deps.add(rd)
        for d in deps:
            if d is o:
                continue
            if d.eng == o.eng and not d.dma and not o.dma:
                if o.eng == "pe":
                    continue
                if not any(self.last_w.get(r) is d for r in o.reads):
                    continue
            o.deps.append(d)
            d.sig = True
        for w_ in o.writes:
            self.last_w[w_] = o
            self.readers[w_] = []
        for r in o.reads:
            if r not in o.writes:
                self.readers.setdefault(r, []).append(o)
        self.ops.append(o)
        return o

    def pe(self, fn, reads=(), writes=(), name=""):
        return self.op("pe", fn, reads, writes, name=name)

    def act(self, fn, reads=(), writes=(), name=""):
        return self.op("act", fn, reads, writes, name=name)

    def dve(self, fn, reads=(), writes=(), name=""):
        return self.op("dve", fn, reads, writes, name=name)

    def pool(self, fn, reads=(), writes=(), name=""):
        return self.op("pool", fn, reads, writes, name=name)

    def ve(self, eng, fn, reads=(), writes=(), name=""):
        return self.op(eng, fn, reads, writes, name=name)

    def dma(self, q, fn, reads=(), writes=(), name=""):
        return self.op(q, fn, reads, writes, dma=True, name=name)

    def _sync_all(self, engines):
        last = {}
        dmas = {q: [] for q in DMAQ}
        for o in self.ops:
            if o.fn is None:
                continue
            if o.dma:
                dmas[o.eng].append(o)
            else:
                last[o.eng] = o
        deps = list(last.values())
        for q in DMAQ:
            deps += dmas[q][-NDSEM:]
        for e in engines:
            o = Op(e, None, (), (), False, "sync_all")
            for d in deps:
                if d.eng == e and not d.dma:
                    continue
                o.deps.append(d)
                d.sig = True
            self.ops.append(o)

    def barrier(self):
        self._sync_all(ENGS)

    def finish(self):
        self._sync_all(("sp",))

    def emit(self, es):
        nc = self.nc
        engsem = {e: es.enter_context(nc.semaphore("s_" + e)) for e in ENGS}
        dsem = {e: [es.enter_context(nc.semaphore(f"d_{e}{i}")) for i in range(NDSEM)] for e in DMAQ}
        cnt = {e: 0 for e in ENGS}
        hist = {e: [] for e in DMAQ}
        per_eng = {e: [] for e in ENGS}
        for o in self.ops:
            if o.dma:
                h = hist[o.eng]
                i = len(h)
                o.sem = dsem[o.eng][i % NDSEM]
                o.val = 16 * (i // NDSEM + 1)
                o.sig = True
                if i >= NDSEM and h[i - NDSEM] not in o.deps:
                    o.deps.append(h[i - NDSEM])
                h.append(o)
            elif o.sig and o.fn is not None:
                cnt[o.eng] += 1
                o.sem = engsem[o.eng]
                o.val = cnt[o.eng]
            per_eng[o.eng].append(o)
        block = es.enter_context(nc.Block())
        handles = {"pe": "tensor", "act": "scalar", "dve": "vector", "pool": "gpsimd", "sp": "sync"}

        def make(e):
            def body(eng):
                seen = {}
                for o in per_eng[e]:
                    for d in o.deps:
                        k = d.sem.name
                        if seen.get(k, 0) >= d.val:
                            continue
                        seen[k] = d.val
                        eng.wait_ge(d.sem, d.val)
                    if o.fn is None:
                        continue
                    ins = o.fn(eng)
                    if o.sig:
                        ins.then_inc(o.sem, 16 if o.dma else 1)
            return body

        for e in ENGS:
            getattr(block, handles[e])(make(e))
        self.stats = {e: len(per_eng[e]) for e in ENGS}


class Arena:
    def __init__(self, ap_bf16, nbytes):
        self.ap = ap_bf16
        self.nbytes = nbytes
        self.off = 0

    def reset(self):
        self.off = 0

    def alloc(self, shape, dt):
        esz = {F32: 4, BF16: 2, I32: 4}[dt]
        n = int(np.prod(shape[1:]))
        nb = (n * esz + 31) // 32 * 32
        assert self.off + nb <= self.nbytes, f"arena overflow {self.off + nb} > {self.nbytes}"
        v = self.ap[:, self.off // 2:(self.off + n * esz) // 2]
        self.off += nb
        if dt != BF16:
            v = v.bitcast(dt)
        if len(shape) == 3:
            v = v.rearrange("p (a b) -> p a b", b=shape[2])
        elif len(shape) == 4:
            v = v.rearrange("p (a b c) -> p a b c", b=shape[2], c=shape[3])
        return v


class Rot:
    def __init__(self, name, aps):
        self.name, self.aps, self.i = name, aps, 0

    def next(self):
        k = self.i % len(self.aps)
        self.i += 1
        return self.aps[k], (self.name, k)


def build(debug=False):
    nc = bass.Bass("TRN2", target_bir_lowering=False)
    dbg_out = {}

    def DT(name, shape, dt, kind="ExternalInput"):
        return nc.dram_tensor(name, list(shape), dt, kind=kind).ap()

    x_d = DT("x", [T, D], F32)
    ccol_d = DT("ccol", [128, 8], F32)
    adaw_d = DT("ada_w", [D, 6 * D], F32)
    adab_d = DT("ada_b", [1, 6 * D], F32)
    win_d = DT("w_in", [D, INC], F32)
    wq_d = DT("w_q", [4, 128, 128], F32)
    wk_d = DT("w_k", [4, 128, 128], F32)
    wpool_d = DT("w_pool", [4, 128, 128], F32)
    wout_d = DT("w_out", [D, D], F32)
    wr_d = DT("w_r", [D, 36], F32)
    wgate_d = DT("w_gate", [NE * 128, 8 * 512], F32)
    wup_d = DT("w_up", [NE * 128, 8 * 512], F32)
    wdown_d = DT("w_down", [NE * 128, 4 * D], F32)
    colp_d = DT("colp", [128, 40], F32)
    NROW = 512 + 36 + 8 + 64 + 32 + JMAX + NB
    rowp_d = DT("rowp", [1, NROW], F32)
    g2_d = DT("norm2_g", [1, D], F32)
    fg_d = DT("final_g", [1, D], F32)
    ident_d = DT("ident", [128, 128], F32)
    tri_d = DT("tri", [128, 128], F32)
    rowoff_d = DT("rowoff", [128, 8], F32)
    out_d = DT("out", [T, D], F32, "ExternalOutput")
    scr = "ExternalOutput"
    X1_d = DT("X1", [T, D], F32, scr)
    H2_d = DT("H2", [T, D], BF16, scr)
    Hs_d = DT("Hs", [NSLOT, D], BF16, "Internal")
    Y_d = DT("Y", [NSLOT, D], F32, "Internal")

    es = ExitStack()
    with es:
        def S(name, shape, dt):
            return es.enter_context(nc.sbuf_tensor("s_" + name, list(shape), dt))

        p = Prog(nc)
        banks = [es.enter_context(nc.psum_tensor(f"bank{i}", [128, 512], F32)) for i in range(8)]
        bank_i = [0]

        def nbank():
            k = bank_i[0] % 7
            bank_i[0] += 1
            return banks[k], ("bank", k)

        ident = S("ident", [128, 128], F32)
        identb = S("identb", [128, 128], BF16)
        tri = S("tri", [128, 128], F32)
        trib = S("trib", [128, 128], BF16)
        striub = S("striub", [128, 128], BF16)
        onesf = S("onesf", [128, 128], F32)
        onesb = S("onesb", [128, 128], BF16)
        colp = S("colp", [128, 40], F32)
        rowb = S("rowb", [128, NROW], F32)
        rowoff = S("rowoff", [128, 8], F32)
        modB = S("modB", [128, 4 * D], F32)
        s2bc = S("s2bc", [128, D], F32)
        wi = S("wi", [128, 8, INC], BF16)
        wo = S("wo", [128, 8, D], BF16)
        wqk = S("wqk", [128, 2, 4, 128], BF16)
        wpl = S("wpl", [128, 4, 128], BF16)
        wr = S("wr", [128, 8, 36], BF16)
        biasbc = S("biasbc", [128, 520], F32)
        biascol = S("biascol", [128, 8], F32)
        bpscol = S("bpscol", [128, 4], F32)
        s1col = S("s1col", [128, 8], F32)
        Cst = S("Cst", [128, 4, 129], F32)
        ohs = S("ohs", [128, NT, 2, 32], BF16)
        ohsum = S("ohsum", [128, NT, 32], BF16)
        gts = S("gts", [128, NT, 2], F32)
        destf = S("destf", [128, NT, 2], F32)
        desti = S("desti", [128, NT, 2], I32)
        widx = S("widx", [128, NB], I32)
        widxc = S("widxc", [128, NB], I32)
        ztile = S("ztile", [128, D], BF16)
        ARENA_BYTES = 119808
        arena_t = S("arena", [128, ARENA_BYTES // 2], BF16)
        arena = Arena(arena_t, ARENA_BYTES)

        g1col = colp[:, 0:8]
        convw = colp[:, 8:24]
        convb = colp[:, 24:28]
        skipc = colp[:, 28:32]
        bpoolc = colp[:, 32:36]
        pscalec = colp[:, 36:40]
        r0 = 0
        normg_bc = rowb[:, r0:r0 + 512]; r0 += 512
        br_bc = rowb[:, r0:r0 + 36]; r0 += 36
        bif_bc = rowb[:, r0:r0 + 8]; r0 += 8
        inv0_bc = rowb[:, r0:r0 + 64]; r0 += 64
        iota_bc = rowb[:, r0:r0 + 32]; r0 += 32
        thr_bc = rowb[:, r0:r0 + JMAX]; r0 += JMAX
        bpos_bc = rowb[:, r0:r0 + NB]; r0 += NB
        gate_a_bc = modB[:, 0:D]
        shift_f_bc = modB[:, D:2 * D]
        scale_f_bc = modB[:, 2 * D:3 * D]
        gate_f_bc = modB[:, 3 * D:4 * D]

        def dump(name, ap, key, dt=F32):
            if not debug:
                return
            shp = list(ap.shape)
            t = DT("dbg_" + name, shp, dt, "ExternalOutput")
            dbg_out["dbg_" + name] = shp
            p.dma("sp", lambda e: e.dma_start(out=t, in_=ap), reads=[key], name="dump")

        p.dma("sp", lambda e: e.dma_start(out=ident[:], in_=ident_d), writes=["ident"])
        p.dma("sp", lambda e: e.dma_start(out=tri[:], in_=tri_d), writes=["tri"])
        p.dma("sp", lambda e: e.dma_start(out=colp[:], in_=colp_d), writes=["colp"])
        p.dma("sp", lambda e: e.dma_start(out=rowb[:], in_=rowp_d.partition_broadcast(128)), writes=["rowb"])
        p.dma("sp", lambda e: e.dma_start(out=rowoff[:], in_=rowoff_d), writes=["rowoff"])
        p.dve(lambda e: e.tensor_copy(out=identb[:], in_=ident[:]), reads=["ident"], writes=["identb"])
        p.dve(lambda e: e.tensor_copy(out=trib[:], in_=tri[:]), reads=["tri"], writes=["trib"])
        p.dve(lambda e: e.tensor_tensor(out=striub[:], in0=tri[:], in1=ident[:], op=ALU.subtract),
              reads=["tri", "ident"], writes=["striub"])
        p.dve(lambda e: e.memset(onesf[:], 1.0), writes=["onesf"])
        p.dve(lambda e: e.memset(onesb[:], 1.0), writes=["onesb"])
        p.dve(lambda e: e.memset(Cst[:], 0.0), writes=[("C", h) for h in range(4)])

        p.pool(lambda e: e.memset(ztile[:], 0.0), writes=["ztile"])
        hs_v = Hs_d.rearrange("(n a p) c -> n p a c", p=128, a=BT)
        nz = NB
        rem = 0
        zero_todo = list(range(nz))

        def pump_zero(n):
            for _ in range(n):
                if not zero_todo:
                    return
                nn = zero_todo.pop(0)
                for a_ in range(BT):
                    p.dma("pool", (lambda nn, a_: lambda e: e.dma_start(out=hs_v[nn][:, a_, :], in_=ztile[:]))(nn, a_),
                          reads=["ztile"], writes=[("Hs", "z", nn, a_)])

        modA = arena.alloc([128, 2 * D], F32)
        shift_a_bc = modA[:, 0:D]
        scale_a_bc = modA[:, D:2 * D]

        def modslice(j):
            return modA[:, j * 512:(j + 1) * 512] if j < 4 else modB[:, (j - 4) * 512:(j - 3) * 512]
        cc = arena.alloc([128, 8], F32)
        scb = arena.alloc([128, 8], BF16)
        screp = arena.alloc([128, 8, 128], BF16)
        p.dma("sp", lambda e: e.dma_start(out=cc, in_=ccol_d), writes=["cc"])
        p.act(lambda e: e.activation(out=scb, in_=cc, func=AF.Silu), reads=["cc"], writes=["scb"])
        p.dve(lambda e: e.tensor_copy(out=screp, in_=scb.unsqueeze(2).to_broadcast([128, 8, 128])),
              reads=["scb"], writes=["screp"])
        wa_r = Rot("wa", [arena.alloc([128, 8, 512], BF16) for _ in range(2)])
        ab_r = Rot("ab", [arena.alloc([128, 512], F32) for _ in range(2)])
        adaw_v = adaw_d.rearrange("(k p) n -> p k n", p=128)
        for j in range(12):
            wa, wak = wa_r.next()
            ab, abk = ab_r.next()
            p.dma("pool", (lambda wa, j: lambda e: e.dma_start(out=wa, in_=adaw_v[:, :, j * 512:(j + 1) * 512]))(wa, j),
                  writes=[wak])
            p.dma("sp", (lambda ab, j: lambda e: e.dma_start(
                out=ab, in_=adab_d[0:1, j * 512:(j + 1) * 512].partition_broadcast(128)))(ab, j), writes=[abk])
            bk, bkk = nbank()

            def mm(e, wa=wa, bk=bk):
                for k in range(8):
                    ins = e.matmul(bk[:], lhsT=screp[:, k, :], rhs=wa[:, k, :], start=(k == 0), stop=(k == 7))
                return ins
            p.pe(mm, reads=["screp", wak], writes=[bkk])
            p.dve((lambda ab, bk, j: lambda e: e.tensor_tensor(out=modslice(j), in0=bk[:], in1=ab,
                                                               op=ALU.add))(ab, bk, j),
                  reads=[bkk, abk], writes=[("mod", j)])
        MODALL = [("mod", j) for j in range(12)]

        win_v = win_d.rearrange("(k p) n -> p k n", p=128)
        for k in range(8):
            p.dma("pool", (lambda k: lambda e: e.dma_start(out=wi[:, k, :], in_=win_v[:, k, :]))(k), writes=[("wi", k)])
        WIALL = [("wi", k) for k in range(8)]
        wout_v = wout_d.rearrange("(k p) n -> p k n", p=128)
        for k in range(0, 8, 2):
            p.dma("pool", (lambda k: lambda e: e.dma_start(out=wo[:, k:k + 2, :], in_=wout_v[:, k:k + 2, :]))(k),
                  writes=[("wo", k)])
        WOALL = [("wo", k) for k in range(0, 8, 2)]
        WOSC = True
        p.dma("pool", lambda e: e.dma_start(out=wqk[:, 0, :, :], in_=wq_d.rearrange("h d e -> d h e")), writes=["wq"])
        p.dma("pool", lambda e: e.dma_start(out=wqk[:, 1, :, :], in_=wk_d.rearrange("h d e -> d h e")), writes=["wk"])
        p.dma("pool", lambda e: e.dma_start(out=wpl[:], in_=wpool_d.rearrange("g c d -> c g d")), writes=["wpl"])
        p.dma("pool", lambda e: e.dma_start(out=wr[:], in_=wr_d.rearrange("(k p) n -> p k n", p=128)), writes=["wr"])

        tmpA = arena.alloc([128, D], F32)
        tmp3 = arena.alloc([128, 8, 128], F32)
        scl = arena.alloc([128, 8], F32)
        shc = arena.alloc([128, 8], F32)
        shcb = arena.alloc([128, 8], BF16)
        shrep = arena.alloc([128, 8, 128], BF16)
        idb3 = ident[:].unsqueeze(1).to_broadcast([128, 8, 128])
        p.dve(lambda e: e.tensor_tensor(out=tmp3, in0=scale_a_bc.rearrange("p (a b) -> p a b", b=128), in1=idb3, op=ALU.mult),
              reads=MODALL + ["ident"], writes=["tmp3"])
        p.dve(lambda e: e.reduce_sum(out=scl, in_=tmp3, axis=AX.X), reads=["tmp3"], writes=["scl"])
        p.dve(lambda e: e.scalar_tensor_tensor(out=s1col[:], in0=scl, scalar=1.0, in1=g1col, op0=ALU.add, op1=ALU.mult),
              reads=["scl", "colp"], writes=["s1col"])
        p.dve(lambda e: e.tensor_tensor(out=tmp3, in0=shift_a_bc.rearrange("p (a b) -> p a b", b=128), in1=idb3, op=ALU.mult),
              reads=MODALL + ["ident", "scl"], writes=["tmp3"])
        p.dve(lambda e: e.reduce_sum(out=shc, in_=tmp3, axis=AX.X), reads=["tmp3"], writes=["shc"])
        p.dve(lambda e: e.tensor_copy(out=shcb, in_=shc), reads=["shc"], writes=["shcb"])
        p.dve(lambda e: e.tensor_copy(out=shrep, in_=shcb.unsqueeze(2).to_broadcast([128, 8, 128])),
              reads=["shcb"], writes=["shrep"])
        bk, bkk = nbank()

        def mmbv(e, bk=bk):
            for k in range(8):
                ins = e.matmul(bk[:], lhsT=shrep[:, k, :], rhs=wi[:, k, COL_V:COL_V + 512], start=(k == 0), stop=(k == 7))
            return ins
        p.pe(mmbv, reads=["shrep"] + WIALL, writes=[bkk])
        p.dve((lambda bk: lambda e: e.tensor_copy(out=biasbc[:, 0:512], in_=bk[:]))(bk), reads=[bkk], writes=["biasbc_v"])
        bk, bkk = nbank()

        def mmbg(e, bk=bk):
            for k in range(8):
                ins = e.matmul(bk[:, 0:8], lhsT=shrep[:, k, :], rhs=wi[:, k, COL_I:COL_I + 8], start=(k == 0), stop=(k == 7))
            return ins
        p.pe(mmbg, reads=["shrep"] + WIALL, writes=[bkk])
        p.dve((lambda bk: lambda e: e.tensor_tensor(out=biasbc[:, 512:520], in0=bk[:, 0:8], in1=bif_bc, op=ALU.add))(bk),
              reads=[bkk, "rowb"], writes=["biasbc_g"])
        bk, bkk = nbank()

        def mmbc(e, bk=bk):
            for c in range(8):
                c0 = (COL_U if c < 4 else COL_O) + (c % 4) * 128
                for k in range(8):
                    ins = e.matmul(bk[:, c:c + 1], lhsT=wi[:, k, c0:c0 + 128], rhs=shcb[:, k:k + 1], start=(k == 0), stop=(k == 7))
            return ins
        p.pe(mmbc, reads=["shcb"] + WIALL, writes=[bkk])
        p.dve((lambda bk: lambda e: e.tensor_copy(out=biascol[:], in_=bk[:, 0:8]))(bk), reads=[bkk], writes=["biascol"])
        for k in range(8):
            p.dve((lambda k: lambda e: e.tensor_scalar(out=wi[:, k, :], in0=wi[:, k, :], scalar1=s1col[:, k:k + 1], scalar2=None,
                                                       op0=ALU.mult))(k),
                  reads=["s1col", ("wi", k)], writes=[("wi", k)])
        p.dma("sp", lambda e: e.dma_start(out=tmpA, in_=g2_d.partition_broadcast(128)), writes=["tmpA"])
        p.dve(lambda e: e.scalar_tensor_tensor(out=s2bc[:], in0=scale_f_bc, scalar=1.0, in1=tmpA, op0=ALU.add, op1=ALU.mult),
              reads=MODALL + ["tmpA"], writes=["s2bc"])
        p.dve(lambda e: e.tensor_tensor(out=bpscol[:], in0=bpoolc, in1=pscalec, op=ALU.mult), reads=["colp"], writes=["bpscol"])
        for k in range(0, 8, 2):
            for kk in (k, k + 1):
                p.dve((lambda kk: lambda e: e.tensor_tensor(out=wo[:, kk, :], in0=wo[:, kk, :], in1=gate_a_bc, op=ALU.mult))(kk),
                      reads=[("wo", k)] + MODALL, writes=[("wo", k)])
        dump("modB", modB[:], ("mod", 11))
        dump("biasbc", biasbc[:], "biasbc_g")
        dump("biascol", biascol[:], "biascol")

        p.barrier()
        arena.reset()
        A = arena.alloc
        xs_r = Rot("xs", [A([128, D], F32) for _ in range(2)])
        xb_r = Rot("xb", [A([128, D], BF16) for _ in range(2)])
        sqj = A([128, D], BF16)
        xT = A([128, 8, SEG], BF16)
        R1 = A([128, SEG], F32)
        ssq1 = A([128, TPS], F32)
        rstd1 = A([128, TPS], F32)
        diag_r = Rot("diag", [A([128, 128], F32) for _ in range(2)])
        ubuf = A([128, 4, SEG + 3], F32)
        scrA = A([128, 2 * SEG], F32)
        ctmp_r = Rot("ctmp", [scrA[:, 0:SEG], scrA[:, SEG:2 * SEG]])
        CT2 = [("ctmp", 0), ("ctmp", 1)]
        ucT = A([128, 4, SEG], BF16)
        sigoT2 = [A([128, 4, SEG], BF16) for _ in range(2)]
        scrB = A([128, 2 * SEG], F32)
        otmp_r = Rot("otmp", [scrB[:, 0:SEG], scrB[:, SEG:2 * SEG]])
        OT2 = [("otmp", 0), ("otmp", 1)]
        pbuf = A([128, 4, SEG + 16], F32)
        ptmp = [A([128, SEG + 16], F32) for _ in range(2)]
        pooled = A([128, 4, SEG], BF16)
        p16 = A([128, 16], F32)
        yT = A([128, 8, SEG], BF16)
        vaug2 = [A([128, TPS, 4, 129], BF16) for _ in range(2)]
        gat = A([128, TPS, 8], F32)
        gsx = A([128, 8, TPS * 4], F32)
        gtmp = A([128, TPS, 4], F32)
        qT = A([128, 4, SEG], BF16)
        kT = A([128, 4, SEG], BF16)
        ktok = A([128, TPS, 4, 128], BF16)
        wv_r = Rot("wv", [A([128, 129], BF16) for _ in range(4)])
        dst_r = Rot("dst", [A([128, 128], BF16) for _ in range(4)])
        cb_r = Rot("cb", [A([128, 129], BF16) for _ in range(4)])
        sm_r = Rot("sm", [A([128, 8], F32) for _ in range(4)])
        hn_r = Rot("hn", [A([128, 512], BF16) for _ in range(2)])
        ytmp_r = Rot("otmp", [scrB[:, 0:SEG].rearrange("p (a b) -> p a b", b=128), scrB[:, SEG:2 * SEG].rearrange("p (a b) -> p a b", b=128)])
        prod_r = Rot("prod", [A([128, 512], F32) for _ in range(2)])
        h2_r = Rot("h2", [A([128, D], BF16) for _ in range(2)])
        h2T_r = Rot("h2T", [A([128, 8, 128], BF16) for _ in range(1)])
        st2 = A([128, 8], F32)
        rt_r = Rot("rt", [A([128, 768], F32) for _ in range(1)])
        print("phase1 arena used", arena.off)

        p.dve(lambda e: e.memset(vaug2[0], 1.0), writes=[("vaug", 0, j) for j in range(TPS)])
        p.dve(lambda e: e.memset(vaug2[1], 1.0), writes=[("vaug", 1, j) for j in range(TPS)])
        p.dve(lambda e: e.memset(ubuf[:, :, 0:3], 0.0), writes=[("u", c, "halo") for c in range(4)])
        p.dve(lambda e: e.memset(pbuf[:, :, 0:16], 0.0), writes=[("p", c, "halo") for c in range(4)])

        x_v = x_d.rearrange("(n p) c -> n p c", p=128)
        X1_v = X1_d.rearrange("(n p) c -> n p c", p=128)
        H2_v = H2_d.rearrange("(n p) c -> n p c", p=128)
        QCS, QTOT, QINVRS, QG, QWS, QAC, QSQA, QTMP = range(8)

        def stF(sg):
            par = sg % 2
            vaug = vaug2[par]
            sigoT = sigoT2[par]
            for j in range(TPS):
                ti = sg * TPS + j
                xs, xsk = xs_r.next()
                xb, xbk = xb_r.next()
                p.dma("sp", (lambda xs, ti: lambda e: e.dma_start(out=xs, in_=x_v[ti]))(xs, ti), writes=[xsk])
                p.act((lambda xs, j: lambda e: e.activation(out=sqj, in_=xs, func=AF.Square, accum_out=ssq1[:, j:j + 1]))(xs, j),
                      reads=[xsk], writes=["sqj", ("ssq1", j)])
                p.act((lambda j: lambda e: e.activation(out=rstd1[:, j:j + 1], in_=ssq1[:, j:j + 1], func=AF.Sqrt, scale=1.0 / D, bias=EPS))(j),
                      reads=[("ssq1", j)], writes=[("std1", j)])
                p.dve((lambda j: lambda e: e.reciprocal(out=rstd1[:, j:j + 1], in_=rstd1[:, j:j + 1]))(j), reads=[("std1", j)], writes=[("rstd1", j)])
                p.dve((lambda xs, xb, j: lambda e: e.tensor_scalar(out=xb, in0=xs, scalar1=rstd1[:, j:j + 1], scalar2=None, op0=ALU.mult))(xs, xb, j),
                      reads=[xsk, ("rstd1", j)], writes=[xbk])
                bk, bkk = nbank()
                bkb = bk[:].bitcast(BF16).rearrange("p (a b) -> p a b", b=128)

                def tr(e, xb=xb, bkb=bkb):
                    for k in range(8):
                        ins = e.transpose(out=bkb[:, k, :], in_=xb[:, k * 128:(k + 1) * 128], identity=identb[:])
                    return ins
                p.pe(tr, reads=[xbk, "identb"], writes=[bkk])
                p.act((lambda bkb, j: lambda e: e.copy(out=xT[:, :, j * 128:(j + 1) * 128], in_=bkb))(bkb, j),
                      reads=[bkk], writes=[("xT", j)])
                yield
            XTALL = [("xT", j) for j in range(TPS)]

            for grp, col0 in (("U", COL_U), ("O", COL_O), ("P", COL_P)):
                for c in range(4):
                    bk, bkk = nbank()
                    c0 = col0 + c * 128

                    def mm(e, bk=bk, c0=c0):
                        for k in range(8):
                            ins = e.matmul(bk[:], lhsT=wi[:, k, c0:c0 + 128], rhs=xT[:, k, :], start=(k == 0), stop=(k == 7))
                        return ins
                    p.pe(mm, reads=WIALL + XTALL, writes=[bkk])
                    if grp == "U":
                        p.act((lambda bk, c: lambda e: e.activation(out=ubuf[:, c, 3:SEG + 3], in_=bk[:], func=AF.Identity,
                                                                    bias=biascol[:, c:c + 1]))(bk, c),
                              reads=[bkk, "biascol"], writes=[("u", c, "body")])
                    elif grp == "O":
                        p.act((lambda bk, c: lambda e: e.activation(out=sigoT[:, c, :], in_=bk[:], func=AF.Sigmoid,
                                                                    bias=biascol[:, 4 + c:5 + c]))(bk, c),
                              reads=[bkk, "biascol"], writes=[("sigo", par, c)])
                    else:
                        p.dve((lambda bk, c: lambda e: e.tensor_copy(out=pbuf[:, c, 16:SEG + 16], in_=bk[:]))(bk, c),
                              reads=[bkk], writes=[("p", c, "body")])
                    yield
            for j in range(TPS):
                bk, bkk = nbank()

                def mmv(e, bk=bk, j=j):
                    for k in range(8):
                        ins = e.matmul(bk[:], lhsT=xT[:, k, j * 128:(j + 1) * 128], rhs=wi[:, k, COL_V:COL_V + 512],
                                       start=(k == 0), stop=(k == 7))
                    return ins
                p.pe(mmv, reads=WIALL + XTALL, writes=[bkk])
                p.dve((lambda bk, j: lambda e: e.tensor_tensor(
                    out=vaug[:, j, :, 0:128], in0=bk[:].rearrange("p (a b) -> p a b", b=128),
                    in1=biasbc[:, 0:512].rearrange("p (a b) -> p a b", b=128), op=ALU.add))(bk, j),
                    reads=[bkk, "biasbc_v"], writes=[("vaug", par, j)])
                bk, bkk = nbank()

                def mmg(e, bk=bk, j=j):
                    for k in range(8):
                        ins = e.matmul(bk[:, 0:8], lhsT=xT[:, k, j * 128:(j + 1) * 128], rhs=wi[:, k, COL_I:COL_I + 8],
                                       start=(k == 0), stop=(k == 7))
                    return ins
                p.pe(mmg, reads=WIALL + XTALL, writes=[bkk])
                p.dve((lambda bk, j: lambda e: e.tensor_tensor(out=gat[:, j, :], in0=bk[:, 0:8], in1=biasbc[:, 512:520], op=ALU.add))(bk, j),
                      reads=[bkk, "biasbc_g"], writes=[("gat", j)])
                yield

        def stM(sg):
            GATALL = [("gat", j) for j in range(TPS)]

            for c in range(4):
                eng = "dve"
                ct, ctk = ctmp_r.next()
                UR = [("u", c, "halo"), ("u", c, "body")]
                p.ve(eng, (lambda ct, c: lambda e: e.tensor_scalar(out=ct, in0=ubuf[:, c, 0:SEG], scalar1=convw[:, c * 4:c * 4 + 1],
                                                                    scalar2=None, op0=ALU.mult))(ct, c),
                     reads=UR + ["colp"], writes=[ctk])
                for k in range(1, 4):
                    p.ve(eng, (lambda ct, c, k: lambda e: e.scalar_tensor_tensor(
                        out=ct, in0=ubuf[:, c, k:k + SEG], scalar=convw[:, c * 4 + k:c * 4 + k + 1], in1=ct,
                        op0=ALU.mult, op1=ALU.add))(ct, c, k), reads=UR + ["colp", ctk], writes=[ctk])
                p.act((lambda ct, c: lambda e: e.activation(out=ucT[:, c, :], in_=ct, func=AF.Silu, bias=convb[:, c:c + 1]))(ct, c),
                      reads=[ctk, "colp"], writes=[("ucT", c)])
                p.ve(eng, (lambda c: lambda e: e.tensor_copy(out=ubuf[:, c, 0:3], in_=ubuf[:, c, SEG:SEG + 3]))(c),
                     reads=[("u", c, "body")], writes=[("u", c, "halo")])
            for g in range(4):
                eng = "pool" if g % 2 == 0 else "dve"
                PR = [("p", g, "halo"), ("p", g, "body")]
                W = SEG + 16
                src = pbuf[:, g, :]
                srck = PR
                sh = 1
                for lvl in range(g + 1):
                    dstt = ptmp[lvl % 2]
                    dk = ("ptmp", lvl % 2)
                    lo = 2 * sh
                    p.ve(eng, (lambda dstt, src, sh, lo: lambda e: e.tensor_tensor(
                        out=dstt[:, lo:W], in0=src[:, lo:W], in1=src[:, lo - sh:W - sh], op=ALU.add))(dstt, src, sh, lo),
                        reads=list(srck), writes=[dk])
                    src, srck, sh = dstt, [dk], sh * 2
                wg_ = float(2 ** (g + 1))
                p.ve("dve", (lambda src, g, wg_: lambda e: e.scalar_tensor_tensor(
                    out=pooled[:, g, :], in0=src[:, 16:W], scalar=1.0 / wg_, in1=pbuf[:, g, 16:W],
                    op0=ALU.mult, op1=ALU.subtract))(src, g, wg_), reads=list(srck) + PR, writes=[("pooled", g)])
                if sg == 0:
                    p.ve(eng, (lambda src, g: lambda e: e.tensor_tensor(out=p16, in0=src[:, 16:32], in1=inv0_bc[:, g * 16:(g + 1) * 16],
                                                                        op=ALU.mult))(src, g),
                         reads=list(srck) + ["rowb"], writes=["p16"])
                    p.ve(eng, (lambda g: lambda e: e.tensor_tensor(out=pooled[:, g, 0:16], in0=p16, in1=pbuf[:, g, 16:32],
                                                                   op=ALU.subtract))(g),
                         reads=["p16"] + PR, writes=[("pooled", g)])
                p.ve(eng, (lambda g: lambda e: e.tensor_copy(out=pbuf[:, g, 0:16], in_=pbuf[:, g, SEG:SEG + 16]))(g),
                     reads=[("p", g, "body")], writes=[("p", g, "halo")])
                bk, bkk = nbank()
                p.pe((lambda bk, g: lambda e: e.matmul(bk[:], lhsT=wpl[:, g, :], rhs=pooled[:, g, :], start=True, stop=True))(bk, g),
                     reads=["wpl", ("pooled", g)], writes=[bkk])
                p.act((lambda bk, g: lambda e: e.activation(out=yT[:, 4 + g, :], in_=bk[:], func=AF.Identity,
                                                            scale=pscalec[:, g:g + 1], bias=bpscol[:, g:g + 1]))(bk, g),
                      reads=[bkk, "colp", "bpscol"], writes=[("yT", 4 + g)])

            gi = gat[:, :, 0:4]
            gf = gat[:, :, 4:8]
            q = lambda n: gsx[:, n, :].rearrange("p (a b) -> p a b", b=4)
            p.act(lambda e: e.activation(out=gtmp, in_=gf, func=AF.Exp, scale=-1.0), reads=GATALL, writes=["gtmp"])
            p.act(lambda e: e.activation(out=gtmp, in_=gtmp, func=AF.Ln, bias=1.0), reads=["gtmp"], writes=["gtmp"])
            bk, bkk = nbank()

            def mmcs(e, bk=bk):
                for j in range(TPS):
                    e.matmul(bk[:, j * 4:(j + 1) * 4], lhsT=tri[:], rhs=gtmp[:, j, :], start=True, stop=True)
                for j in range(TPS):
                    ins = e.matmul(bk[:, 64 + j * 4:64 + (j + 1) * 4], lhsT=onesf[:], rhs=gtmp[:, j, :], start=True, stop=True)
                return ins
            p.pe(mmcs, reads=["gtmp", "tri", "onesf"], writes=[bkk])
            p.dve((lambda bk: lambda e: e.tensor_copy(out=gsx[:, QCS, :], in_=bk[:, 0:TPS * 4]))(bk), reads=[bkk], writes=["q_cs"])
            p.dve((lambda bk: lambda e: e.tensor_copy(out=gsx[:, QTOT, :], in_=bk[:, 64:64 + TPS * 4]))(bk), reads=[bkk], writes=["q_tot"])
            p.dve(lambda e: e.scalar_tensor_tensor(out=gsx[:, QTMP, :], in0=gsx[:, QTOT, :], scalar=-0.5, in1=gsx[:, QCS, :],
                                                   op0=ALU.mult, op1=ALU.add), reads=["q_cs", "q_tot"], writes=["q_tmp"])
            p.act(lambda e: e.activation(out=gsx[:, QINVRS, :], in_=gsx[:, QTMP, :], func=AF.Exp), reads=["q_tmp"], writes=["q_invrs"])
            p.dve(lambda e: e.tensor_tensor(out=q(QG), in0=q(QTMP), in1=gi, op=ALU.add), reads=["q_tmp"] + GATALL, writes=["q_g"])
            p.act(lambda e: e.activation(out=gsx[:, QG, :], in_=gsx[:, QG, :], func=AF.Exp), reads=["q_g"], writes=["q_g"])
            p.dve(lambda e: e.tensor_tensor(out=gsx[:, QWS, :], in0=gsx[:, QCS, :], in1=gsx[:, QTOT, :], op=ALU.subtract),
                  reads=["q_cs", "q_tot"], writes=["q_ws"])
            p.dve(lambda e: e.tensor_tensor(out=q(QWS), in0=q(QWS), in1=gi, op=ALU.add), reads=["q_ws"] + GATALL, writes=["q_ws"])
            p.act(lambda e: e.activation(out=gsx[:, QWS, :], in_=gsx[:, QWS, :], func=AF.Exp), reads=["q_ws"], writes=["q_ws"])
            p.act(lambda e: e.activation(out=gsx[:, QAC, :], in_=gsx[:, QTOT, :], func=AF.Exp, scale=-1.0), reads=["q_tot"], writes=["q_ac"])
            p.act(lambda e: e.activation(out=gsx[:, QSQA, :], in_=gsx[:, QTOT, :], func=AF.Exp, scale=-0.5), reads=["q_tot"], writes=["q_sqa"])

            for h in range(4):
                for wsel, dstT, sc, nm in ((0, qT, 128.0 ** -0.5, "qT"), (1, kT, 1.0, "kT")):
                    bk, bkk = nbank()
                    p.pe((lambda bk, h, wsel: lambda e: e.matmul(bk[:], lhsT=wqk[:, wsel, h, :], rhs=ucT[:, h, :], start=True, stop=True))(bk, h, wsel),
                         reads=["wq", "wk", ("ucT", h)], writes=[bkk])
                    p.act((lambda bk, h, dstT, sc: lambda e: e.activation(out=dstT[:, h, :], in_=bk[:], func=AF.Copy, scale=sc))(bk, h, dstT, sc),
                          reads=[bkk], writes=[(nm, h)])
            for j in range(TPS):
                bk, bkk = nbank()

                def mmk(e, bk=bk, j=j):
                    for h in range(4):
                        ins = e.matmul(bk[:, h * 128:(h + 1) * 128], lhsT=ucT[:, h, j * 128:(j + 1) * 128], rhs=wqk[:, 1, h, :],
                                       start=True, stop=True)
                    return ins
                p.pe(mmk, reads=["wk"] + [("ucT", h) for h in range(4)], writes=[bkk])
                p.dve((lambda bk, j: lambda e: e.tensor_copy(out=ktok[:, j, :, :], in_=bk[:].rearrange("p (a b) -> p a b", b=128)))(bk, j),
                      reads=[bkk], writes=[("ktok", j)])

        def stK(sg, pump):
            par = sg % 2
            vaug = vaug2[par]
            sigoT = sigoT2[par]
            ctxs = {}
            hns = {}

            def S1(n):
                j, h = divmod(n, 4)
                jh = n
                if h == 0:
                    hns[j] = hn_r.next()
                c = {}
                eng2 = "pool" if h % 2 == 0 else "dve"
                wv, wvk = wv_r.next()
                p.ve(eng2, (lambda wv, j, h, jh: lambda e: e.tensor_scalar(out=wv, in0=vaug[:, j, h, :], scalar1=gsx[:, QWS, jh:jh + 1],
                                                                          scalar2=None, op0=ALU.mult))(wv, j, h, jh),
                     reads=[("vaug", par, j), "q_ws"], writes=[wvk])
                bkA, bkAk = nbank()
                p.pe((lambda bkA, wv, j, h: lambda e: e.matmul(bkA[:, 0:129], lhsT=ktok[:, j, h, :], rhs=wv, start=True, stop=True))(bkA, wv, j, h),
                     reads=[("ktok", j), wvk], writes=[bkAk])
                p.pe((lambda bkA, j, h: lambda e: e.matmul(bkA[:, 256:384], lhsT=kT[:, h, j * 128:(j + 1) * 128],
                                                           rhs=qT[:, h, j * 128:(j + 1) * 128], start=True, stop=True))(bkA, j, h),
                     reads=[("kT", h), ("qT", h)], writes=[bkAk])
                ds, dsk = dst_r.next()
                p.dve((lambda ds, bkA, jh: lambda e: e.scalar_tensor_tensor(out=ds, in0=bkA[:, 256:384], scalar=gsx[:, QG, jh:jh + 1],
                                                                           in1=trib[:], op0=ALU.mult, op1=ALU.mult))(ds, bkA, jh),
                      reads=[bkAk, "q_g", "trib"], writes=[dsk])
                cb, cbk = cb_r.next()
                p.ve(eng2, (lambda cb, h, jh: lambda e: e.tensor_scalar(out=cb, in0=Cst[:, h, :], scalar1=gsx[:, QSQA, jh:jh + 1],
                                                                       scalar2=None, op0=ALU.mult))(cb, h, jh),
                     reads=[("C", h), "q_sqa"], writes=[cbk])
                p.dve((lambda bkA, h, jh: lambda e: e.scalar_tensor_tensor(out=Cst[:, h, :], in0=Cst[:, h, :], scalar=gsx[:, QAC, jh:jh + 1],
                                                                          in1=bkA[:, 0:129], op0=ALU.mult, op1=ALU.add))(bkA, h, jh),
                      reads=[("C", h), "q_ac", bkAk], writes=[("C", h)])
                c.update(ds=ds, dsk=dsk, cb=cb, cbk=cbk)
                ctxs[n] = c

            def S2(n):
                j, h = divmod(n, 4)
                jh = n
                c = ctxs[n]
                ds, dsk, cb, cbk = c["ds"], c["dsk"], c["cb"], c["cbk"]
                bkO, bkOk = nbank()

                def mmo(e, bkO=bkO, ds=ds, cb=cb, j=j, h=h):
                    e.matmul(bkO[:, 0:129], lhsT=ds, rhs=vaug[:, j, h, :], start=True, stop=False)
                    return e.matmul(bkO[:, 0:129], lhsT=qT[:, h, j * 128:(j + 1) * 128], rhs=cb, start=False, stop=True)
                p.pe(mmo, reads=[dsk, ("vaug", par, j), ("qT", h), cbk], writes=[bkOk])
                sm, smk = sm_r.next()
                p.act((lambda sm, bkO: lambda e: e.activation(out=sm[:, 0:1], in_=bkO[:, 128:129], func=AF.Abs))(sm, bkO),
                      reads=[bkOk], writes=[(smk, 0)])
                p.dve((lambda sm, jh: lambda e: e.tensor_tensor(out=sm[:, 1:2], in0=sm[:, 0:1], in1=gsx[:, QINVRS, jh:jh + 1], op=ALU.max))(sm, jh),
                      reads=[(smk, 0), "q_invrs"], writes=[(smk, 1)])
                p.dve((lambda sm: lambda e: e.reciprocal(out=sm[:, 2:3], in_=sm[:, 1:2]))(sm), reads=[(smk, 1)], writes=[(smk, 2)])
                p.act((lambda sm, bkO: lambda e: e.activation(out=sqj[:, 0:128], in_=bkO[:, 0:128], func=AF.Square, scale=sm[:, 2:3],
                                                              accum_out=sm[:, 3:4]))(sm, bkO),
                      reads=[bkOk, (smk, 2)], writes=["sqj", (smk, 3)])
                p.act((lambda sm: lambda e: e.activation(out=sm[:, 4:5], in_=sm[:, 3:4], func=AF.Sqrt, scale=1.0 / 128, bias=EPS))(sm),
                      reads=[(smk, 3)], writes=[(smk, 4)])
                c.update(bkO=bkO, bkOk=bkOk, sm=sm, smk=smk)

            def S3(n):
                j, h = divmod(n, 4)
                c = ctxs[n]
                bkO, bkOk, sm, smk = c["bkO"], c["bkOk"], c["sm"], c["smk"]
                hn, hnk = hns[j]
                p.dve((lambda sm: lambda e: e.reciprocal(out=sm[:, 5:6], in_=sm[:, 4:5]))(sm), reads=[(smk, 4)], writes=[(smk, 5)])
                p.dve((lambda sm: lambda e: e.tensor_tensor(out=sm[:, 6:7], in0=sm[:, 5:6], in1=sm[:, 2:3], op=ALU.mult))(sm),
                      reads=[(smk, 5), (smk, 2)], writes=[(smk, 6)])
                p.dve((lambda sm, bkO, hn, h: lambda e: e.scalar_tensor_tensor(
                    out=hn[:, h * 128:(h + 1) * 128], in0=bkO[:, 0:128], scalar=sm[:, 6:7], in1=normg_bc[:, h * 128:(h + 1) * 128],
                    op0=ALU.mult, op1=ALU.mult))(sm, bkO, hn, h), reads=[bkOk, (smk, 6), "rowb"], writes=[(hnk, h)])
                if h == 3:
                    bk, bkk = nbank()
                    bkb = bk[:].bitcast(BF16).rearrange("p (a b) -> p a b", b=128)

                    def trh(e, hn=hn, bkb=bkb):
                        for hh in range(4):
                            ins = e.transpose(out=bkb[:, hh, :], in_=hn[:, hh * 128:(hh + 1) * 128], identity=identb[:])
                        return ins
                    p.pe(trh, reads=[(hnk, hh) for hh in range(4)] + ["identb"], writes=[bkk])
                    yt, ytk = ytmp_r.next()
                    for hh in range(4):
                        p.dve((lambda yt, bkb, j, hh: lambda e: e.scalar_tensor_tensor(
                            out=yt[:, hh, :], in0=ucT[:, hh, j * 128:(j + 1) * 128], scalar=skipc[:, hh:hh + 1], in1=bkb[:, hh, :],
                            op0=ALU.mult, op1=ALU.add))(yt, bkb, j, hh),
                            reads=[bkk, ("ucT", hh), "colp"], writes=[ytk])
                    p.pool((lambda yt, j: lambda e: e.tensor_tensor(out=yT[:, 0:4, j * 128:(j + 1) * 128], in0=yt, in1=sigoT[:, :, j * 128:(j + 1) * 128],
                                                                    op=ALU.mult))(yt, j),
                           reads=[ytk] + [("sigo", par, cc_) for cc_ in range(4)], writes=[("yTm", j)])

            NIT = TPS * 4
            for step in range(NIT + 2):
                if step < NIT:
                    S1(step)
                if 0 <= step - 1 < NIT:
                    S2(step - 1)
                if 0 <= step - 2 < NIT:
                    S3(step - 2)
                pump(2 if step % 2 == 0 else 1)

        def stE(sg):
            YTALL = [("yT", 4 + g) for g in range(4)] + [("yTm", j) for j in range(TPS)]

            ectx = {}

            def EW(j):
                ti = sg * TPS + j
                xs, xsk = xs_r.next()
                p.dma("sp", (lambda xs, ti: lambda e: e.dma_start(out=xs, in_=x_v[ti]))(xs, ti), writes=[xsk])
                x1, x1k = xs, xsk
                X1K = [xsk]
                for half in range(2):
                    bk, bkk = nbank()

                    def mmw(e, bk=bk, j=j, half=half):
                        for k in range(8):
                            ins = e.matmul(bk[:], lhsT=yT[:, k, j * 128:(j + 1) * 128], rhs=wo[:, k, half * 512:(half + 1) * 512],
                                           start=(k == 0), stop=(k == 7))
                        return ins
                    p.pe(mmw, reads=YTALL + WOALL, writes=[bkk])
                    p.dve((lambda bk, x1, half: lambda e: e.tensor_tensor(out=x1[:, half * 512:(half + 1) * 512], in0=bk[:],
                                                                         in1=x1[:, half * 512:(half + 1) * 512], op=ALU.add))(bk, x1, half),
                          reads=[bkk, xsk], writes=[xsk])
                ectx[j] = (x1, x1k, X1K, ti)

            def EP(j):
                x1, x1k, X1K, ti = ectx[j]
                p.dma("act", (lambda x1, ti: lambda e: e.dma_start(out=X1_v[ti], in_=x1))(x1, ti), reads=X1K, writes=[("X1", ti)])
                p.act((lambda x1, j: lambda e: e.activation(out=sqj, in_=x1, func=AF.Square, accum_out=st2[:, j:j + 1]))(x1, j),
                      reads=X1K, writes=["sqj", ("st2a", j)])
                p.act((lambda j: lambda e: e.activation(out=st2[:, 4 + j:5 + j], in_=st2[:, j:j + 1], func=AF.Sqrt, scale=1.0 / D, bias=EPS))(j),
                      reads=[("st2a", j)], writes=[("st2b", j)])
                p.dve((lambda j: lambda e: e.reciprocal(out=st2[:, 4 + j:5 + j], in_=st2[:, 4 + j:5 + j]))(j), reads=[("st2b", j)], writes=[("st2c", j)])
                h2f, h2fk = scrA, "h2f"
                h2, h2k = h2_r.next()
                p.dve((lambda h2f, x1, j: lambda e: e.scalar_tensor_tensor(out=h2f, in0=x1, scalar=st2[:, 4 + j:5 + j], in1=s2bc[:],
                                                                          op0=ALU.mult, op1=ALU.mult))(h2f, x1, j),
                      reads=X1K + [("st2c", j), "s2bc"], writes=[h2fk] + CT2)
                p.pool((lambda h2, h2f: lambda e: e.tensor_tensor(out=h2, in0=h2f, in1=shift_f_bc, op=ALU.add))(h2, h2f),
                       reads=[h2fk] + CT2 + MODALL, writes=[h2k])
                p.dma("act", (lambda h2, ti: lambda e: e.dma_start(out=H2_v[ti], in_=h2))(h2, ti), reads=[h2k], writes=[("H2", ti)])
                bk, bkk = nbank()
                bkb = bk[:].bitcast(BF16).rearrange("p (a b) -> p a b", b=128)

                def tr2(e, h2=h2, bkb=bkb):
                    for k in range(8):
                        ins = e.transpose(out=bkb[:, k, :], in_=h2[:, k * 128:(k + 1) * 128], identity=identb[:])
                    return ins
                p.pe(tr2, reads=[h2k, "identb"], writes=[bkk])
                h2T, h2Tk = h2T_r.next()
                p.act((lambda h2T, bkb: lambda e: e.copy(out=h2T, in_=bkb))(h2T, bkb), reads=[bkk], writes=[h2Tk])

                def mmr(e, j=j, h2T=h2T):
                    for k in range(8):
                        ins = e.matmul(banks[7][:, j * 36:(j + 1) * 36], lhsT=h2T[:, k, :], rhs=wr[:, k, :], start=(k == 0), stop=(k == 7))
                    return ins
                p.pe(mmr, reads=[h2Tk, "wr"], writes=[("rbank", j)])
                if j < TPS - 1:
                    return
                t0 = sg * TPS
                rt, rtk = rt_r.next()
                RB = [("rbank", jj) for jj in range(TPS)]
                v3 = lambda a, n_: rt[:, a:a + TPS * n_].rearrange("p (t n) -> p t n", n=n_)
                lg = v3(0, 36)
                elm = v3(144, 32)
                elm2 = v3(272, 32)
                oh1f = v3(400, 32)
                oh2f = v3(528, 32)
                ohg = v3(656, 4)
                pen = v3(672, 4)
                egj = v3(688, 4)
                s4 = lambda a: rt[:, 704 + a * 4:708 + a * 4]
                R = lambda *a: [(rtk, x) for x in a]
                bc3 = lambda ap2, n_: ap2.unsqueeze(2).to_broadcast([128, TPS, n_])
                p.dve(lambda e: e.tensor_tensor(out=lg, in0=banks[7][:, 0:TPS * 36].rearrange("p (t n) -> p t n", n=36),
                                                in1=br_bc.unsqueeze(1).to_broadcast([128, TPS, 36]), op=ALU.add),
                      reads=RB + ["rowb"], writes=R("lg"))
                p.dve(lambda e: e.reduce_max(out=s4(0), in_=lg[:, :, 0:4], axis=AX.X), reads=R("lg"), writes=R("gmax"))
                p.dve(lambda e: e.tensor_tensor(out=ohg, in0=lg[:, :, 0:4], in1=bc3(s4(0), 4), op=ALU.is_equal), reads=R("lg", "gmax"), writes=R("ohg"))
                p.dve(lambda e: e.tensor_tensor(out=egj, in0=lg[:, :, 0:4], in1=bc3(s4(0), 4), op=ALU.subtract), reads=R("lg", "gmax"), writes=R("egj"))
                p.act(lambda e: e.activation(out=egj, in_=egj, func=AF.Exp), reads=R("egj"), writes=R("egj"))
                p.dve(lambda e: e.reduce_sum(out=s4(1), in_=egj, axis=AX.X), reads=R("egj"), writes=R("sumg"))
                p.dve(lambda e: e.reciprocal(out=s4(2), in_=s4(1)), reads=R("sumg"), writes=R("ggate"))
                p.dve(lambda e: e.tensor_scalar(out=pen, in0=ohg, scalar1=-1.0, scalar2=BIG, op0=ALU.add, op1=ALU.mult), reads=R("ohg"), writes=R("pen"))
                p.dve(lambda e: e.tensor_tensor(out=elm.rearrange("p t (a b) -> p t a b", b=8),
                                                in0=lg[:, :, 4:36].rearrange("p t (a b) -> p t a b", b=8),
                                                in1=pen.unsqueeze(3).to_broadcast([128, TPS, 4, 8]), op=ALU.add),
                      reads=R("lg", "pen"), writes=R("elm"))
                p.dve(lambda e: e.reduce_max(out=s4(3), in_=elm, axis=AX.X), reads=R("elm"), writes=R("m1"))
                p.dve(lambda e: e.tensor_tensor(out=oh1f, in0=elm, in1=bc3(s4(3), 32), op=ALU.is_equal), reads=R("elm", "m1"), writes=R("oh1"))
                p.dve(lambda e: e.scalar_tensor_tensor(out=elm2, in0=oh1f, scalar=-BIG, in1=elm, op0=ALU.mult, op1=ALU.add),
                      reads=R("elm", "oh1"), writes=R("elm2"))
                p.dve(lambda e: e.reduce_max(out=s4(4), in_=elm2, axis=AX.X), reads=R("elm2"), writes=R("m2"))
                p.dve(lambda e: e.tensor_tensor(out=oh2f, in0=elm2, in1=bc3(s4(4), 32), op=ALU.is_equal), reads=R("elm2", "m2"), writes=R("oh2"))
                p.dve(lambda e: e.tensor_tensor(out=s4(5), in0=s4(3), in1=s4(4), op=ALU.subtract), reads=R("m1", "m2"), writes=R("d12"))
                p.act(lambda e: e.activation(out=s4(6), in_=s4(5), func=AF.Sigmoid), reads=R("d12"), writes=R("p1"))
                p.act(lambda e: e.activation(out=s4(7), in_=s4(5), func=AF.Sigmoid, scale=-1.0), reads=R("d12"), writes=R("p2"))
                p.dve(lambda e: e.tensor_tensor(out=gts[:, t0:t0 + TPS, 0], in0=s4(6), in1=s4(2), op=ALU.mult), reads=R("p1", "ggate"), writes=[("gts", t0, 0)])
                p.dve(lambda e: e.tensor_tensor(out=gts[:, t0:t0 + TPS, 1], in0=s4(7), in1=s4(2), op=ALU.mult), reads=R("p2", "ggate"), writes=[("gts", t0, 1)])
                p.dve(lambda e: e.tensor_copy(out=ohs[:, t0:t0 + TPS, 0, :], in_=oh1f), reads=R("oh1"), writes=[("ohs", t0, 0)])
                p.dve(lambda e: e.tensor_copy(out=ohs[:, t0:t0 + TPS, 1, :], in_=oh2f), reads=R("oh2"), writes=[("ohs", t0, 1)])
                p.dve(lambda e: e.tensor_tensor(out=ohsum[:, t0:t0 + TPS, :], in0=oh1f, in1=oh2f, op=ALU.add), reads=R("oh1", "oh2"), writes=[("ohsum", t0)])
            EW(0)
            EW(1)
            EP(0)
            EW(2)
            EP(1)
            EW(3)
            EP(2)
            EP(3)
            pump_zero((NB + NSEG - 1) // NSEG)

        for _ in stF(0):
            pass
        for sg in range(NSEG):
            stM(sg)
            gen = stF(sg + 1) if sg + 1 < NSEG else iter(())

            def pump(n, gen=gen):
                for _ in range(n):
                    next(gen, None)
            stK(sg, pump)
            for _ in gen:
                pass
            stE(sg)

        pump_zero(NB)
        p.barrier()
        arena.reset()
        cnt = A([128, 32], F32)
        cmpJ = A([128, 32, JMAX], F32)
        nbk = A([128, 32], F32)
        cs_a = A([128, 32], F32)
        cs_b = A([128, 32], F32)
        pstart = A([128, 32], F32)
        cmpB = A([128, NB, 32], F32)
        bef = A([128, NB], F32)
        widxf = A([128, NB, 4], F32)
        gidxf = A([128, NB], F32)
        pos_r = Rot("pos", [A([128, 96], F32) for _ in range(2)])
        hsb_r = Rot("hsb", [A([128, D], BF16) for _ in range(3)])
        OHSUM = [("ohsum", i) for i in range(0, NT, TPS)]
        bk, bkk = nbank()

        def mmcnt(e, bk=bk):
            for i in range(NT):
                ins = e.matmul(bk[:, 0:32], lhsT=onesb[:], rhs=ohsum[:, i, :], start=(i == 0), stop=(i == NT - 1))
            return ins
        p.pe(mmcnt, reads=OHSUM + ["onesb"], writes=[bkk])
        p.dve((lambda bk: lambda e: e.tensor_copy(out=cnt, in_=bk[:, 0:32]))(bk), reads=[bkk], writes=["cnt"])
        p.dve(lambda e: e.tensor_tensor(out=cmpJ, in0=cnt.unsqueeze(2).to_broadcast([128, 32, JMAX]),
                                        in1=thr_bc.unsqueeze(1).to_broadcast([128, 32, JMAX]), op=ALU.is_gt),
              reads=["cnt", "rowb"], writes=["cmpJ"])
        p.dve(lambda e: e.reduce_sum(out=nbk, in_=cmpJ, axis=AX.X), reads=["cmpJ"], writes=["nbk"])
        p.dve(lambda e: e.tensor_scalar(out=nbk, in0=nbk, scalar1=float(B), scalar2=None, op0=ALU.mult), reads=["nbk"], writes=["padded"])
        src, srck = nbk, "padded"
        bufs = [(cs_a, "cs_a"), (cs_b, "cs_b")]
        for li, sh in enumerate((1, 2, 4, 8, 16)):
            dstt, dk = bufs[li % 2]
            p.dve((lambda dstt, src, sh: lambda e: e.tensor_copy(out=dstt[:, 0:sh], in_=src[:, 0:sh]))(dstt, src, sh), reads=[srck], writes=[(dk, 0)])
            p.dve((lambda dstt, src, sh: lambda e: e.tensor_tensor(out=dstt[:, sh:32], in0=src[:, sh:32], in1=src[:, 0:32 - sh], op=ALU.add))(dstt, src, sh),
                  reads=[srck], writes=[(dk, 1)])
            p.dve((lambda dstt: lambda e: e.tensor_copy(out=dstt[:, 0:1], in_=dstt[:, 0:1]))(dstt), reads=[(dk, 0), (dk, 1)], writes=[dk])
            src, srck = dstt, dk
        pend, pendk = src, srck
        p.dve(lambda e: e.tensor_tensor(out=pstart, in0=pend, in1=nbk, op=ALU.subtract), reads=[pendk, "padded"], writes=["pstart"])
        p.dve(lambda e: e.tensor_tensor(out=cmpB, in0=pend.unsqueeze(1).to_broadcast([128, NB, 32]),
                                        in1=bpos_bc.unsqueeze(2).to_broadcast([128, NB, 32]), op=ALU.is_le),
              reads=[pendk, "rowb"], writes=["cmpB"])
        p.dve(lambda e: e.reduce_sum(out=bef, in_=cmpB, axis=AX.X), reads=["cmpB"], writes=["bef0"])
        p.dve(lambda e: e.tensor_scalar(out=gidxf, in0=bef, scalar1=128.0, scalar2=None, op0=ALU.mult), reads=["bef0"], writes=["gidxf0"])
        p.dve(lambda e: e.tensor_tensor(out=gidxf, in0=gidxf, in1=rowoff[:, 0:1].to_broadcast([128, NB]), op=ALU.add),
              reads=["gidxf0", "rowoff"], writes=["gidxf"])
        p.dve(lambda e: e.tensor_copy(out=widx[:], in_=gidxf), reads=["gidxf"], writes=["widx"])
        p.dve(lambda e: e.tensor_scalar(out=gidxf, in0=gidxf, scalar1=float(NE * 128 - 1), scalar2=None, op0=ALU.min),
              reads=["gidxf", "widx"], writes=["gidxc"])
        p.dve(lambda e: e.tensor_copy(out=widxc[:], in_=gidxf), reads=["gidxc"], writes=["widxc"])
        wg_r = Rot("wg", [A([128, 8, 512], BF16) for _ in range(2)])
        wu_r = Rot("wu", [A([128, 8, 512], BF16) for _ in range(2)])
        wd_r = Rot("wd", [A([128, 4, D], BF16) for _ in range(2)])
        NSKIP = 15

        def gather_kw(b):
            if b >= NB - NSKIP:
                return dict(in_offset=bass.IndirectOffsetOnAxis(ap=widx[:, b:b + 1], axis=0), bounds_check=NE * 128 - 1, oob_is_err=False)
            return dict(in_offset=bass.IndirectOffsetOnAxis(ap=widxc[:, b:b + 1], axis=0))
        order = []
        lo, hi = 0, NB - 1
        while lo <= hi:
            order.append(lo); lo += 1
            if lo <= hi:
                order.append(lo); lo += 1
            if lo <= hi and hi >= NB - NSKIP:
                order.append(hi); hi -= 1
        assert sorted(order) == list(range(NB))
        nxt = {order[i]: (order[i + 2] if i + 2 < NB else None) for i in range(NB)}
        wbufs = {}

        def issue_gathers(b):
            wg_, wgk = wg_r.next()
            wu_, wuk = wu_r.next()
            wd_, wdk = wd_r.next()
            p.dma("pool", (lambda wg_, b: lambda e: e.indirect_dma_start(
                out=wg_.rearrange("p a b -> p (a b)"), out_offset=None, in_=wgate_d,
                **gather_kw(b)))(wg_, b),
                reads=["widx", "widxc"], writes=[(wgk, k) for k in range(8)])
            p.dma("pool", (lambda wu_, b: lambda e: e.indirect_dma_start(
                out=wu_.rearrange("p a b -> p (a b)"), out_offset=None, in_=wup_d,
                **gather_kw(b)))(wu_, b),
                reads=["widx", "widxc"], writes=[(wuk, k) for k in range(8)])
            p.dma("pool", (lambda wd_, b: lambda e: e.indirect_dma_start(
                out=wd_.rearrange("p a b -> p (a b)"), out_offset=None, in_=wdown_d,
                **gather_kw(b)))(wd_, b),
                reads=["widx", "widxc"], writes=[(wdk, k) for k in range(4)])
            wbufs[b] = (wg_, wgk, wu_, wuk, wd_, wdk)
        issue_gathers(order[0])
        issue_gathers(order[1])
        for i in range(NT):
            bk, bkk = nbank()

            def mmrk(e, bk=bk, i=i):
                ins = e.matmul(bk[:, 0:32], lhsT=striub[:], rhs=ohsum[:, i, :], start=True, stop=(i == 0))
                for j2 in range(i):
                    ins = e.matmul(bk[:, 0:32], lhsT=onesb[:], rhs=ohsum[:, j2, :], start=False, stop=(j2 == i - 1))
                return ins
            p.pe(mmrk, reads=OHSUM + ["onesb", "striub"], writes=[bkk])
            ps, psk = pos_r.next()
            p.dve((lambda bk, ps: lambda e: e.tensor_tensor(out=ps[:, 0:32], in0=bk[:, 0:32], in1=pstart, op=ALU.add))(bk, ps),
                  reads=[bkk, "pstart"], writes=[(psk, 0)])
            p.dve((lambda ps, i: lambda e: e.tensor_tensor(out=ps[:, 32:96].rearrange("p (a b) -> p a b", b=32), in0=ohs[:, i, :, :],
                                                           in1=ps[:, 0:32].unsqueeze(1).to_broadcast([128, 2, 32]), op=ALU.mult))(ps, i),
                  reads=[(psk, 0), ("ohs", i // TPS * TPS, 0), ("ohs", i // TPS * TPS, 1)], writes=[(psk, 1)])
            p.dve((lambda ps, i: lambda e: e.reduce_sum(out=destf[:, i, :], in_=ps[:, 32:96].rearrange("p (a b) -> p a b", b=32), axis=AX.X))(ps, i),
                  reads=[(psk, 1)], writes=[("destf", i)])
        p.dve(lambda e: e.tensor_copy(out=desti[:], in_=destf[:]), reads=[("destf", i) for i in range(NT)], writes=["desti"])
        dump("destf", destf[:], "desti")
        HSZ = [("Hs", "z", n, a_) for n in range(nz) for a_ in range(BT)]
        for i in range(NT):
            hb, hbk = hsb_r.next()
            p.dma("sp", (lambda hb, i: lambda e: e.dma_start(out=hb, in_=H2_v[i]))(hb, i), reads=[("H2", i)], writes=[hbk])
            for k in range(2):
                p.dma("pool", (lambda hb, i, k: lambda e: e.indirect_dma_start(
                    out=Hs_d, out_offset=bass.IndirectOffsetOnAxis(ap=desti[:, i, k:k + 1], axis=0), in_=hb, in_offset=None))(hb, i, k),
                    reads=[hbk, "desti"] + HSZ, writes=[("Hs", "s", i, k)])
        HSALL = [("Hs", "s", i, k) for i in range(NT) for k in range(2)]

        hsl_r = Rot("hsl", [A([128, BT, D], BF16) for _ in range(2)])
        hT_r = Rot("hT", [A([128, 8, B], BF16) for _ in range(2)])
        aT_r = Rot("aT", [A([128, 4, B], BF16) for _ in range(2)])
        sg_r = Rot("sg", [A([128, B], F32) for _ in range(2)])
        ysb_r = Rot("ysb", [A([128, D], F32) for _ in range(3)])
        print("phase3 arena used", arena.off)
        Hs_b = Hs_d.rearrange("(n a p) c -> n p a c", p=128, a=BT)
        Y_v = Y_d.rearrange("(n p) c -> n p c", p=128)
        hsl_bufs = {}

        def load_hsl(b):
            hsl, hslk = hsl_r.next()
            p.dma("sp", (lambda hsl, b: lambda e: e.dma_start(out=hsl, in_=Hs_b[b]))(hsl, b), reads=HSALL + HSZ, writes=[hslk])
            hsl_bufs[b] = (hsl, hslk)
        load_hsl(order[0])
        load_hsl(order[1])
        for b in order:
            if b not in wbufs:
                issue_gathers(b)
            wg_, wgk, wu_, wuk, wd_, wdk = wbufs[b]
            WG = [(wgk, k) for k in range(8)]
            WU = [(wuk, k) for k in range(8)]
            WD = [(wdk, k) for k in range(4)]
            hsl, hslk = hsl_bufs[b]
            hT, hTk = hT_r.next()
            for a in range(BT):
                bk, bkk = nbank()
                bkb = bk[:].bitcast(BF16).rearrange("p (a b) -> p a b", b=128)

                def tr3(e, hsl=hsl, bkb=bkb, a=a):
                    for k in range(8):
                        ins = e.transpose(out=bkb[:, k, :], in_=hsl[:, a, :].rearrange("s (p k) -> s k p", k=8)[:, k, :], identity=identb[:])
                    return ins
                p.pe(tr3, reads=[hslk, "identb"], writes=[bkk])
                eng = "act" if a % 2 == 0 else "dve"
                if eng == "act":
                    p.act((lambda hT, bkb, a: lambda e: e.copy(out=hT[:, :, a * 128:(a + 1) * 128], in_=bkb))(hT, bkb, a),
                          reads=[bkk], writes=[(hTk, a)])
                else:
                    p.dve((lambda hT, bkb, a: lambda e: e.tensor_copy(out=hT[:, :, a * 128:(a + 1) * 128], in_=bkb))(hT, bkb, a),
                          reads=[bkk], writes=[(hTk, a)])
            HT = [(hTk, a) for a in range(BT)]
            if nxt[b] is not None:
                load_hsl(nxt[b])
            aT, aTk = aT_r.next()
            for hc in range(4):
                bkG, bkGk = nbank()
                bkU, bkUk = nbank()

                def mmg2(e, bkG=bkG, wg_=wg_, hT=hT, hc=hc):
                    for k in range(8):
                        ins = e.matmul(bkG[:, 0:B], lhsT=wg_[:, k, :].rearrange("d (p c) -> d c p", c=4)[:, hc, :], rhs=hT[:, k, :], start=(k == 0), stop=(k == 7))
                    return ins
                p.pe(mmg2, reads=WG + HT, writes=[bkGk])

                def mmu2(e, bkU=bkU, wu_=wu_, hT=hT, hc=hc):
                    for k in range(8):
                        ins = e.matmul(bkU[:, 0:B], lhsT=wu_[:, k, :].rearrange("d (p c) -> d c p", c=4)[:, hc, :], rhs=hT[:, k, :], start=(k == 0), stop=(k == 7))
                    return ins
                p.pe(mmu2, reads=WU + HT, writes=[bkUk])
                sgt, sgk = sg_r.next()
                p.act((lambda sgt, bkG: lambda e: e.activation(out=sgt, in_=bkG[:, 0:B], func=AF.Silu))(sgt, bkG), reads=[bkGk], writes=[sgk])
                p.dve((lambda aT, sgt, bkU, hc: lambda e: e.tensor_tensor(out=aT[:, hc, :], in0=bkU[:, 0:B], in1=sgt, op=ALU.mult))(aT, sgt, bkU, hc),
                      reads=[bkUk, sgk], writes=[(aTk, hc)])
            AT = [(aTk, hc) for hc in range(4)]
            for a in range(BT):
                ysb, ysbk = ysb_r.next()
                for half in range(2):
                    bk, bkk = nbank()

                    def mmd(e, bk=bk, aT=aT, wd_=wd_, a=a, half=half):
                        for hc in range(4):
                            ins = e.matmul(bk[:], lhsT=aT[:, hc, a * 128:(a + 1) * 128], rhs=wd_[:, hc, half * 512:(half + 1) * 512],
                                           start=(hc == 0), stop=(hc == 3))
                        return ins
                    p.pe(mmd, reads=AT + WD, writes=[bkk])
                    if half == 0:
                        p.act((lambda ysb, bk: lambda e: e.copy(out=ysb[:, 0:512], in_=bk[:]))(ysb, bk), reads=[bkk], writes=[(ysbk, 0)])
                    else:
                        p.dve((lambda ysb, bk: lambda e: e.tensor_copy(out=ysb[:, 512:1024], in_=bk[:]))(ysb, bk), reads=[bkk], writes=[(ysbk, 1)])
                p.dma("act", (lambda ysb, b, a: lambda e: e.dma_start(out=Y_v[b * BT + a], in_=ysb))(ysb, b, a),
                      reads=[(ysbk, 0), (ysbk, 1)], writes=[("Y", b, a)])
        YALL = [("Y", b, a) for b in range(NB) for a in range(BT)]

        p.barrier()
        arena.reset()
        fgbc = A([128, D], F32)
        y1_r = Rot("y1", [A([128, D], F32) for _ in range(2)])
        y2_r = Rot("y2", [A([128, D], F32) for _ in range(2)])
        xr_r = Rot("xr", [A([128, D], F32) for _ in range(2)])
        ob_r = Rot("ob", [A([128, D], F32) for _ in range(2)])
        sq4 = A([128, D], BF16)
        st4 = A([128, 2 * NT], F32)
        p.dma("sp", lambda e: e.dma_start(out=fgbc, in_=fg_d.partition_broadcast(128)), writes=["fgbc"])
        out_v = out_d.rearrange("(n p) c -> n p c", p=128)
        for i in range(NT):
            y1, y1k = y1_r.next()
            y2, y2k = y2_r.next()
            xr, xrk = xr_r.next()
            ob, obk = ob_r.next()
            p.dma("pool", (lambda y1, i: lambda e: e.indirect_dma_start(
                out=y1, out_offset=None, in_=Y_d, in_offset=bass.IndirectOffsetOnAxis(ap=desti[:, i, 0:1], axis=0)))(y1, i),
                reads=["desti"] + YALL, writes=[y1k])
            p.dma("pool", (lambda y2, i: lambda e: e.indirect_dma_start(
                out=y2, out_offset=None, in_=Y_d, in_offset=bass.IndirectOffsetOnAxis(ap=desti[:, i, 1:2], axis=0)))(y2, i),
                reads=["desti"] + YALL, writes=[y2k])
            p.dma("sp", (lambda xr, i: lambda e: e.dma_start(out=xr, in_=X1_v[i]))(xr, i), reads=[("X1", i)], writes=[xrk])
            p.dve((lambda y1, i: lambda e: e.tensor_scalar(out=y1, in0=y1, scalar1=gts[:, i, 0:1], scalar2=None, op0=ALU.mult))(y1, i),
                  reads=[y1k, ("gts", i // TPS * TPS, 0)], writes=[y1k])
            p.dve((lambda y1, y2, i: lambda e: e.scalar_tensor_tensor(out=y1, in0=y2, scalar=gts[:, i, 1:2], in1=y1, op0=ALU.mult, op1=ALU.add))(y1, y2, i),
                  reads=[y1k, y2k, ("gts", i // TPS * TPS, 1)], writes=[y1k])
            p.dve((lambda y1: lambda e: e.tensor_tensor(out=y1, in0=y1, in1=gate_f_bc, op=ALU.mult))(y1), reads=[y1k], writes=[y1k])
            p.dve((lambda y1, xr: lambda e: e.tensor_tensor(out=xr, in0=y1, in1=xr, op=ALU.add))(y1, xr), reads=[y1k, xrk], writes=[xrk])
            p.act((lambda xr, i: lambda e: e.activation(out=sq4, in_=xr, func=AF.Square, accum_out=st4[:, i:i + 1]))(xr, i),
                  reads=[xrk], writes=["sq4", ("st4a", i)])
            p.act((lambda i: lambda e: e.activation(out=st4[:, NT + i:NT + i + 1], in_=st4[:, i:i + 1], func=AF.Sqrt, scale=1.0 / D, bias=EPS))(i),
                  reads=[("st4a", i)], writes=[("st4b", i)])
            p.dve((lambda i: lambda e: e.reciprocal(out=st4[:, NT + i:NT + i + 1], in_=st4[:, NT + i:NT + i + 1]))(i),
                  reads=[("st4b", i)], writes=[("st4c", i)])
            p.dve((lambda ob, xr, i: lambda e: e.scalar_tensor_tensor(out=ob, in0=xr, scalar=st4[:, NT + i:NT + i + 1], in1=fgbc,
                                                                     op0=ALU.mult, op1=ALU.mult))(ob, xr, i),
                  reads=[xrk, ("st4c", i), "fgbc"], writes=[obk])
            p.dma("act", (lambda ob, i: lambda e: e.dma_start(out=out_v[i], in_=ob))(ob, i), reads=[obk], writes=[("out", i)])
        p.finish()
        p.emit(es)
        print("ops per engine", p.stats)
    return nc, dbg_out


def _host_consts():
    ident = np.eye(128, dtype=np.float32)
    tri = np.triu(np.ones((128, 128), np.float32))
    rowoff = (np.arange(8)[None, :] * 128 + np.arange(128)[:, None]).astype(np.float32)
    inv0 = np.zeros((4, 16), np.float32)
    for g in range(4):
        w = 2 ** (g + 1)
        inv0[g] = 1.0 / np.minimum(np.arange(16) + 1, w)
    iota = np.arange(32, dtype=np.float32)
    thr = (np.arange(JMAX) * B).astype(np.float32)
    bpos = (np.arange(NB) * B).astype(np.float32)
    return ident, tri, rowoff, inv0, iota, thr, bpos


_CACHE = {}


def _get_nc(debug=False):
    if debug not in _CACHE:
        _CACHE[debug] = build(debug)
    return _CACHE[debug]


def make_in_maps(inp):
    f = lambda a: np.ascontiguousarray(np.asarray(a, dtype=np.float32))
    ident, tri, rowoff, inv0, iota, thr, bpos = _host_consts()
    x = f(inp["x"]); c = f(inp["c"])
    colv = lambda v: v.reshape(-1, 128).T
    colp = np.concatenate([
        colv(f(inp["norm1_g"])[0]),
        f(inp["conv_w"])[0].T.reshape(4, 128, 4).transpose(1, 0, 2).reshape(128, 16),
        colv(f(inp["conv_b"])[0]), colv(f(inp["mlstm_skip"])[0]),
        colv(f(inp["b_pool"])[0]), colv(f(inp["pool_scale"])[0])], axis=1).astype(np.float32)
    rowp = np.concatenate([
        f(inp["mlstm_norm_g"])[0], f(inp["b_router_group"])[0], f(inp["b_router_expert"])[0],
        f(inp["b_igate"])[0], f(inp["b_fgate"])[0], inv0.reshape(-1), iota, thr, bpos])[None, :].astype(np.float32)
    w_r = np.concatenate([f(inp["w_router_group"])[0], f(inp["w_router_expert"])[0]], axis=1)
    shared = dict(
        ada_w=f(inp["ada_w"])[0], ada_b=f(inp["ada_b"]), w_in=f(inp["w_in"])[0], w_q=f(inp["w_q"])[0], w_k=f(inp["w_k"])[0],
        w_pool=f(inp["w_pool"])[0], w_out=f(inp["w_out"])[0], w_r=np.ascontiguousarray(w_r),
        w_gate=f(inp["w_expert_gate"])[0].reshape(NE * 128, 8 * 512), w_up=f(inp["w_expert_up"])[0].reshape(NE * 128, 8 * 512),
        w_down=f(inp["w_expert_down"])[0].reshape(NE * 128, 4 * D),
        colp=np.ascontiguousarray(colp), rowp=np.ascontiguousarray(rowp), norm2_g=f(inp["norm2_g"]),
        final_g=f(inp["final_g"]).reshape(1, D), ident=ident, tri=tri, rowoff=rowoff)
    maps = []
    for b in range(8):
        m = dict(shared)
        m["x"] = np.ascontiguousarray(x[b])
        m["ccol"] = np.ascontiguousarray(c[b].reshape(8, 128).T)
        maps.append(m)
    return maps


def kernel(**inputs):
    nc, _ = _get_nc(False)
    in_maps = make_in_maps(inputs)
    res = run_bass_kernel_spmd(nc, in_maps, core_ids=list(range(8)))
    out = np.stack([np.asarray(r["out"], dtype=np.float32) for r in res.results], axis=0)
    return out
```

```python
import numpy as np
from contextlib import ExitStack
import concourse.bass as bass
import concourse.mybir as mybir
from concourse.bass_utils import run_bass_kernel_spmd

F32 = mybir.dt.float32
BF16 = mybir.dt.bfloat16
I32 = mybir.dt.int32
AF = mybir.ActivationFunctionType
ALU = mybir.AluOpType
AX = mybir.AxisListType

T = 4096
D = 1024
NT = T // 128
SEG = 512
NSEG = T // SEG
TPS = SEG // 128
INC = 2056
COL_U, COL_V, COL_O, COL_I, COL_F, COL_P = 0, 512, 1024, 1536, 1540, 1544
NE = 32
B = 384
BT = B // 128
NB = (2 * T) // B + NE
NSLOT = NB * B
JMAX = (T + B - 1) // B + 1
EPS = 1e-6
BIG = 30000.0

ENGS = ("pe", "act", "dve", "pool", "sp")
DMAQ = ("sp", "act", "pool")
NDSEM = 8


class Op:
    __slots__ = ("eng", "fn", "reads", "writes", "dma", "deps", "sig", "sem", "val", "name")

    def __init__(self, eng, fn, reads, writes, dma, name):
        self.eng, self.fn, self.reads, self.writes, self.dma, self.name = eng, fn, reads, writes, dma, name
        self.deps = []
        self.sig = False
        self.sem = None
        self.val = 0


class Prog:
    def __init__(self, nc):
        self.nc = nc
        self.ops = []
        self.last_w = {}
        self.readers = {}

    def op(self, eng, fn, reads=(), writes=(), dma=False, name=""):
        o = Op(eng, fn, tuple(reads), tuple(writes), dma, name)
        deps = set()
        for r in o.reads:
            w = self.last_w.get(r)
            if w is not None:
                deps.add(w)
        for w_ in o.writes:
            w = self.last_w.get(w_)
            if w is not None:
                deps.add(w)
            for rd in self.readers.get(w_, ()):
                deps.add(rd)
        for d in deps:
            if d is o:
                continue
            if d.eng == o.eng and not d.dma and not o.dma:
                if o.eng == "pe":
                    continue
                if not any(self.last_w.get(r) is d for r in o.reads):
                    continue
            o.deps.append(d)
            d.sig = True
        for w_ in o.writes:
            self.last_w[w_] = o
            self.readers[w_] = []
        for r in o.reads:
            if r not in o.writes:
                self.readers.setdefault(r, []).append(o)
        self.ops.append(o)
        return o

    def pe(self, fn, reads=(), writes=(), name=""):
        return self.op("pe", fn, reads, writes, name=name)

    def act(self, fn, reads=(), writes=(), name=""):
        return self.op("act", fn, reads, writes, name=name)

    def dve(self, fn, reads=(), writes=(), name=""):
        return self.op("dve", fn, reads, writes, name=name)

    def pool(self, fn, reads=(), writes=(), name=""):
        return self.op("pool", fn, reads, writes, name=name)

    def ve(self, eng, fn, reads=(), writes=(), name=""):
        return self.op(eng, fn, reads, writes, name=name)

    def dma(self, q, fn, reads=(), writes=(), name=""):
        return self.op(q, fn, reads, writes, dma=True, name=name)

    def _sync_all(self, engines):
        last = {}
        dmas = {q: [] for q in DMAQ}
        for o in self.ops:
            if o.fn is None:
                continue
            if o.dma:
                dmas[o.eng].append(o)
            else:
                last[o.eng] = o
        deps = list(last.values())
        for q in DMAQ:
            deps += dmas[q][-NDSEM:]
        for e in engines:
            o = Op(e, None, (), (), False, "sync_all")
            for d in deps:
                if d.eng == e and not d.dma:
                    continue
                o.deps.append(d)
                d.sig = True
            self.ops.append(o)

    def barrier(self):
        self._sync_all(ENGS)

    def finish(self):
        self._sync_all(("sp",))

    def emit(self, es):
        nc = self.nc
        engsem = {e: es.enter_context(nc.semaphore("s_" + e)) for e in ENGS}
        dsem = {e: [es.enter_context(nc.semaphore(f"d_{e}{i}")) for i in range(NDSEM)] for e in DMAQ}
        cnt = {e: 0 for e in ENGS}
        hist = {e: [] for e in DMAQ}
        per_eng = {e: [] for e in ENGS}
        for o in self.ops:
            if o.dma:
                h = hist[o.eng]
                i = len(h)
                o.sem = dsem[o.eng][i % NDSEM]
                o.val = 16 * (i // NDSEM + 1)
                o.sig = True
                if i >= NDSEM and h[i - NDSEM] not in o.deps:
                    o.deps.append(h[i - NDSEM])
                h.append(o)
            elif o.sig and o.fn is not None:
                cnt[o.eng] += 1
                o.sem = engsem[o.eng]
                o.val = cnt[o.eng]
            per_eng[o.eng].append(o)
        block = es.enter_context(nc.Block())
        handles = {"pe": "tensor", "act": "scalar", "dve": "vector", "pool": "gpsimd", "sp": "sync"}

        def make(e):
            def body(eng):
                seen = {}
                for o in per_eng[e]:
                    for d in o.deps:
                        k = d.sem.name
                        if seen.get(k, 0) >= d.val:
                            continue
                        seen[k] = d.val
                        eng.wait_ge(d.sem, d.val)
                    if o.fn is None:
                        continue
                    ins = o.fn(eng)
                    if o.sig:
                        ins.then_inc(o.sem, 16 if o.dma else 1)
            return body

        for e in ENGS:
            getattr(block, handles[e])(make(e))
        self.stats = {e: len(per_eng[e]) for e in ENGS}


class Arena:
    def __init__(self, ap_bf16, nbytes):
        self.ap = ap_bf16
        self.nbytes = nbytes
        self.off = 0

    def reset(self):
        self.off = 0

    def alloc(self, shape, dt):
        esz = {F32: 4, BF16: 2, I32: 4}[dt]
        n = int(np.prod(shape[1:]))
        nb = (n * esz + 31) // 32 * 32
        assert self.off + nb <= self.nbytes, f"arena overflow {self.off + nb} > {self.nbytes}"
        v = self.ap[:, self.off // 2:(self.off + n * esz) // 2]
        self.off += nb
        if dt != BF16:
            v = v.bitcast(dt)
        if len(shape) == 3:
            v = v.rearrange("p (a b) -> p a b", b=shape[2])
        elif len(shape) == 4:
            v = v.rearrange("p (a b c) -> p a b c", b=shape[2], c=shape[3])
        return v


class Rot:
    def __init__(self, name, aps):
        self.name, self.aps, self.i = name, aps, 0

    def next(self):
        k = self.i % len(self.aps)
        self.i += 1
        return self.aps[k], (self.name, k)


def build(debug=False):
    nc = bass.Bass("TRN2", target_bir_lowering=False)
    dbg_out = {}

    def DT(name, shape, dt, kind="ExternalInput"):
        return nc.dram_tensor(name, list(shape), dt, kind=kind).ap()

    x_d = DT("x", [T, D], F32)
    ccol_d = DT("ccol", [128, 8], F32)
    adaw_d = DT("ada_w", [D, 6 * D], F32)
    adab_d = DT("ada_b", [1, 6 * D], F32)
    win_d = DT("w_in", [D, INC], F32)
    wq_d = DT("w_q", [4, 128, 128], F32)
    wk_d = DT("w_k", [4, 128, 128], F32)
    wpool_d = DT("w_pool", [4, 128, 128], F32)
    wout_d = DT("w_out", [D, D], F32)
    wr_d = DT("w_r", [D, 36], F32)
    wgate_d = DT("w_gate", [NE * 128, 8 * 512], F32)
    wup_d = DT("w_up", [NE * 128, 8 * 512], F32)
    wdown_d = DT("w_down", [NE * 128, 4 * D], F32)
    colp_d = DT("colp", [128, 40], F32)
    NROW = 512 + 36 + 8 + 64 + 32 + JMAX + NB
    rowp_d = DT("rowp", [1, NROW], F32)
    g2_d = DT("norm2_g", [1, D], F32)
    fg_d = DT("final_g", [1, D], F32)
    ident_d = DT("ident", [128, 128], F32)
    tri_d = DT("tri", [128, 128], F32)
    rowoff_d = DT("rowoff", [128, 8], F32)
    out_d = DT("out", [T, D], F32, "ExternalOutput")
    scr = "ExternalOutput"
    X1_d = DT("X1", [T, D], F32, scr)
    H2_d = DT("H2", [T, D], BF16, scr)
    Hs_d = DT("Hs", [NSLOT, D], BF16, "Internal")
    Y_d = DT("Y", [NSLOT, D], F32, "Internal")

    es = ExitStack()
    with es:
        def S(name, shape, dt):
            return es.enter_context(nc.sbuf_tensor("s_" + name, list(shape), dt))

        p = Prog(nc)
        banks = [es.enter_context(nc.psum_tensor(f"bank{i}", [128, 512], F32)) for i in range(8)]
        bank_i = [0]

        def nbank():
            k = bank_i[0] % 5
            bank_i[0] += 1
            return banks[k], ("bank", k)

        obank_i = [0]

        def obank():
            k = 5 + obank_i[0] % 2
            obank_i[0] += 1
            return banks[k], ("bank", k)

        ident = S("ident", [128, 128], F32)
        identb = S("identb", [128, 128], BF16)
        tri = S("tri", [128, 128], F32)
        trib = S("trib", [128, 128], BF16)
        striub = S("striub", [128, 128], BF16)
        onesf = S("onesf", [128, 128], F32)
        onesb = S("onesb", [128, 128], BF16)
        colp = S("colp", [128, 40], F32)
        rowb = S("rowb", [128, NROW], F32)
        rowoff = S("rowoff", [128, 8], F32)
        modB = S("modB", [128, 4 * D], F32)
        s2bc = S("s2bc", [128, D], F32)
        wi = S("wi", [128, 8, INC], BF16)
        wo = S("wo", [128, 8, D], BF16)
        wqk = S("wqk", [128, 2, 4, 128], BF16)
        wpl = S("wpl", [128, 4, 128], BF16)
        wr = S("wr", [128, 8, 36], BF16)
        biasbc = S("biasbc", [128, 520], F32)
        biascol = S("biascol", [128, 8], F32)
        bpscol = S("bpscol", [128, 4], F32)
        s1col = S("s1col", [128, 8], F32)
        Cst = S("Cst", [128, 4, 129], F32)
        ohs = S("ohs", [128, NT, 2, 32], BF16)
        ohsum = S("ohsum", [128, NT, 32], BF16)
        gts = S("gts", [128, NT, 2], F32)
        destf = S("destf", [128, NT, 2], F32)
        desti = S("desti", [128, NT, 2], I32)
        widx = S("widx", [128, NB], I32)
        widxc = S("widxc", [128, NB], I32)
        ztile = S("ztile", [128, D], BF16)
        ARENA_BYTES = 119808
        arena_t = S("arena", [128, ARENA_BYTES // 2], BF16)
        arena = Arena(arena_t, ARENA_BYTES)

        g1col = colp[:, 0:8]
        convw = colp[:, 8:24]
        convb = colp[:, 24:28]
        skipc = colp[:, 28:32]
        bpoolc = colp[:, 32:36]
        pscalec = colp[:, 36:40]
        r0 = 0
        normg_bc = rowb[:, r0:r0 + 512]; r0 += 512
        br_bc = rowb[:, r0:r0 + 36]; r0 += 36
        bif_bc = rowb[:, r0:r0 + 8]; r0 += 8
        inv0_bc = rowb[:, r0:r0 + 64]; r0 += 64
        iota_bc = rowb[:, r0:r0 + 32]; r0 += 32
        thr_bc = rowb[:, r0:r0 + JMAX]; r0 += JMAX
        bpos_bc = rowb[:, r0:r0 + NB]; r0 += NB
        gate_a_bc = modB[:, 0:D]
        shift_f_bc = modB[:, D:2 * D]
        scale_f_bc = modB[:, 2 * D:3 * D]
        gate_f_bc = modB[:, 3 * D:4 * D]

        def dump(name, ap, key, dt=F32):
            if not debug:
                return
            shp = list(ap.shape)
            t = DT("dbg_" + name, shp, dt, "ExternalOutput")
            dbg_out["dbg_" + name] = shp
            p.dma("sp", lambda e: e.dma_start(out=t, in_=ap), reads=[key], name="dump")

        p.dma("sp", lambda e: e.dma_start(out=ident[:], in_=ident_d), writes=["ident"])
        p.dma("sp", lambda e: e.dma_start(out=tri[:], in_=tri_d), writes=["tri"])
        p.dma("sp", lambda e: e.dma_start(out=colp[:], in_=colp_d), writes=["colp"])
        p.dma("sp", lambda e: e.dma_start(out=rowb[:], in_=rowp_d.partition_broadcast(128)), writes=["rowb"])
        p.dma("sp", lambda e: e.dma_start(out=rowoff[:], in_=rowoff_d), writes=["rowoff"])
        p.dve(lambda e: e.tensor_copy(out=identb[:], in_=ident[:]), reads=["ident"], writes=["identb"])
        p.dve(lambda e: e.tensor_copy(out=trib[:], in_=tri[:]), reads=["tri"], writes=["trib"])
        p.dve(lambda e: e.tensor_tensor(out=striub[:], in0=tri[:], in1=ident[:], op=ALU.subtract),
              reads=["tri", "ident"], writes=["striub"])
        p.dve(lambda e: e.memset(onesf[:], 1.0), writes=["onesf"])
        p.dve(lambda e: e.memset(onesb[:], 1.0), writes=["onesb"])
        p.dve(lambda e: e.memset(Cst[:], 0.0), writes=[("C", h) for h in range(4)])

        p.pool(lambda e: e.memset(ztile[:], 0.0), writes=["ztile"])
        hs_v = Hs_d.rearrange("(n a p) c -> n p a c", p=128, a=BT)
        nz = NB
        rem = 0
        zero_todo = list(range(nz))

        def pump_zero(n):
            for _ in range(n):
                if not zero_todo:
                    return
                nn = zero_todo.pop(0)
                for a_ in range(BT):
                    p.dma("pool", (lambda nn, a_: lambda e: e.dma_start(out=hs_v[nn][:, a_, :], in_=ztile[:]))(nn, a_),
                          reads=["ztile"], writes=[("Hs", "z", nn, a_)])

        modA = arena.alloc([128, 2 * D], F32)
        shift_a_bc = modA[:, 0:D]
        scale_a_bc = modA[:, D:2 * D]

        def modslice(j):
            return modA[:, j * 512:(j + 1) * 512] if j < 4 else modB[:, (j - 4) * 512:(j - 3) * 512]
        cc = arena.alloc([128, 8], F32)
        scb = arena.alloc([128, 8], BF16)
        screp = arena.alloc([128, 8, 128], BF16)
        p.dma("sp", lambda e: e.dma_start(out=cc, in_=ccol_d), writes=["cc"])
        p.act(lambda e: e.activation(out=scb, in_=cc, func=AF.Silu), reads=["cc"], writes=["scb"])
        p.dve(lambda e: e.tensor_copy(out=screp, in_=scb.unsqueeze(2).to_broadcast([128, 8, 128])),
              reads=["scb"], writes=["screp"])
        wa_r = Rot("wa", [arena.alloc([128, 8, 512], BF16) for _ in range(2)])
        ab_r = Rot("ab", [arena.alloc([128, 512], F32) for _ in range(2)])
        adaw_v = adaw_d.rearrange("(k p) n -> p k n", p=128)
        for j in range(12):
            wa, wak = wa_r.next()
            ab, abk = ab_r.next()
            p.dma("pool", (lambda wa, j: lambda e: e.dma_start(out=wa, in_=adaw_v[:, :, j * 512:(j + 1) * 512]))(wa, j),
                  writes=[wak])
            p.dma("sp", (lambda ab, j: lambda e: e.dma_start(
                out=ab, in_=adab_d[0:1, j * 512:(j + 1) * 512].partition_broadcast(128)))(ab, j), writes=[abk])
            bk, bkk = nbank()

            def mm(e, wa=wa, bk=bk):
                for k in range(8):
                    ins = e.matmul(bk[:], lhsT=screp[:, k, :], rhs=wa[:, k, :], start=(k == 0), stop=(k == 7))
                return ins
            p.pe(mm, reads=["screp", wak], writes=[bkk])
            p.dve((lambda ab, bk, j: lambda e: e.tensor_tensor(out=modslice(j), in0=bk[:], in1=ab,
                                                               op=ALU.add))(ab, bk, j),
                  reads=[bkk, abk], writes=[("mod", j)])
        MODALL = [("mod", j) for j in range(12)]

        win_v = win_d.rearrange("(k p) n -> p k n", p=128)
        for k in range(8):
            p.dma("pool", (lambda k: lambda e: e.dma_start(out=wi[:, k, :], in_=win_v[:, k, :]))(k), writes=[("wi", k)])
        WIALL = [("wi", k) for k in range(8)]
        wout_v = wout_d.rearrange("(k p) n -> p k n", p=128)
        for k in range(0, 8, 2):
            p.dma("pool", (lambda k: lambda e: e.dma_start(out=wo[:, k:k + 2, :], in_=wout_v[:, k:k + 2, :]))(k),
                  writes=[("wo", k)])
        WOALL = [("wo", k) for k in range(0, 8, 2)]
        WOSC = True
        p.dma("pool", lambda e: e.dma_start(out=wqk[:, 0, :, :], in_=wq_d.rearrange("h d e -> d h e")), writes=["wq"])
        p.dma("pool", lambda e: e.dma_start(out=wqk[:, 1, :, :], in_=wk_d.rearrange("h d e -> d h e")), writes=["wk"])
        p.dma("pool", lambda e: e.dma_start(out=wpl[:], in_=wpool_d.rearrange("g c d -> c g d")), writes=["wpl"])
        p.dma("pool", lambda e: e.dma_start(out=wr[:], in_=wr_d.rearrange("(k p) n -> p k n", p=128)), writes=["wr"])

        tmpA = arena.alloc([128, D], F32)
        tmp3 = arena.alloc([128, 8, 128], F32)
        scl = arena.alloc([128, 8], F32)
        shc = arena.alloc([128, 8], F32)
        shcb = arena.alloc([128, 8], BF16)
        shrep = arena.alloc([128, 8, 128], BF16)
        idb3 = ident[:].unsqueeze(1).to_broadcast([128, 8, 128])
        p.dve(lambda e: e.tensor_tensor(out=tmp3, in0=scale_a_bc.rearrange("p (a b) -> p a b", b=128), in1=idb3, op=ALU.mult),
              reads=MODALL + ["ident"], writes=["tmp3"])
        p.dve(lambda e: e.reduce_sum(out=scl, in_=tmp3, axis=AX.X), reads=["tmp3"], writes=["scl"])
        p.dve(lambda e: e.scalar_tensor_tensor(out=s1col[:], in0=scl, scalar=1.0, in1=g1col, op0=ALU.add, op1=ALU.mult),
              reads=["scl", "colp"], writes=["s1col"])
        p.dve(lambda e: e.tensor_tensor(out=tmp3, in0=shift_a_bc.rearrange("p (a b) -> p a b", b=128), in1=idb3, op=ALU.mult),
              reads=MODALL + ["ident", "scl"], writes=["tmp3"])
        p.dve(lambda e: e.reduce_sum(out=shc, in_=tmp3, axis=AX.X), reads=["tmp3"], writes=["shc"])
        p.dve(lambda e: e.tensor_copy(out=shcb, in_=shc), reads=["shc"], writes=["shcb"])
        p.dve(lambda e: e.tensor_copy(out=shrep, in_=shcb.unsqueeze(2).to_broadcast([128, 8, 128])),
              reads=["shcb"], writes=["shrep"])
        bk, bkk = nbank()

        def mmbv(e, bk=bk):
            for k in range(8):
                ins = e.matmul(bk[:], lhsT=shrep[:, k, :], rhs=wi[:, k, COL_V:COL_V + 512], start=(k == 0), stop=(k == 7))
            return ins
        p.pe(mmbv, reads=["shrep"] + WIALL, writes=[bkk])
        p.dve((lambda bk: lambda e: e.tensor_copy(out=biasbc[:, 0:512], in_=bk[:]))(bk), reads=[bkk], writes=["biasbc_v"])
        bk, bkk = nbank()

        def mmbg(e, bk=bk):
            for k in range(8):
                ins = e.matmul(bk[:, 0:8], lhsT=shrep[:, k, :], rhs=wi[:, k, COL_I:COL_I + 8], start=(k == 0), stop=(k == 7))
            return ins
        p.pe(mmbg, reads=["shrep"] + WIALL, writes=[bkk])
        p.dve((lambda bk: lambda e: e.tensor_tensor(out=biasbc[:, 512:520], in0=bk[:, 0:8], in1=bif_bc, op=ALU.add))(bk),
              reads=[bkk, "rowb"], writes=["biasbc_g"])
        bk, bkk = nbank()

        def mmbc(e, bk=bk):
            for c in range(8):
                c0 = (COL_U if c < 4 else COL_O) + (c % 4) * 128
                for k in range(8):
                    ins = e.matmul(bk[:, c:c + 1], lhsT=wi[:, k, c0:c0 + 128], rhs=shcb[:, k:k + 1], start=(k == 0), stop=(k == 7))
            return ins
        p.pe(mmbc, reads=["shcb"] + WIALL, writes=[bkk])
        p.dve((lambda bk: lambda e: e.tensor_copy(out=biascol[:], in_=bk[:, 0:8]))(bk), reads=[bkk], writes=["biascol"])
        for k in range(8):
            p.dve((lambda k: lambda e: e.tensor_scalar(out=wi[:, k, :], in0=wi[:, k, :], scalar1=s1col[:, k:k + 1], scalar2=None,
                                                       op0=ALU.mult))(k),
                  reads=["s1col", ("wi", k)], writes=[("wi", k)])
        p.dma("sp", lambda e: e.dma_start(out=tmpA, in_=g2_d.partition_broadcast(128)), writes=["tmpA"])
        p.dve(lambda e: e.scalar_tensor_tensor(out=s2bc[:], in0=scale_f_bc, scalar=1.0, in1=tmpA, op0=ALU.add, op1=ALU.mult),
              reads=MODALL + ["tmpA"], writes=["s2bc"])
        p.dve(lambda e: e.tensor_tensor(out=bpscol[:], in0=bpoolc, in1=pscalec, op=ALU.mult), reads=["colp"], writes=["bpscol"])
        for k in range(0, 8, 2):
            for kk in (k, k + 1):
                p.dve((lambda kk: lambda e: e.tensor_tensor(out=wo[:, kk, :], in0=wo[:, kk, :], in1=gate_a_bc, op=ALU.mult))(kk),
                      reads=[("wo", k)] + MODALL, writes=[("wo", k)])
        dump("modB", modB[:], ("mod", 11))
        dump("biasbc", biasbc[:], "biasbc_g")
        dump("biascol", biascol[:], "biascol")

        p.barrier()
        arena.reset()
        A = arena.alloc
        xs_r = Rot("xs", [A([128, D], F32) for _ in range(2)])
        xb_r = Rot("xb", [A([128, D], BF16) for _ in range(2)])
        sqj = A([128, D], BF16)
        xT = A([128, 8, SEG], BF16)
        R1 = A([128, SEG], F32)
        ssq1 = A([128, TPS], F32)
        rstd1 = A([128, TPS], F32)
        diag_r = Rot("diag", [A([128, 128], F32) for _ in range(2)])
        ubuf = A([128, 4, SEG + 3], F32)
        scrA = A([128, 2 * SEG], F32)
        ctmp_r = Rot("ctmp", [scrA[:, 0:SEG], scrA[:, SEG:2 * SEG]])
        CT2 = [("ctmp", 0), ("ctmp", 1)]
        ucT = A([128, 4, SEG], BF16)
        sigoT2 = [A([128, 4, SEG], BF16) for _ in range(2)]
        scrB = A([128, 2 * SEG], F32)
        otmp_r = Rot("otmp", [scrB[:, 0:SEG], scrB[:, SEG:2 * SEG]])
        OT2 = [("otmp", 0), ("otmp", 1)]
        pbuf = A([128, 4, SEG + 16], F32)
        ptmp = [A([128, SEG + 16], F32) for _ in range(2)]
        pooled = A([128, 4, SEG], BF16)
        p16 = A([128, 16], F32)
        yT = A([128, 8, SEG], BF16)
        vaug2 = [A([128, TPS, 4, 129], BF16) for _ in range(2)]
        gat = A([128, TPS, 8], F32)
        gsx = A([128, 8, TPS * 4], F32)
        gtmp = A([128, TPS, 4], F32)
        qT = A([128, 4, SEG], BF16)
        kT = A([128, 4, SEG], BF16)
        ktok = A([128, TPS, 4, 128], BF16)
        wv_r = Rot("wv", [A([128, 129], BF16) for _ in range(4)])
        dst_r = Rot("dst", [A([128, 128], BF16) for _ in range(4)])
        cb_r = Rot("cb", [A([128, 129], BF16) for _ in range(4)])
        sm_r = Rot("sm", [A([128, 8], F32) for _ in range(4)])
        hn_r = Rot("hn", [A([128, 512], BF16) for _ in range(2)])
        ytmp_r = Rot("otmp", [scrB[:, 0:SEG].rearrange("p (a b) -> p a b", b=128), scrB[:, SEG:2 * SEG].rearrange("p (a b) -> p a b", b=128)])
        prod_r = Rot("prod", [A([128, 512], F32) for _ in range(2)])
        h2_r = Rot("h2", [A([128, D], BF16) for _ in range(2)])
        h2T_r = Rot("h2T", [A([128, 8, 128], BF16) for _ in range(1)])
        st2 = A([128, 8], F32)
        rt_r = Rot("rt", [A([128, 768], F32) for _ in range(1)])
        print("phase1 arena used", arena.off)

        p.dve(lambda e: e.memset(vaug2[0], 1.0), writes=[("vaug", 0, j) for j in range(TPS)])
        p.dve(lambda e: e.memset(vaug2[1], 1.0), writes=[("vaug", 1, j) for j in range(TPS)])
        p.dve(lambda e: e.memset(ubuf[:, :, 0:3], 0.0), writes=[("u", c, "halo") for c in range(4)])
        p.dve(lambda e: e.memset(pbuf[:, :, 0:16], 0.0), writes=[("p", c, "halo") for c in range(4)])

        x_v = x_d.rearrange("(n p) c -> n p c", p=128)
        X1_v = X1_d.rearrange("(n p) c -> n p c", p=128)
        H2_v = H2_d.rearrange("(n p) c -> n p c", p=128)
        QCS, QTOT, QINVRS, QG, QWS, QAC, QSQA, QTMP = range(8)

        def stF(sg):
            par = sg % 2
            vaug = vaug2[par]
            sigoT = sigoT2[par]
            for j in range(TPS):
                ti = sg * TPS + j
                xs, xsk = xs_r.next()
                xb, xbk = xb_r.next()
                p.dma("sp", (lambda xs, ti: lambda e: e.dma_start(out=xs, in_=x_v[ti]))(xs, ti), writes=[xsk])
                p.act((lambda xs, j: lambda e: e.activation(out=sqj, in_=xs, func=AF.Square, accum_out=ssq1[:, j:j + 1]))(xs, j),
                      reads=[xsk], writes=["sqj", ("ssq1", j)])
                p.act((lambda j: lambda e: e.activation(out=rstd1[:, j:j + 1], in_=ssq1[:, j:j + 1], func=AF.Sqrt, scale=1.0 / D, bias=EPS))(j),
                      reads=[("ssq1", j)], writes=[("std1", j)])
                p.dve((lambda j: lambda e: e.reciprocal(out=rstd1[:, j:j + 1], in_=rstd1[:, j:j + 1]))(j), reads=[("std1", j)], writes=[("rstd1", j)])
                p.dve((lambda xs, xb, j: lambda e: e.tensor_scalar(out=xb, in0=xs, scalar1=rstd1[:, j:j + 1], scalar2=None, op0=ALU.mult))(xs, xb, j),
                      reads=[xsk, ("rstd1", j)], writes=[xbk])
                bk, bkk = nbank()
                bkb = bk[:].bitcast(BF16).rearrange("p (a b) -> p a b", b=128)

                def tr(e, xb=xb, bkb=bkb):
                    for k in range(8):
                        ins = e.transpose(out=bkb[:, k, :], in_=xb[:, k * 128:(k + 1) * 128], identity=identb[:])
                    return ins
                p.pe(tr, reads=[xbk, "identb"], writes=[bkk])
                p.act((lambda bkb, j: lambda e: e.copy(out=xT[:, :, j * 128:(j + 1) * 128], in_=bkb))(bkb, j),
                      reads=[bkk], writes=[("xT", j)])
                yield
            XTALL = [("xT", j) for j in range(TPS)]

            for grp, col0 in (("U", COL_U), ("O", COL_O), ("P", COL_P)):
                for c in range(4):
                    bk, bkk = nbank()
                    c0 = col0 + c * 128

                    def mm(e, bk=bk, c0=c0):
                        for k in range(8):
                            ins = e.matmul(bk[:], lhsT=wi[:, k, c0:c0 + 128], rhs=xT[:, k, :], start=(k == 0), stop=(k == 7))
                        return ins
                    p.pe(mm, reads=WIALL + XTALL, writes=[bkk])
                    if grp == "U":
                        p.act((lambda bk, c: lambda e: e.activation(out=ubuf[:, c, 3:SEG + 3], in_=bk[:], func=AF.Identity,
                                                                    bias=biascol[:, c:c + 1]))(bk, c),
                              reads=[bkk, "biascol"], writes=[("u", c, "body")])
                    elif grp == "O":
                        p.act((lambda bk, c: lambda e: e.activation(out=sigoT[:, c, :], in_=bk[:], func=AF.Sigmoid,
                                                                    bias=biascol[:, 4 + c:5 + c]))(bk, c),
                              reads=[bkk, "biascol"], writes=[("sigo", par, c)])
                    else:
                        p.dve((lambda bk, c: lambda e: e.tensor_copy(out=pbuf[:, c, 16:SEG + 16], in_=bk[:]))(bk, c),
                              reads=[bkk], writes=[("p", c, "body")])
                    yield
            for j in range(TPS):
                bk, bkk = nbank()

                def mmv(e, bk=bk, j=j):
                    for k in range(8):
                        ins = e.matmul(bk[:], lhsT=xT[:, k, j * 128:(j + 1) * 128], rhs=wi[:, k, COL_V:COL_V + 512],
                                       start=(k == 0), stop=(k == 7))
                    return ins
                p.pe(mmv, reads=WIALL + XTALL, writes=[bkk])
                p.dve((lambda bk, j: lambda e: e.tensor_tensor(
                    out=vaug[:, j, :, 0:128], in0=bk[:].rearrange("p (a b) -> p a b", b=128),
                    in1=biasbc[:, 0:512].rearrange("p (a b) -> p a b", b=128), op=ALU.add))(bk, j),
                    reads=[bkk, "biasbc_v"], writes=[("vaug", par, j)])
                bk, bkk = nbank()

                def mmg(e, bk=bk, j=j):
                    for k in range(8):
                        ins = e.matmul(bk[:, 0:8], lhsT=xT[:, k, j * 128:(j + 1) * 128], rhs=wi[:, k, COL_I:COL_I + 8],
                                       start=(k == 0), stop=(k == 7))
                    return ins
                p.pe(mmg, reads=WIALL + XTALL, writes=[bkk])
                p.dve((lambda bk, j: lambda e: e.tensor_tensor(out=gat[:, j, :], in0=bk[:, 0:8], in1=biasbc[:, 512:520], op=ALU.add))(bk, j),
                      reads=[bkk, "biasbc_g"], writes=[("gat", j)])
                yield

        def stM(sg):
            GATALL = [("gat", j) for j in range(TPS)]

            for c in range(4):
                eng = "dve"
                ct, ctk = ctmp_r.next()
                UR = [("u", c, "halo"), ("u", c, "body")]
                p.ve(eng, (lambda ct, c: lambda e: e.tensor_scalar(out=ct, in0=ubuf[:, c, 0:SEG], scalar1=convw[:, c * 4:c * 4 + 1],
                                                                    scalar2=None, op0=ALU.mult))(ct, c),
                     reads=UR + ["colp"], writes=[ctk])
                for k in range(1, 4):
                    p.ve(eng, (lambda ct, c, k: lambda e: e.scalar_tensor_tensor(
                        out=ct, in0=ubuf[:, c, k:k + SEG], scalar=convw[:, c * 4 + k:c * 4 + k + 1], in1=ct,
                        op0=ALU.mult, op1=ALU.add))(ct, c, k), reads=UR + ["colp", ctk], writes=[ctk])
                p.act((lambda ct, c: lambda e: e.activation(out=ucT[:, c, :], in_=ct, func=AF.Silu, bias=convb[:, c:c + 1]))(ct, c),
                      reads=[ctk, "colp"], writes=[("ucT", c)])
                p.ve(eng, (lambda c: lambda e: e.tensor_copy(out=ubuf[:, c, 0:3], in_=ubuf[:, c, SEG:SEG + 3]))(c),
                     reads=[("u", c, "body")], writes=[("u", c, "halo")])
            for g in range(4):
                eng = "pool" if g % 2 == 0 else "dve"
                PR = [("p", g, "halo"), ("p", g, "body")]
                W = SEG + 16
                src = pbuf[:, g, :]
                srck = PR
                sh = 1
                for lvl in range(g + 1):
                    dstt = ptmp[lvl % 2]
                    dk = ("ptmp", lvl % 2)
                    lo = 2 * sh
                    p.ve(eng, (lambda dstt, src, sh, lo: lambda e: e.tensor_tensor(
                        out=dstt[:, lo:W], in0=src[:, lo:W], in1=src[:, lo - sh:W - sh], op=ALU.add))(dstt, src, sh, lo),
                        reads=list(srck), writes=[dk])
                    src, srck, sh = dstt, [dk], sh * 2
                wg_ = float(2 ** (g + 1))
                p.ve("dve", (lambda src, g, wg_: lambda e: e.scalar_tensor_tensor(
                    out=pooled[:, g, :], in0=src[:, 16:W], scalar=1.0 / wg_, in1=pbuf[:, g, 16:W],
                    op0=ALU.mult, op1=ALU.subtract))(src, g, wg_), reads=list(srck) + PR, writes=[("pooled", g)])
                if sg == 0:
                    p.ve(eng, (lambda src, g: lambda e: e.tensor_tensor(out=p16, in0=src[:, 16:32], in1=inv0_bc[:, g * 16:(g + 1) * 16],
                                                                        op=ALU.mult))(src, g),
                         reads=list(srck) + ["rowb"], writes=["p16"])
                    p.ve(eng, (lambda g: lambda e: e.tensor_tensor(out=pooled[:, g, 0:16], in0=p16, in1=pbuf[:, g, 16:32],
                                                                   op=ALU.subtract))(g),
                         reads=["p16"] + PR, writes=[("pooled", g)])
                p.ve(eng, (lambda g: lambda e: e.tensor_copy(out=pbuf[:, g, 0:16], in_=pbuf[:, g, SEG:SEG + 16]))(g),
                     reads=[("p", g, "body")], writes=[("p", g, "halo")])
                bk, bkk = nbank()
                p.pe((lambda bk, g: lambda e: e.matmul(bk[:], lhsT=wpl[:, g, :], rhs=pooled[:, g, :], start=True, stop=True))(bk, g),
                     reads=["wpl", ("pooled", g)], writes=[bkk])
                p.act((lambda bk, g: lambda e: e.activation(out=yT[:, 4 + g, :], in_=bk[:], func=AF.Identity,
                                                            scale=pscalec[:, g:g + 1], bias=bpscol[:, g:g + 1]))(bk, g),
                      reads=[bkk, "colp", "bpscol"], writes=[("yT", 4 + g)])

            gi = gat[:, :, 0:4]
            gf = gat[:, :, 4:8]
            q = lambda n: gsx[:, n, :].rearrange("p (a b) -> p a b", b=4)
            p.act(lambda e: e.activation(out=gtmp, in_=gf, func=AF.Exp, scale=-1.0), reads=GATALL, writes=["gtmp"])
            p.act(lambda e: e.activation(out=gtmp, in_=gtmp, func=AF.Ln, bias=1.0), reads=["gtmp"], writes=["gtmp"])
            bk, bkk = nbank()

            def mmcs(e, bk=bk):
                for j in range(TPS):
                    e.matmul(bk[:, j * 4:(j + 1) * 4], lhsT=tri[:], rhs=gtmp[:, j, :], start=True, stop=True)
                for j in range(TPS):
                    ins = e.matmul(bk[:, 64 + j * 4:64 + (j + 1) * 4], lhsT=onesf[:], rhs=gtmp[:, j, :], start=True, stop=True)
                return ins
            p.pe(mmcs, reads=["gtmp", "tri", "onesf"], writes=[bkk])
            p.dve((lambda bk: lambda e: e.tensor_copy(out=gsx[:, QCS, :], in_=bk[:, 0:TPS * 4]))(bk), reads=[bkk], writes=["q_cs"])
            p.dve((lambda bk: lambda e: e.tensor_copy(out=gsx[:, QTOT, :], in_=bk[:, 64:64 + TPS * 4]))(bk), reads=[bkk], writes=["q_tot"])
            p.dve(lambda e: e.scalar_tensor_tensor(out=gsx[:, QTMP, :], in0=gsx[:, QTOT, :], scalar=-0.5, in1=gsx[:, QCS, :],
                                                   op0=ALU.mult, op1=ALU.add), reads=["q_cs", "q_tot"], writes=["q_tmp"])
            p.act(lambda e: e.activation(out=gsx[:, QINVRS, :], in_=gsx[:, QTMP, :], func=AF.Exp), reads=["q_tmp"], writes=["q_invrs"])
            p.dve(lambda e: e.tensor_tensor(out=q(QG), in0=q(QTMP), in1=gi, op=ALU.add), reads=["q_tmp"] + GATALL, writes=["q_g"])
            p.act(lambda e: e.activation(out=gsx[:, QG, :], in_=gsx[:, QG, :], func=AF.Exp), reads=["q_g"], writes=["q_g"])
            p.dve(lambda e: e.tensor_tensor(out=gsx[:, QWS, :], in0=gsx[:, QCS, :], in1=gsx[:, QTOT, :], op=ALU.subtract),
                  reads=["q_cs", "q_tot"], writes=["q_ws"])
            p.dve(lambda e: e.tensor_tensor(out=q(QWS), in0=q(QWS), in1=gi, op=ALU.add), reads=["q_ws"] + GATALL, writes=["q_ws"])
            p.act(lambda e: e.activation(out=gsx[:, QWS, :], in_=gsx[:, QWS, :], func=AF.Exp), reads=["q_ws"], writes=["q_ws"])
            p.act(lambda e: e.activation(out=gsx[:, QAC, :], in_=gsx[:, QTOT, :], func=AF.Exp, scale=-1.0), reads=["q_tot"], writes=["q_ac"])
            p.act(lambda e: e.activation(out=gsx[:, QSQA, :], in_=gsx[:, QTOT, :], func=AF.Exp, scale=-0.5), reads=["q_tot"], writes=["q_sqa"])

            for h in range(4):
                for wsel, dstT, sc, nm in ((0, qT, 128.0 ** -0.5, "qT"), (1, kT, 1.0, "kT")):
                    bk, bkk = nbank()
                    p.pe((lambda bk, h, wsel: lambda e: e.matmul(bk[:], lhsT=wqk[:, wsel, h, :], rhs=ucT[:, h, :], start=True, stop=True))(bk, h, wsel),
                         reads=["wq", "wk", ("ucT", h)], writes=[bkk])
                    p.act((lambda bk, h, dstT, sc: lambda e: e.activation(out=dstT[:, h, :], in_=bk[:], func=AF.Copy, scale=sc))(bk, h, dstT, sc),
                          reads=[bkk], writes=[(nm, h)])
            for j in range(TPS):
                bk, bkk = nbank()

                def mmk(e, bk=bk, j=j):
                    for h in range(4):
                        ins = e.matmul(bk[:, h * 128:(h + 1) * 128], lhsT=ucT[:, h, j * 128:(j + 1) * 128], rhs=wqk[:, 1, h, :],
                                       start=True, stop=True)
                    return ins
                p.pe(mmk, reads=["wk"] + [("ucT", h) for h in range(4)], writes=[bkk])
                p.dve((lambda bk, j: lambda e: e.tensor_copy(out=ktok[:, j, :, :], in_=bk[:].rearrange("p (a b) -> p a b", b=128)))(bk, j),
                      reads=[bkk], writes=[("ktok", j)])

        def stK(sg, pump, after_tile):
            par = sg % 2
            vaug = vaug2[par]
            sigoT = sigoT2[par]
            ctxs = {}
            hns = {}

            def S1(n):
                j, h = divmod(n, 4)
                jh = n
                if h == 0:
                    hns[j] = hn_r.next()
                c = {}
                eng2 = "pool" if h % 2 == 0 else "dve"
                wv, wvk = wv_r.next()
                p.ve(eng2, (lambda wv, j, h, jh: lambda e: e.tensor_scalar(out=wv, in0=vaug[:, j, h, :], scalar1=gsx[:, QWS, jh:jh + 1],
                                                                          scalar2=None, op0=ALU.mult))(wv, j, h, jh),
                     reads=[("vaug", par, j), "q_ws"], writes=[wvk])
                bkA, bkAk = nbank()
                p.pe((lambda bkA, wv, j, h: lambda e: e.matmul(bkA[:, 0:129], lhsT=ktok[:, j, h, :], rhs=wv, start=True, stop=True))(bkA, wv, j, h),
                     reads=[("ktok", j), wvk], writes=[bkAk])
                p.pe((lambda bkA, j, h: lambda e: e.matmul(bkA[:, 256:384], lhsT=kT[:, h, j * 128:(j + 1) * 128],
                                                           rhs=qT[:, h, j * 128:(j + 1) * 128], start=True, stop=True))(bkA, j, h),
                     reads=[("kT", h), ("qT", h)], writes=[bkAk])
                ds, dsk = dst_r.next()
                p.dve((lambda ds, bkA, jh: lambda e: e.scalar_tensor_tensor(out=ds, in0=bkA[:, 256:384], scalar=gsx[:, QG, jh:jh + 1],
                                                                           in1=trib[:], op0=ALU.mult, op1=ALU.mult))(ds, bkA, jh),
                      reads=[bkAk, "q_g", "trib"], writes=[dsk])
                cb, cbk = cb_r.next()
                p.ve(eng2, (lambda cb, h, jh: lambda e: e.tensor_scalar(out=cb, in0=Cst[:, h, :], scalar1=gsx[:, QSQA, jh:jh + 1],
                                                                       scalar2=None, op0=ALU.mult))(cb, h, jh),
                     reads=[("C", h), "q_sqa"], writes=[cbk])
                p.dve((lambda bkA, h, jh: lambda e: e.scalar_tensor_tensor(out=Cst[:, h, :], in0=Cst[:, h, :], scalar=gsx[:, QAC, jh:jh + 1],
                                                                          in1=bkA[:, 0:129], op0=ALU.mult, op1=ALU.add))(bkA, h, jh),
                      reads=[("C", h), "q_ac", bkAk], writes=[("C", h)])
                c.update(ds=ds, dsk=dsk, cb=cb, cbk=cbk)
                ctxs[n] = c

            def S2(n):
                j, h = divmod(n, 4)
                jh = n
                c = ctxs[n]
                ds, dsk, cb, cbk = c["ds"], c["dsk"], c["cb"], c["cbk"]
                bkO, bkOk = obank()

                def mmo(e, bkO=bkO, ds=ds, cb=cb, j=j, h=h):
                    e.matmul(bkO[:, 0:129], lhsT=ds, rhs=vaug[:, j, h, :], start=True, stop=False)
                    return e.matmul(bkO[:, 0:129], lhsT=qT[:, h, j * 128:(j + 1) * 128], rhs=cb, start=False, stop=True)
                p.pe(mmo, reads=[dsk, ("vaug", par, j), ("qT", h), cbk], writes=[bkOk])
                sm, smk = sm_r.next()
                p.act((lambda sm, bkO: lambda e: e.activation(out=sm[:, 0:1], in_=bkO[:, 128:129], func=AF.Abs))(sm, bkO),
                      reads=[bkOk], writes=[(smk, 0)])
                p.dve((lambda sm, jh: lambda e: e.tensor_tensor(out=sm[:, 1:2], in0=sm[:, 0:1], in1=gsx[:, QINVRS, jh:jh + 1], op=ALU.max))(sm, jh),
                      reads=[(smk, 0), "q_invrs"], writes=[(smk, 1)])
                p.dve((lambda sm: lambda e: e.reciprocal(out=sm[:, 2:3], in_=sm[:, 1:2]))(sm), reads=[(smk, 1)], writes=[(smk, 2)])
                p.act((lambda sm, bkO: lambda e: e.activation(out=sqj[:, 0:128], in_=bkO[:, 0:128], func=AF.Square, scale=sm[:, 2:3],
                                                              accum_out=sm[:, 3:4]))(sm, bkO),
                      reads=[bkOk, (smk, 2)], writes=["sqj", (smk, 3)])
                p.act((lambda sm: lambda e: e.activation(out=sm[:, 4:5], in_=sm[:, 3:4], func=AF.Sqrt, scale=1.0 / 128, bias=EPS))(sm),
                      reads=[(smk, 3)], writes=[(smk, 4)])
                c.update(bkO=bkO, bkOk=bkOk, sm=sm, smk=smk)

            def S3(n):
                j, h = divmod(n, 4)
                c = ctxs[n]
                bkO, bkOk, sm, smk = c["bkO"], c["bkOk"], c["sm"], c["smk"]
                hn, hnk = hns[j]
                p.dve((lambda sm: lambda e: e.reciprocal(out=sm[:, 5:6], in_=sm[:, 4:5]))(sm), reads=[(smk, 4)], writes=[(smk, 5)])
                p.dve((lambda sm: lambda e: e.tensor_tensor(out=sm[:, 6:7], in0=sm[:, 5:6], in1=sm[:, 2:3], op=ALU.mult))(sm),
                      reads=[(smk, 5), (smk, 2)], writes=[(smk, 6)])
                p.dve((lambda sm, bkO, hn, h: lambda e: e.scalar_tensor_tensor(
                    out=hn[:, h * 128:(h + 1) * 128], in0=bkO[:, 0:128], scalar=sm[:, 6:7], in1=normg_bc[:, h * 128:(h + 1) * 128],
                    op0=ALU.mult, op1=ALU.mult))(sm, bkO, hn, h), reads=[bkOk, (smk, 6), "rowb"], writes=[(hnk, h)])
                if h == 3:
                    bk, bkk = nbank()
                    bkb = bk[:].bitcast(BF16).rearrange("p (a b) -> p a b", b=128)

                    def trh(e, hn=hn, bkb=bkb):
                        for hh in range(4):
                            ins = e.transpose(out=bkb[:, hh, :], in_=hn[:, hh * 128:(hh + 1) * 128], identity=identb[:])
                        return ins
                    p.pe(trh, reads=[(hnk, hh) for hh in range(4)] + ["identb"], writes=[bkk])
                    yt, ytk = ytmp_r.next()
                    for hh in range(4):
                        p.dve((lambda yt, bkb, j, hh: lambda e: e.scalar_tensor_tensor(
                            out=yt[:, hh, :], in0=ucT[:, hh, j * 128:(j + 1) * 128], scalar=skipc[:, hh:hh + 1], in1=bkb[:, hh, :],
                            op0=ALU.mult, op1=ALU.add))(yt, bkb, j, hh),
                            reads=[bkk, ("ucT", hh), "colp"], writes=[ytk])
                    p.pool((lambda yt, j: lambda e: e.tensor_tensor(out=yT[:, 0:4, j * 128:(j + 1) * 128], in0=yt, in1=sigoT[:, :, j * 128:(j + 1) * 128],
                                                                    op=ALU.mult))(yt, j),
                           reads=[ytk] + [("sigo", par, cc_) for cc_ in range(4)], writes=[("yTm", j)])
                    after_tile(j)

            NIT = TPS * 4
            for step in range(NIT + 2):
                if step < NIT:
                    S1(step)
                if 0 <= step - 1 < NIT:
                    S2(step - 1)
                if 0 <= step - 2 < NIT:
                    S3(step - 2)
                pump(2 if step % 2 == 0 else 1)

        def stE(sg):
            YTALL = [("yT", 4 + g) for g in range(4)] + [("yTm", j) for j in range(TPS)]

            ectx = {}

            def EW(j):
                ti = sg * TPS + j
                xs, xsk = xs_r.next()
                p.dma("sp", (lambda xs, ti: lambda e: e.dma_start(out=xs, in_=x_v[ti]))(xs, ti), writes=[xsk])
                x1, x1k = xs, xsk
                X1K = [xsk]
                for half in range(2):
                    bk, bkk = nbank()

                    def mmw(e, bk=bk, j=j, half=half):
                        for k in range(8):
                            ins = e.matmul(bk[:], lhsT=yT[:, k, j * 128:(j + 1) * 128], rhs=wo[:, k, half * 512:(half + 1) * 512],
                                           start=(k == 0), stop=(k == 7))
                        return ins
                    p.pe(mmw, reads=[("yT", 4 + g) for g in range(4)] + [("yTm", j)] + WOALL, writes=[bkk])
                    p.dve((lambda bk, x1, half: lambda e: e.tensor_tensor(out=x1[:, half * 512:(half + 1) * 512], in0=bk[:],
                                                                         in1=x1[:, half * 512:(half + 1) * 512], op=ALU.add))(bk, x1, half),
                          reads=[bkk, xsk], writes=[xsk])
                ectx[j] = (x1, x1k, X1K, ti)

            def EP(j):
                x1, x1k, X1K, ti = ectx[j]
                p.dma("act", (lambda x1, ti: lambda e: e.dma_start(out=X1_v[ti], in_=x1))(x1, ti), reads=X1K, writes=[("X1", ti)])
                p.act((lambda x1, j: lambda e: e.activation(out=sqj, in_=x1, func=AF.Square, accum_out=st2[:, j:j + 1]))(x1, j),
                      reads=X1K, writes=["sqj", ("st2a", j)])
                p.act((lambda j: lambda e: e.activation(out=st2[:, 4 + j:5 + j], in_=st2[:, j:j + 1], func=AF.Sqrt, scale=1.0 / D, bias=EPS))(j),
                      reads=[("st2a", j)], writes=[("st2b", j)])
                p.dve((lambda j: lambda e: e.reciprocal(out=st2[:, 4 + j:5 + j], in_=st2[:, 4 + j:5 + j]))(j), reads=[("st2b", j)], writes=[("st2c", j)])
                h2f, h2fk = scrA, "h2f"
                h2, h2k = h2_r.next()
                p.dve((lambda h2f, x1, j: lambda e: e.scalar_tensor_tensor(out=h2f, in0=x1, scalar=st2[:, 4 + j:5 + j], in1=s2bc[:],
                                                                          op0=ALU.mult, op1=ALU.mult))(h2f, x1, j),
                      reads=X1K + [("st2c", j), "s2bc"], writes=[h2fk] + CT2)
                p.pool((lambda h2, h2f: lambda e: e.tensor_tensor(out=h2, in0=h2f, in1=shift_f_bc, op=ALU.add))(h2, h2f),
                       reads=[h2fk] + CT2 + MODALL, writes=[h2k])
                p.dma("act", (lambda h2, ti: lambda e: e.dma_start(out=H2_v[ti], in_=h2))(h2, ti), reads=[h2k], writes=[("H2", ti)])
                bk, bkk = nbank()
                bkb = bk[:].bitcast(BF16).rearrange("p (a b) -> p a b", b=128)

                def tr2(e, h2=h2, bkb=bkb):
                    for k in range(8):
                        ins = e.transpose(out=bkb[:, k, :], in_=h2[:, k * 128:(k + 1) * 128], identity=identb[:])
                    return ins
                p.pe(tr2, reads=[h2k, "identb"], writes=[bkk])
                h2T, h2Tk = h2T_r.next()
                p.act((lambda h2T, bkb: lambda e: e.copy(out=h2T, in_=bkb))(h2T, bkb), reads=[bkk], writes=[h2Tk])

                def mmr(e, j=j, h2T=h2T):
                    for k in range(8):
                        ins = e.matmul(banks[7][:, j * 36:(j + 1) * 36], lhsT=h2T[:, k, :], rhs=wr[:, k, :], start=(k == 0), stop=(k == 7))
                    return ins
                p.pe(mmr, reads=[h2Tk, "wr"], writes=[("rbank", j)])
                if j < TPS - 1:
                    return
                t0 = sg * TPS
                rt, rtk = rt_r.next()
                RB = [("rbank", jj) for jj in range(TPS)]
                v3 = lambda a, n_: rt[:, a:a + TPS * n_].rearrange("p (t n) -> p t n", n=n_)
                lg = v3(0, 36)
                elm = v3(144, 32)
                elm2 = v3(272, 32)
                oh1f = v3(400, 32)
                oh2f = v3(528, 32)
                ohg = v3(656, 4)
                pen = v3(672, 4)
                egj = v3(688, 4)
                s4 = lambda a: rt[:, 704 + a * 4:708 + a * 4]
                R = lambda *a: [(rtk, x) for x in a]
                bc3 = lambda ap2, n_: ap2.unsqueeze(2).to_broadcast([128, TPS, n_])
                p.dve(lambda e: e.tensor_tensor(out=lg, in0=banks[7][:, 0:TPS * 36].rearrange("p (t n) -> p t n", n=36),
                                                in1=br_bc.unsqueeze(1).to_broadcast([128, TPS, 36]), op=ALU.add),
                      reads=RB + ["rowb"], writes=R("lg"))
                p.dve(lambda e: e.reduce_max(out=s4(0), in_=lg[:, :, 0:4], axis=AX.X), reads=R("lg"), writes=R("gmax"))
                p.dve(lambda e: e.tensor_tensor(out=ohg, in0=lg[:, :, 0:4], in1=bc3(s4(0), 4), op=ALU.is_equal), reads=R("lg", "gmax"), writes=R("ohg"))
                p.dve(lambda e: e.tensor_tensor(out=egj, in0=lg[:, :, 0:4], in1=bc3(s4(0), 4), op=ALU.subtract), reads=R("lg", "gmax"), writes=R("egj"))
                p.act(lambda e: e.activation(out=egj, in_=egj, func=AF.Exp), reads=R("egj"), writes=R("egj"))
                p.dve(lambda e: e.reduce_sum(out=s4(1), in_=egj, axis=AX.X), reads=R("egj"), writes=R("sumg"))
                p.dve(lambda e: e.reciprocal(out=s4(2), in_=s4(1)), reads=R("sumg"), writes=R("ggate"))
                p.dve(lambda e: e.tensor_scalar(out=pen, in0=ohg, scalar1=-1.0, scalar2=BIG, op0=ALU.add, op1=ALU.mult), reads=R("ohg"), writes=R("pen"))
                p.dve(lambda e: e.tensor_tensor(out=elm.rearrange("p t (a b) -> p t a b", b=8),
                                                in0=lg[:, :, 4:36].rearrange("p t (a b) -> p t a b", b=8),
                                                in1=pen.unsqueeze(3).to_broadcast([128, TPS, 4, 8]), op=ALU.add),
                      reads=R("lg", "pen"), writes=R("elm"))
                p.dve(lambda e: e.reduce_max(out=s4(3), in_=elm, axis=AX.X), reads=R("elm"), writes=R("m1"))
                p.dve(lambda e: e.tensor_tensor(out=oh1f, in0=elm, in1=bc3(s4(3), 32), op=ALU.is_equal), reads=R("elm", "m1"), writes=R("oh1"))
                p.dve(lambda e: e.scalar_tensor_tensor(out=elm2, in0=oh1f, scalar=-BIG, in1=elm, op0=ALU.mult, op1=ALU.add),
                      reads=R("elm", "oh1"), writes=R("elm2"))
                p.dve(lambda e: e.reduce_max(out=s4(4), in_=elm2, axis=AX.X), reads=R("elm2"), writes=R("m2"))
                p.dve(lambda e: e.tensor_tensor(out=oh2f, in0=elm2, in1=bc3(s4(4), 32), op=ALU.is_equal), reads=R("elm2", "m2"), writes=R("oh2"))
                p.dve(lambda e: e.tensor_tensor(out=s4(5), in0=s4(3), in1=s4(4), op=ALU.subtract), reads=R("m1", "m2"), writes=R("d12"))
                p.act(lambda e: e.activation(out=s4(6), in_=s4(5), func=AF.Sigmoid), reads=R("d12"), writes=R("p1"))
                p.act(lambda e: e.activation(out=s4(7), in_=s4(5), func=AF.Sigmoid, scale=-1.0), reads=R("d12"), writes=R("p2"))
                p.dve(lambda e: e.tensor_tensor(out=gts[:, t0:t0 + TPS, 0], in0=s4(6), in1=s4(2), op=ALU.mult), reads=R("p1", "ggate"), writes=[("gts", t0, 0)])
                p.dve(lambda e: e.tensor_tensor(out=gts[:, t0:t0 + TPS, 1], in0=s4(7), in1=s4(2), op=ALU.mult), reads=R("p2", "ggate"), writes=[("gts", t0, 1)])
                p.dve(lambda e: e.tensor_copy(out=ohs[:, t0:t0 + TPS, 0, :], in_=oh1f), reads=R("oh1"), writes=[("ohs", t0, 0)])
                p.dve(lambda e: e.tensor_copy(out=ohs[:, t0:t0 + TPS, 1, :], in_=oh2f), reads=R("oh2"), writes=[("ohs", t0, 1)])
                p.dve(lambda e: e.tensor_tensor(out=ohsum[:, t0:t0 + TPS, :], in0=oh1f, in1=oh2f, op=ALU.add), reads=R("oh1", "oh2"), writes=[("ohsum", t0)])
            return EW, EP

        for _ in stF(0):
            pass
        for sg in range(NSEG):
            stM(sg)
            gen = stF(sg + 1) if sg + 1 < NSEG else iter(())

            def pump(n, gen=gen):
                for _ in range(n):
                    next(gen, None)
            EW, EP = stE(sg)

            def after_tile(j, EW=EW, EP=EP):
                EW(j)
                if j >= 1:
                    EP(j - 1)
            stK(sg, pump, after_tile)
            EP(TPS - 1)
            for _ in gen:
                pass
            pump_zero((NB + NSEG - 1) // NSEG)

        pump_zero(NB)
        p.barrier()
        arena.reset()
        cnt = A([128, 32], F32)
        cmpJ = A([128, 32, JMAX], F32)
        nbk = A([128, 32], F32)
        cs_a = A([128, 32], F32)
        cs_b = A([128, 32], F32)
        pstart = A([128, 32], F32)
        cmpB = A([128, NB, 32], F32)
        bef = A([128, NB], F32)
        widxf = A([128, NB, 4], F32)
        gidxf = A([128, NB], F32)
        pos_r = Rot("pos", [A([128, 96], F32) for _ in range(2)])
        hsb_r = Rot("hsb", [A([128, D], BF16) for _ in range(3)])
        OHSUM = [("ohsum", i) for i in range(0, NT, TPS)]
        bk, bkk = nbank()

        def mmcnt(e, bk=bk):
            for i in range(NT):
                ins = e.matmul(bk[:, 0:32], lhsT=onesb[:], rhs=ohsum[:, i, :], start=(i == 0), stop=(i == NT - 1))
            return ins
        p.pe(mmcnt, reads=OHSUM + ["onesb"], writes=[bkk])
        p.dve((lambda bk: lambda e: e.tensor_copy(out=cnt, in_=bk[:, 0:32]))(bk), reads=[bkk], writes=["cnt"])
        p.dve(lambda e: e.tensor_tensor(out=cmpJ, in0=cnt.unsqueeze(2).to_broadcast([128, 32, JMAX]),
                                        in1=thr_bc.unsqueeze(1).to_broadcast([128, 32, JMAX]), op=ALU.is_gt),
              reads=["cnt", "rowb"], writes=["cmpJ"])
        p.dve(lambda e: e.reduce_sum(out=nbk, in_=cmpJ, axis=AX.X), reads=["cmpJ"], writes=["nbk"])
        p.dve(lambda e: e.tensor_scalar(out=nbk, in0=nbk, scalar1=float(B), scalar2=None, op0=ALU.mult), reads=["nbk"], writes=["padded"])
        src, srck = nbk, "padded"
        bufs = [(cs_a, "cs_a"), (cs_b, "cs_b")]
        for li, sh in enumerate((1, 2, 4, 8, 16)):
            dstt, dk = bufs[li % 2]
            p.dve((lambda dstt, src, sh: lambda e: e.tensor_copy(out=dstt[:, 0:sh], in_=src[:, 0:sh]))(dstt, src, sh), reads=[srck], writes=[(dk, 0)])
            p.dve((lambda dstt, src, sh: lambda e: e.tensor_tensor(out=dstt[:, sh:32], in0=src[:, sh:32], in1=src[:, 0:32 - sh], op=ALU.add))(dstt, src, sh),
                  reads=[srck], writes=[(dk, 1)])
            p.dve((lambda dstt: lambda e: e.tensor_copy(out=dstt[:, 0:1], in_=dstt[:, 0:1]))(dstt), reads=[(dk, 0), (dk, 1)], writes=[dk])
            src, srck = dstt, dk
        pend, pendk = src, srck
        p.dve(lambda e: e.tensor_tensor(out=pstart, in0=pend, in1=nbk, op=ALU.subtract), reads=[pendk, "padded"], writes=["pstart"])
        p.dve(lambda e: e.tensor_tensor(out=cmpB, in0=pend.unsqueeze(1).to_broadcast([128, NB, 32]),
                                        in1=bpos_bc.unsqueeze(2).to_broadcast([128, NB, 32]), op=ALU.is_le),
              reads=[pendk, "rowb"], writes=["cmpB"])
        p.dve(lambda e: e.reduce_sum(out=bef, in_=cmpB, axis=AX.X), reads=["cmpB"], writes=["bef0"])
        p.dve(lambda e: e.tensor_scalar(out=gidxf, in0=bef, scalar1=128.0, scalar2=None, op0=ALU.mult), reads=["bef0"], writes=["gidxf0"])
        p.dve(lambda e: e.tensor_tensor(out=gidxf, in0=gidxf, in1=rowoff[:, 0:1].to_broadcast([128, NB]), op=ALU.add),
              reads=["gidxf0", "rowoff"], writes=["gidxf"])
        p.dve(lambda e: e.tensor_copy(out=widx[:], in_=gidxf), reads=["gidxf"], writes=["widx"])
        p.dve(lambda e: e.tensor_scalar(out=gidxf, in0=gidxf, scalar1=float(NE * 128 - 1), scalar2=None, op0=ALU.min),
              reads=["gidxf", "widx"], writes=["gidxc"])
        p.dve(lambda e: e.tensor_copy(out=widxc[:], in_=gidxf), reads=["gidxc"], writes=["widxc"])
        wg_r = Rot("wg", [A([128, 8, 512], BF16) for _ in range(2)])
        wu_r = Rot("wu", [A([128, 8, 512], BF16) for _ in range(2)])
        wd_r = Rot("wd", [A([128, 4, D], BF16) for _ in range(2)])
        NSKIP = 15

        def gather_kw(b):
            if b >= NB - NSKIP:
                return dict(in_offset=bass.IndirectOffsetOnAxis(ap=widx[:, b:b + 1], axis=0), bounds_check=NE * 128 - 1, oob_is_err=False)
            return dict(in_offset=bass.IndirectOffsetOnAxis(ap=widxc[:, b:b + 1], axis=0))
        order = []
        lo, hi = 0, NB - 1
        while lo <= hi:
            order.append(lo); lo += 1
            if lo <= hi:
                order.append(lo); lo += 1
            if lo <= hi and hi >= NB - NSKIP:
                order.append(hi); hi -= 1
        assert sorted(order) == list(range(NB))
        nxt = {order[i]: (order[i + 2] if i + 2 < NB else None) for i in range(NB)}
        wbufs = {}

        def issue_gathers(b):
            wg_, wgk = wg_r.next()
            wu_, wuk = wu_r.next()
            wd_, wdk = wd_r.next()
            p.dma("pool", (lambda wg_, b: lambda e: e.indirect_dma_start(
                out=wg_.rearrange("p a b -> p (a b)"), out_offset=None, in_=wgate_d,
                **gather_kw(b)))(wg_, b),
                reads=["widx", "widxc"], writes=[(wgk, k) for k in range(8)])
            p.dma("pool", (lambda wu_, b: lambda e: e.indirect_dma_start(
                out=wu_.rearrange("p a b -> p (a b)"), out_offset=None, in_=wup_d,
                **gather_kw(b)))(wu_, b),
                reads=["widx", "widxc"], writes=[(wuk, k) for k in range(8)])
            p.dma("pool", (lambda wd_, b: lambda e: e.indirect_dma_start(
                out=wd_.rearrange("p a b -> p (a b)"), out_offset=None, in_=wdown_d,
                **gather_kw(b)))(wd_, b),
                reads=["widx", "widxc"], writes=[(wdk, k) for k in range(4)])
            wbufs[b] = (wg_, wgk, wu_, wuk, wd_, wdk)
        issue_gathers(order[0])
        issue_gathers(order[1])
        for i in range(NT):
            bk, bkk = nbank()

            def mmrk(e, bk=bk, i=i):
                ins = e.matmul(bk[:, 0:32], lhsT=striub[:], rhs=ohsum[:, i, :], start=True, stop=(i == 0))
                for j2 in range(i):
                    ins = e.matmul(bk[:, 0:32], lhsT=onesb[:], rhs=ohsum[:, j2, :], start=False, stop=(j2 == i - 1))
                return ins
            p.pe(mmrk, reads=OHSUM + ["onesb", "striub"], writes=[bkk])
            ps, psk = pos_r.next()
            p.dve((lambda bk, ps: lambda e: e.tensor_tensor(out=ps[:, 0:32], in0=bk[:, 0:32], in1=pstart, op=ALU.add))(bk, ps),
                  reads=[bkk, "pstart"], writes=[(psk, 0)])
            p.dve((lambda ps, i: lambda e: e.tensor_tensor(out=ps[:, 32:96].rearrange("p (a b) -> p a b", b=32), in0=ohs[:, i, :, :],
                                                           in1=ps[:, 0:32].unsqueeze(1).to_broadcast([128, 2, 32]), op=ALU.mult))(ps, i),
                  reads=[(psk, 0), ("ohs", i // TPS * TPS, 0), ("ohs", i // TPS * TPS, 1)], writes=[(psk, 1)])
            p.dve((lambda ps, i: lambda e: e.reduce_sum(out=destf[:, i, :], in_=ps[:, 32:96].rearrange("p (a b) -> p a b", b=32), axis=AX.X))(ps, i),
                  reads=[(psk, 1)], writes=[("destf", i)])
        p.dve(lambda e: e.tensor_copy(out=desti[:], in_=destf[:]), reads=[("destf", i) for i in range(NT)], writes=["desti"])
        dump("destf", destf[:], "desti")
        HSZ = [("Hs", "z", n, a_) for n in range(nz) for a_ in range(BT)]
        for i in range(NT):
            hb, hbk = hsb_r.next()
            p.dma("sp", (lambda hb, i: lambda e: e.dma_start(out=hb, in_=H2_v[i]))(hb, i), reads=[("H2", i)], writes=[hbk])
            for k in range(2):
                p.dma("pool", (lambda hb, i, k: lambda e: e.indirect_dma_start(
                    out=Hs_d, out_offset=bass.IndirectOffsetOnAxis(ap=desti[:, i, k:k + 1], axis=0), in_=hb, in_offset=None))(hb, i, k),
                    reads=[hbk, "desti"] + HSZ, writes=[("Hs", "s", i, k)])
        HSALL = [("Hs", "s", i, k) for i in range(NT) for k in range(2)]

        hsl_r = Rot("hsl", [A([128, BT, D], BF16) for _ in range(2)])
        hT_r = Rot("hT", [A([128, 8, B], BF16) for _ in range(2)])
        aT_r = Rot("aT", [A([128, 4, B], BF16) for _ in range(2)])
        sg_r = Rot("sg", [A([128, B], F32) for _ in range(2)])
        ysb_r = Rot("ysb", [A([128, D], F32) for _ in range(3)])
        print("phase3 arena used", arena.off)
        Hs_b = Hs_d.rearrange("(n a p) c -> n p a c", p=128, a=BT)
        Y_v = Y_d.rearrange("(n p) c -> n p c", p=128)
        hsl_bufs = {}

        def load_hsl(b):
            hsl, hslk = hsl_r.next()
            p.dma("sp", (lambda hsl, b: lambda e: e.dma_start(out=hsl, in_=Hs_b[b]))(hsl, b), reads=HSALL + HSZ, writes=[hslk])
            hsl_bufs[b] = (hsl, hslk)
        load_hsl(order[0])
        load_hsl(order[1])
        for b in order:
            if b not in wbufs:
                issue_gathers(b)
            wg_, wgk, wu_, wuk, wd_, wdk = wbufs[b]
            WG = [(wgk, k) for k in range(8)]
            WU = [(wuk, k) for k in range(8)]
            WD = [(wdk, k) for k in range(4)]
            hsl, hslk = hsl_bufs[b]
            hT, hTk = hT_r.next()
            for a in range(BT):
                bk, bkk = nbank()
                bkb = bk[:].bitcast(BF16).rearrange("p (a b) -> p a b", b=128)

                def tr3(e, hsl=hsl, bkb=bkb, a=a):
                    for k in range(8):
                        ins = e.transpose(out=bkb[:, k, :], in_=hsl[:, a, :].rearrange("s (p k) -> s k p", k=8)[:, k, :], identity=identb[:])
                    return ins
                p.pe(tr3, reads=[hslk, "identb"], writes=[bkk])
                eng = "act" if a % 2 == 0 else "dve"
                if eng == "act":
                    p.act((lambda hT, bkb, a: lambda e: e.copy(out=hT[:, :, a * 128:(a + 1) * 128], in_=bkb))(hT, bkb, a),
                          reads=[bkk], writes=[(hTk, a)])
                else:
                    p.dve((lambda hT, bkb, a: lambda e: e.tensor_copy(out=hT[:, :, a * 128:(a + 1) * 128], in_=bkb))(hT, bkb, a),
                          reads=[bkk], writes=[(hTk, a)])
            HT = [(hTk, a) for a in range(BT)]
            if nxt[b] is not None:
                load_hsl(nxt[b])
            aT, aTk = aT_r.next()
            for hc in range(4):
                bkG, bkGk = nbank()
                bkU, bkUk = nbank()

                def mmg2(e, bkG=bkG, wg_=wg_, hT=hT, hc=hc):
                    for k in range(8):
                        ins = e.matmul(bkG[:, 0:B], lhsT=wg_[:, k, :].rearrange("d (p c) -> d c p", c=4)[:, hc, :], rhs=hT[:, k, :], start=(k == 0), stop=(k == 7))
                    return ins
                p.pe(mmg2, reads=WG + HT, writes=[bkGk])

                def mmu2(e, bkU=bkU, wu_=wu_, hT=hT, hc=hc):
                    for k in range(8):
                        ins = e.matmul(bkU[:, 0:B], lhsT=wu_[:, k, :].rearrange("d (p c) -> d c p", c=4)[:, hc, :], rhs=hT[:, k, :], start=(k == 0), stop=(k == 7))
                    return ins
                p.pe(mmu2, reads=WU + HT, writes=[bkUk])
                sgt, sgk = sg_r.next()
                p.act((lambda sgt, bkG: lambda e: e.activation(out=sgt, in_=bkG[:, 0:B], func=AF.Silu))(sgt, bkG), reads=[bkGk], writes=[sgk])
                p.dve((lambda aT, sgt, bkU, hc: lambda e: e.tensor_tensor(out=aT[:, hc, :], in0=bkU[:, 0:B], in1=sgt, op=ALU.mult))(aT, sgt, bkU, hc),
                      reads=[bkUk, sgk], writes=[(aTk, hc)])
            AT = [(aTk, hc) for hc in range(4)]
            for a in range(BT):
                ysb, ysbk = ysb_r.next()
                for half in range(2):
                    bk, bkk = nbank()

                    def mmd(e, bk=bk, aT=aT, wd_=wd_, a=a, half=half):
                        for hc in range(4):
                            ins = e.matmul(bk[:], lhsT=aT[:, hc, a * 128:(a + 1) * 128], rhs=wd_[:, hc, half * 512:(half + 1) * 512],
                                           start=(hc == 0), stop=(hc == 3))
                        return ins
                    p.pe(mmd, reads=AT + WD, writes=[bkk])
                    if half == 0:
                        p.act((lambda ysb, bk: lambda e: e.copy(out=ysb[:, 0:512], in_=bk[:]))(ysb, bk), reads=[bkk], writes=[(ysbk, 0)])
                    else:
                        p.dve((lambda ysb, bk: lambda e: e.tensor_copy(out=ysb[:, 512:1024], in_=bk[:]))(ysb, bk), reads=[bkk], writes=[(ysbk, 1)])
                p.dma("act", (lambda ysb, b, a: lambda e: e.dma_start(out=Y_v[b * BT + a], in_=ysb))(ysb, b, a),
                      reads=[(ysbk, 0), (ysbk, 1)], writes=[("Y", b, a)])
        YALL = [("Y", b, a) for b in range(NB) for a in range(BT)]

        p.barrier()
        arena.reset()
        fgbc = A([128, D], F32)
        y1_r = Rot("y1", [A([128, D], F32) for _ in range(4)])
        y2_r = Rot("y2", [A([128, D], F32) for _ in range(4)])
        xr_r = Rot("xr", [A([128, D], F32) for _ in range(4)])
        ob_r = Rot("ob", [A([128, D], F32) for _ in range(3)])
        sq4 = A([128, D], BF16)
        st4 = A([128, 2 * NT], F32)
        p.dma("sp", lambda e: e.dma_start(out=fgbc, in_=fg_d.partition_broadcast(128)), writes=["fgbc"])
        out_v = out_d.rearrange("(n p) c -> n p c", p=128)
        for i in range(NT):
            y1, y1k = y1_r.next()
            y2, y2k = y2_r.next()
            xr, xrk = xr_r.next()
            ob, obk = ob_r.next()
            p.dma("pool", (lambda y1, i: lambda e: e.indirect_dma_start(
                out=y1, out_offset=None, in_=Y_d, in_offset=bass.IndirectOffsetOnAxis(ap=desti[:, i, 0:1], axis=0)))(y1, i),
                reads=["desti"] + YALL, writes=[y1k])
            p.dma("pool", (lambda y2, i: lambda e: e.indirect_dma_start(
                out=y2, out_offset=None, in_=Y_d, in_offset=bass.IndirectOffsetOnAxis(ap=desti[:, i, 1:2], axis=0)))(y2, i),
                reads=["desti"] + YALL, writes=[y2k])
            p.dma("sp", (lambda xr, i: lambda e: e.dma_start(out=xr, in_=X1_v[i]))(xr, i), reads=[("X1", i)], writes=[xrk])
            p.dve((lambda y1, i: lambda e: e.tensor_scalar(out=y1, in0=y1, scalar1=gts[:, i, 0:1], scalar2=None, op0=ALU.mult))(y1, i),
                  reads=[y1k, ("gts", i // TPS * TPS, 0)], writes=[y1k])
            p.dve((lambda y1, y2, i: lambda e: e.scalar_tensor_tensor(out=y1, in0=y2, scalar=gts[:, i, 1:2], in1=y1, op0=ALU.mult, op1=ALU.add))(y1, y2, i),
                  reads=[y1k, y2k, ("gts", i // TPS * TPS, 1)], writes=[y1k])
            p.dve((lambda y1: lambda e: e.tensor_tensor(out=y1, in0=y1, in1=gate_f_bc, op=ALU.mult))(y1), reads=[y1k], writes=[y1k])
            p.dve((lambda y1, xr: lambda e: e.tensor_tensor(out=xr, in0=y1, in1=xr, op=ALU.add))(y1, xr), reads=[y1k, xrk], writes=[xrk])
            p.act((lambda xr, i: lambda e: e.activation(out=sq4, in_=xr, func=AF.Square, accum_out=st4[:, i:i + 1]))(xr, i),
                  reads=[xrk], writes=["sq4", ("st4a", i)])
            p.act((lambda i: lambda e: e.activation(out=st4[:, NT + i:NT + i + 1], in_=st4[:, i:i + 1], func=AF.Sqrt, scale=1.0 / D, bias=EPS))(i),
                  reads=[("st4a", i)], writes=[("st4b", i)])
            p.dve((lambda i: lambda e: e.reciprocal(out=st4[:, NT + i:NT + i + 1], in_=st4[:, NT + i:NT + i + 1]))(i),
                  reads=[("st4b", i)], writes=[("st4c", i)])
            p.dve((lambda ob, xr, i: lambda e: e.scalar_tensor_tensor(out=ob, in0=xr, scalar=st4[:, NT + i:NT + i + 1], in1=fgbc,
                                                                     op0=ALU.mult, op1=ALU.mult))(ob, xr, i),
                  reads=[xrk, ("st4c", i), "fgbc"], writes=[obk])
            p.dma("act", (lambda ob, i: lambda e: e.dma_start(out=out_v[i], in_=ob))(ob, i), reads=[obk], writes=[("out", i)])
        p.finish()
        p.emit(es)
        print("ops per engine", p.stats)
    return nc, dbg_out


def _host_consts():
    ident = np.eye(128, dtype=np.float32)
    tri = np.triu(np.ones((128, 128), np.float32))
    rowoff = (np.arange(8)[None, :] * 128 + np.arange(128)[:, None]).astype(np.float32)
    inv0 = np.zeros((4, 16), np.float32)
    for g in range(4):
        w = 2 ** (g + 1)
        inv0[g] = 1.0 / np.minimum(np.arange(16) + 1, w)
    iota = np.arange(32, dtype=np.float32)
    thr = (np.arange(JMAX) * B).astype(np.float32)
    bpos = (np.arange(NB) * B).astype(np.float32)
    return ident, tri, rowoff, inv0, iota, thr, bpos


_CACHE = {}


def _get_nc(debug=False):
    if debug not in _CACHE:
        _CACHE[debug] = build(debug)
    return _CACHE[debug]


def make_in_maps(inp):
    f = lambda a: np.ascontiguousarray(np.asarray(a, dtype=np.float32))
    ident, tri, rowoff, inv0, iota, thr, bpos = _host_consts()
    x = f(inp["x"]); c = f(inp["c"])
    colv = lambda v: v.reshape(-1, 128).T
    colp = np.concatenate([
        colv(f(inp["norm1_g"])[0]),
        f(inp["conv_w"])[0].T.reshape(4, 128, 4).transpose(1, 0, 2).reshape(128, 16),
        colv(f(inp["conv_b"])[0]), colv(f(inp["mlstm_skip"])[0]),
        colv(f(inp["b_pool"])[0]), colv(f(inp["pool_scale"])[0])], axis=1).astype(np.float32)
    rowp = np.concatenate([
        f(inp["mlstm_norm_g"])[0], f(inp["b_router_group"])[0], f(inp["b_router_expert"])[0],
        f(inp["b_igate"])[0], f(inp["b_fgate"])[0], inv0.reshape(-1), iota, thr, bpos])[None, :].astype(np.float32)
    w_r = np.concatenate([f(inp["w_router_group"])[0], f(inp["w_router_expert"])[0]], axis=1)
    shared = dict(
        ada_w=f(inp["ada_w"])[0], ada_b=f(inp["ada_b"]), w_in=f(inp["w_in"])[0], w_q=f(inp["w_q"])[0], w_k=f(inp["w_k"])[0],
        w_pool=f(inp["w_pool"])[0], w_out=f(inp["w_out"])[0], w_r=np.ascontiguousarray(w_r),
        w_gate=f(inp["w_expert_gate"])[0].reshape(NE * 128, 8 * 512), w_up=f(inp["w_expert_up"])[0].reshape(NE * 128, 8 * 512),
        w_down=f(inp["w_expert_down"])[0].reshape(NE * 128, 4 * D),
        colp=np.ascontiguousarray(colp), rowp=np.ascontiguousarray(rowp), norm2_g=f(inp["norm2_g"]),
        final_g=f(inp["final_g"]).reshape(1, D), ident=ident, tri=tri, rowoff=rowoff)
    maps = []
    for b in range(8):
        m = dict(shared)
        m["x"] = np.ascontiguousarray(x[b])
        m["ccol"] = np.ascontiguousarray(c[b].reshape(8, 128).T)
        maps.append(m)
    return maps


def kernel(**inputs):
    nc, _ = _get_nc(False)
    in_maps = make_in_maps(inputs)
    res = run_bass_kernel_spmd(nc, in_maps, core_ids=list(range(8)))
    out = np.stack([np.asarray(r["out"], dtype=np.float32) for r in res.results], axis=0)
    return out
```

```python
import numpy as np
from contextlib import ExitStack
import concourse.bass as bass
import concourse.mybir as mybir
from concourse.bass_utils import run_bass_kernel_spmd

F32 = mybir.dt.float32
BF16 = mybir.dt.bfloat16
I32 = mybir.dt.int32
AF = mybir.ActivationFunctionType
ALU = mybir.AluOpType
AX = mybir.AxisListType

T = 4096
D = 1024
NT = T // 128
SEG = 512
NSEG = T // SEG
TPS = SEG // 128
INC = 2056
COL_U, COL_V, COL_O, COL_I, COL_F, COL_P = 0, 512, 1024, 1536, 1540, 1544
NE = 32
B = 384
BT = B // 128
NB = (2 * T) // B + NE
NSLOT = NB * B
JMAX = (T + B - 1) // B + 1
EPS = 1e-6
BIG = 30000.0

ENGS = ("pe", "act", "dve", "pool", "sp")
DMAQ = ("sp", "act", "pool")
NDSEM = 12


class Op:
    __slots__ = ("eng", "fn", "reads", "writes", "dma", "deps", "sig", "sem", "val", "name")

    def __init__(self, eng, fn, reads, writes, dma, name):
        self.eng, self.fn, self.reads, self.writes, self.dma, self.name = eng, fn, reads, writes, dma, name
        self.deps = []
        self.sig = False
        self.sem = None
        self.val = 0


class Prog:
    def __init__(self, nc):
        self.nc = nc
        self.ops = []
        self.last_w = {}
        self.readers = {}

    def op(self, eng, fn, reads=(), writes=(), dma=False, name=""):
        o = Op(eng, fn, tuple(reads), tuple(writes), dma, name)
        deps = set()
        for r in o.reads:
            w = self.last_w.get(r)
            if w is not None:
                deps.add(w)
        for w_ in o.writes:
            w = self.last_w.get(w_)
            if w is not None:
                deps.add(w)
            for rd in self.readers.get(w_, ()):
                deps.add(rd)
        for d in deps:
            if d is o:
                continue
            if d.eng == o.eng and not d.dma and not o.dma:
                if o.eng == "pe":
                    continue
                if not any(self.last_w.get(r) is d for r in o.reads):
                    continue
            o.deps.append(d)
            d.sig = True
        for w_ in o.writes:
            self.last_w[w_] = o
            self.readers[w_] = []
        for r in o.reads:
            if r not in o.writes:
                self.readers.setdefault(r, []).append(o)
        self.ops.append(o)
        return o

    def pe(self, fn, reads=(), writes=(), name=""):
        return self.op("pe", fn, reads, writes, name=name)

    def act(self, fn, reads=(), writes=(), name=""):
        return self.op("act", fn, reads, writes, name=name)

    def dve(self, fn, reads=(), writes=(), name=""):
        return self.op("dve", fn, reads, writes, name=name)

    def pool(self, fn, reads=(), writes=(), name=""):
        return self.op("pool", fn, reads, writes, name=name)

    def ve(self, eng, fn, reads=(), writes=(), name=""):
        return self.op(eng, fn, reads, writes, name=name)

    def dma(self, q, fn, reads=(), writes=(), name=""):
        return self.op(q, fn, reads, writes, dma=True, name=name)

    def _sync_all(self, engines):
        last = {}
        dmas = {q: [] for q in DMAQ}
        for o in self.ops:
            if o.fn is None:
                continue
            if o.dma:
                dmas[o.eng].append(o)
            else:
                last[o.eng] = o
        deps = list(last.values())
        for q in DMAQ:
            deps += dmas[q][-NDSEM:]
        for e in engines:
            o = Op(e, None, (), (), False, "sync_all")
            for d in deps:
                if d.eng == e and not d.dma:
                    continue
                o.deps.append(d)
                d.sig = True
            self.ops.append(o)

    def barrier(self):
        self._sync_all(ENGS)

    def finish(self):
        self._sync_all(("sp",))

    def emit(self, es):
        nc = self.nc
        engsem = {e: es.enter_context(nc.semaphore("s_" + e)) for e in ENGS}
        dsem = {e: [es.enter_context(nc.semaphore(f"d_{e}{i}")) for i in range(NDSEM)] for e in DMAQ}
        cnt = {e: 0 for e in ENGS}
        hist = {e: [] for e in DMAQ}
        per_eng = {e: [] for e in ENGS}
        for o in self.ops:
            if o.dma:
                h = hist[o.eng]
                i = len(h)
                o.sem = dsem[o.eng][i % NDSEM]
                o.val = 16 * (i // NDSEM + 1)
                o.sig = True
                if i >= NDSEM and h[i - NDSEM] not in o.deps:
                    o.deps.append(h[i - NDSEM])
                h.append(o)
            elif o.sig and o.fn is not None:
                cnt[o.eng] += 1
                o.sem = engsem[o.eng]
                o.val = cnt[o.eng]
            per_eng[o.eng].append(o)
        block = es.enter_context(nc.Block())
        handles = {"pe": "tensor", "act": "scalar", "dve": "vector", "pool": "gpsimd", "sp": "sync"}

        def make(e):
            def body(eng):
                seen = {}
                for o in per_eng[e]:
                    for d in o.deps:
                        k = d.sem.name
                        if seen.get(k, 0) >= d.val:
                            continue
                        seen[k] = d.val
                        eng.wait_ge(d.sem, d.val)
                    if o.fn is None:
                        continue
                    ins = o.fn(eng)
                    if o.sig:
                        ins.then_inc(o.sem, 16 if o.dma else 1)
            return body

        for e in ENGS:
            getattr(block, handles[e])(make(e))
        self.stats = {e: len(per_eng[e]) for e in ENGS}


class Arena:
    def __init__(self, ap_bf16, nbytes):
        self.ap = ap_bf16
        self.nbytes = nbytes
        self.off = 0

    def reset(self):
        self.off = 0

    def alloc(self, shape, dt):
        esz = {F32: 4, BF16: 2, I32: 4}[dt]
        n = int(np.prod(shape[1:]))
        nb = (n * esz + 31) // 32 * 32
        assert self.off + nb <= self.nbytes, f"arena overflow {self.off + nb} > {self.nbytes}"
        v = self.ap[:, self.off // 2:(self.off + n * esz) // 2]
        self.off += nb
        if dt != BF16:
            v = v.bitcast(dt)
        if len(shape) == 3:
            v = v.rearrange("p (a b) -> p a b", b=shape[2])
        elif len(shape) == 4:
            v = v.rearrange("p (a b c) -> p a b c", b=shape[2], c=shape[3])
        return v


class Rot:
    def __init__(self, name, aps):
        self.name, self.aps, self.i = name, aps, 0

    def next(self):
        k = self.i % len(self.aps)
        self.i += 1
        return self.aps[k], (self.name, k)


def build(debug=False):
    nc = bass.Bass("TRN2", target_bir_lowering=False)
    dbg_out = {}

    def DT(name, shape, dt, kind="ExternalInput"):
        return nc.dram_tensor(name, list(shape), dt, kind=kind).ap()

    x_d = DT("x", [T, D], F32)
    ccol_d = DT("ccol", [128, 8], F32)
    adaw_d = DT("ada_w", [D, 6 * D], F32)
    adab_d = DT("ada_b", [1, 6 * D], F32)
    win_d = DT("w_in", [D, INC], F32)
    wq_d = DT("w_q", [4, 128, 128], F32)
    wk_d = DT("w_k", [4, 128, 128], F32)
    wpool_d = DT("w_pool", [4, 128, 128], F32)
    wout_d = DT("w_out", [D, D], F32)
    wr_d = DT("w_r", [D, 36], F32)
    wgate_d = DT("w_gate", [NE * 128, 8 * 512], F32)
    wup_d = DT("w_up", [NE * 128, 8 * 512], F32)
    wdown_d = DT("w_down", [NE * 128, 4 * D], F32)
    colp_d = DT("colp", [128, 40], F32)
    NROW = 512 + 36 + 8 + 64 + 32 + JMAX + NB
    rowp_d = DT("rowp", [1, NROW], F32)
    g2_d = DT("norm2_g", [1, D], F32)
    fg_d = DT("final_g", [1, D], F32)
    ident_d = DT("ident", [128, 128], F32)
    tri_d = DT("tri", [128, 128], F32)
    rowoff_d = DT("rowoff", [128, 8], F32)
    out_d = DT("out", [T, D], F32, "ExternalOutput")
    scr = "ExternalOutput"
    X1_d = DT("X1", [T, D], F32, scr)
    H2_d = DT("H2", [T, D], BF16, scr)
    Hs_d = DT("Hs", [NSLOT, D], BF16, "Internal")
    Y_d = DT("Y", [NSLOT, D], F32, "Internal")

    es = ExitStack()
    with es:
        def S(name, shape, dt):
            return es.enter_context(nc.sbuf_tensor("s_" + name, list(shape), dt))

        p = Prog(nc)
        banks = [es.enter_context(nc.psum_tensor(f"bank{i}", [128, 512], F32)) for i in range(8)]
        bank_i = [0]

        def nbank():
            k = bank_i[0] % 5
            bank_i[0] += 1
            return banks[k], ("bank", k)

        obank_i = [0]

        def obank():
            k = 5 + obank_i[0] % 2
            obank_i[0] += 1
            return banks[k], ("bank", k)

        ident = S("ident", [128, 128], F32)
        identb = S("identb", [128, 128], BF16)
        tri = S("tri", [128, 128], F32)
        trib = S("trib", [128, 128], BF16)
        striub = S("striub", [128, 128], BF16)
        onesf = S("onesf", [128, 128], F32)
        onesb = S("onesb", [128, 128], BF16)
        colp = S("colp", [128, 40], F32)
        rowb = S("rowb", [128, NROW], F32)
        rowoff = S("rowoff", [128, 8], F32)
        modB = S("modB", [128, 4 * D], F32)
        s2bc = S("s2bc", [128, D], F32)
        wi = S("wi", [128, 8, INC], BF16)
        wo = S("wo", [128, 8, D], BF16)
        wqk = S("wqk", [128, 2, 4, 128], BF16)
        wpl = S("wpl", [128, 4, 128], BF16)
        wr = S("wr", [128, 8, 36], BF16)
        biasbc = S("biasbc", [128, 520], F32)
        biascol = S("biascol", [128, 8], F32)
        bpscol = S("bpscol", [128, 4], F32)
        s1col = S("s1col", [128, 8], F32)
        Cst = S("Cst", [128, 4, 129], F32)
        ohs = S("ohs", [128, NT, 2, 32], BF16)
        ohsum = S("ohsum", [128, NT, 32], BF16)
        gts = S("gts", [128, NT, 2], F32)
        destf = S("destf", [128, NT, 2], F32)
        desti = S("desti", [128, NT, 2], I32)
        widx = S("widx", [128, NB], I32)
        widxc = S("widxc", [128, NB], I32)
        ztile = S("ztile", [128, D], BF16)
        ARENA_BYTES = 119808
        arena_t = S("arena", [128, ARENA_BYTES // 2], BF16)
        arena = Arena(arena_t, ARENA_BYTES)

        g1col = colp[:, 0:8]
        convw = colp[:, 8:24]
        convb = colp[:, 24:28]
        skipc = colp[:, 28:32]
        bpoolc = colp[:, 32:36]
        pscalec = colp[:, 36:40]
        r0 = 0
        normg_bc = rowb[:, r0:r0 + 512]; r0 += 512
        br_bc = rowb[:, r0:r0 + 36]; r0 += 36
        bif_bc = rowb[:, r0:r0 + 8]; r0 += 8
        inv0_bc = rowb[:, r0:r0 + 64]; r0 += 64
        iota_bc = rowb[:, r0:r0 + 32]; r0 += 32
        thr_bc = rowb[:, r0:r0 + JMAX]; r0 += JMAX
        bpos_bc = rowb[:, r0:r0 + NB]; r0 += NB
        gate_a_bc = modB[:, 0:D]
        shift_f_bc = modB[:, D:2 * D]
        scale_f_bc = modB[:, 2 * D:3 * D]
        gate_f_bc = modB[:, 3 * D:4 * D]

        def dump(name, ap, key, dt=F32):
            if not debug:
                return
            shp = list(ap.shape)
            t = DT("dbg_" + name, shp, dt, "ExternalOutput")
            dbg_out["dbg_" + name] = shp
            p.dma("sp", lambda e: e.dma_start(out=t, in_=ap), reads=[key], name="dump")

        p.dma("sp", lambda e: e.dma_start(out=ident[:], in_=ident_d), writes=["ident"])
        p.dma("sp", lambda e: e.dma_start(out=tri[:], in_=tri_d), writes=["tri"])
        p.dma("sp", lambda e: e.dma_start(out=colp[:], in_=colp_d), writes=["colp"])
        p.dma("sp", lambda e: e.dma_start(out=rowb[:], in_=rowp_d.partition_broadcast(128)), writes=["rowb"])
        p.dma("sp", lambda e: e.dma_start(out=rowoff[:], in_=rowoff_d), writes=["rowoff"])
        p.dve(lambda e: e.tensor_copy(out=identb[:], in_=ident[:]), reads=["ident"], writes=["identb"])
        p.dve(lambda e: e.tensor_copy(out=trib[:], in_=tri[:]), reads=["tri"], writes=["trib"])
        p.dve(lambda e: e.tensor_tensor(out=striub[:], in0=tri[:], in1=ident[:], op=ALU.subtract),
              reads=["tri", "ident"], writes=["striub"])
        p.dve(lambda e: e.memset(onesf[:], 1.0), writes=["onesf"])
        p.dve(lambda e: e.memset(onesb[:], 1.0), writes=["onesb"])
        p.dve(lambda e: e.memset(Cst[:], 0.0), writes=[("C", h) for h in range(4)])

        p.pool(lambda e: e.memset(ztile[:], 0.0), writes=["ztile"])
        hs_v = Hs_d.rearrange("(n a p) c -> n p a c", p=128, a=BT)
        nz = NB
        rem = 0
        zero_todo = list(range(nz))

        def pump_zero(n):
            for _ in range(n):
                if not zero_todo:
                    return
                nn = zero_todo.pop(0)
                for a_ in range(BT):
                    p.dma("pool", (lambda nn, a_: lambda e: e.dma_start(out=hs_v[nn][:, a_, :], in_=ztile[:]))(nn, a_),
                          reads=["ztile"], writes=[("Hs", "z", nn, a_)])

        modA = arena.alloc([128, 2 * D], F32)
        shift_a_bc = modA[:, 0:D]
        scale_a_bc = modA[:, D:2 * D]

        def modslice(j):
            return modA[:, j * 512:(j + 1) * 512] if j < 4 else modB[:, (j - 4) * 512:(j - 3) * 512]
        cc = arena.alloc([128, 8], F32)
        scb = arena.alloc([128, 8], BF16)
        screp = arena.alloc([128, 8, 128], BF16)
        p.dma("sp", lambda e: e.dma_start(out=cc, in_=ccol_d), writes=["cc"])
        p.act(lambda e: e.activation(out=scb, in_=cc, func=AF.Silu), reads=["cc"], writes=["scb"])
        p.dve(lambda e: e.tensor_copy(out=screp, in_=scb.unsqueeze(2).to_broadcast([128, 8, 128])),
              reads=["scb"], writes=["screp"])
        wa_r = Rot("wa", [arena.alloc([128, 8, 512], BF16) for _ in range(2)])
        ab_r = Rot("ab", [arena.alloc([128, 512], F32) for _ in range(2)])
        adaw_v = adaw_d.rearrange("(k p) n -> p k n", p=128)
        for j in range(12):
            wa, wak = wa_r.next()
            ab, abk = ab_r.next()
            p.dma("pool", (lambda wa, j: lambda e: e.dma_start(out=wa, in_=adaw_v[:, :, j * 512:(j + 1) * 512]))(wa, j),
                  writes=[wak])
            p.dma("sp", (lambda ab, j: lambda e: e.dma_start(
                out=ab, in_=adab_d[0:1, j * 512:(j + 1) * 512].partition_broadcast(128)))(ab, j), writes=[abk])
            bk, bkk = nbank()

            def mm(e, wa=wa, bk=bk):
                for k in range(8):
                    ins = e.matmul(bk[:], lhsT=screp[:, k, :], rhs=wa[:, k, :], start=(k == 0), stop=(k == 7))
                return ins
            p.pe(mm, reads=["screp", wak], writes=[bkk])
            p.dve((lambda ab, bk, j: lambda e: e.tensor_tensor(out=modslice(j), in0=bk[:], in1=ab,
                                                               op=ALU.add))(ab, bk, j),
                  reads=[bkk, abk], writes=[("mod", j)])
        MODALL = [("mod", j) for j in range(12)]

        win_v = win_d.rearrange("(k p) n -> p k n", p=128)
        for k in range(8):
            p.dma("pool", (lambda k: lambda e: e.dma_start(out=wi[:, k, :], in_=win_v[:, k, :]))(k), writes=[("wi", k)])
        WIALL = [("wi", k) for k in range(8)]
        wout_v = wout_d.rearrange("(k p) n -> p k n", p=128)
        for k in range(0, 8, 2):
            p.dma("pool", (lambda k: lambda e: e.dma_start(out=wo[:, k:k + 2, :], in_=wout_v[:, k:k + 2, :]))(k),
                  writes=[("wo", k)])
        WOALL = [("wo", k) for k in range(0, 8, 2)]
        WOSC = True
        p.dma("pool", lambda e: e.dma_start(out=wqk[:, 0, :, :], in_=wq_d.rearrange("h d e -> d h e")), writes=["wq"])
        p.dma("pool", lambda e: e.dma_start(out=wqk[:, 1, :, :], in_=wk_d.rearrange("h d e -> d h e")), writes=["wk"])
        p.dma("pool", lambda e: e.dma_start(out=wpl[:], in_=wpool_d.rearrange("g c d -> c g d")), writes=["wpl"])
        p.dma("pool", lambda e: e.dma_start(out=wr[:], in_=wr_d.rearrange("(k p) n -> p k n", p=128)), writes=["wr"])

        tmpA = arena.alloc([128, D], F32)
        tmp3 = arena.alloc([128, 8, 128], F32)
        scl = arena.alloc([128, 8], F32)
        shc = arena.alloc([128, 8], F32)
        shcb = arena.alloc([128, 8], BF16)
        shrep = arena.alloc([128, 8, 128], BF16)
        idb3 = ident[:].unsqueeze(1).to_broadcast([128, 8, 128])
        p.dve(lambda e: e.tensor_tensor(out=tmp3, in0=scale_a_bc.rearrange("p (a b) -> p a b", b=128), in1=idb3, op=ALU.mult),
              reads=MODALL + ["ident"], writes=["tmp3"])
        p.dve(lambda e: e.reduce_sum(out=scl, in_=tmp3, axis=AX.X), reads=["tmp3"], writes=["scl"])
        p.dve(lambda e: e.scalar_tensor_tensor(out=s1col[:], in0=scl, scalar=1.0, in1=g1col, op0=ALU.add, op1=ALU.mult),
              reads=["scl", "colp"], writes=["s1col"])
        p.dve(lambda e: e.tensor_tensor(out=tmp3, in0=shift_a_bc.rearrange("p (a b) -> p a b", b=128), in1=idb3, op=ALU.mult),
              reads=MODALL + ["ident", "scl"], writes=["tmp3"])
        p.dve(lambda e: e.reduce_sum(out=shc, in_=tmp3, axis=AX.X), reads=["tmp3"], writes=["shc"])
        p.dve(lambda e: e.tensor_copy(out=shcb, in_=shc), reads=["shc"], writes=["shcb"])
        p.dve(lambda e: e.tensor_copy(out=shrep, in_=shcb.unsqueeze(2).to_broadcast([128, 8, 128])),
              reads=["shcb"], writes=["shrep"])
        bk, bkk = nbank()

        def mmbv(e, bk=bk):
            for k in range(8):
                ins = e.matmul(bk[:], lhsT=shrep[:, k, :], rhs=wi[:, k, COL_V:COL_V + 512], start=(k == 0), stop=(k == 7))
            return ins
        p.pe(mmbv, reads=["shrep"] + WIALL, writes=[bkk])
        p.dve((lambda bk: lambda e: e.tensor_copy(out=biasbc[:, 0:512], in_=bk[:]))(bk), reads=[bkk], writes=["biasbc_v"])
        bk, bkk = nbank()

        def mmbg(e, bk=bk):
            for k in range(8):
                ins = e.matmul(bk[:, 0:8], lhsT=shrep[:, k, :], rhs=wi[:, k, COL_I:COL_I + 8], start=(k == 0), stop=(k == 7))
            return ins
        p.pe(mmbg, reads=["shrep"] + WIALL, writes=[bkk])
        p.dve((lambda bk: lambda e: e.tensor_tensor(out=biasbc[:, 512:520], in0=bk[:, 0:8], in1=bif_bc, op=ALU.add))(bk),
              reads=[bkk, "rowb"], writes=["biasbc_g"])
        bk, bkk = nbank()

        def mmbc(e, bk=bk):
            for c in range(8):
                c0 = (COL_U if c < 4 else COL_O) + (c % 4) * 128
                for k in range(8):
                    ins = e.matmul(bk[:, c:c + 1], lhsT=wi[:, k, c0:c0 + 128], rhs=shcb[:, k:k + 1], start=(k == 0), stop=(k == 7))
            return ins
        p.pe(mmbc, reads=["shcb"] + WIALL, writes=[bkk])
        p.dve((lambda bk: lambda e: e.tensor_copy(out=biascol[:], in_=bk[:, 0:8]))(bk), reads=[bkk], writes=["biascol"])
        for k in range(8):
            p.dve((lambda k: lambda e: e.tensor_scalar(out=wi[:, k, :], in0=wi[:, k, :], scalar1=s1col[:, k:k + 1], scalar2=None,
                                                       op0=ALU.mult))(k),
                  reads=["s1col", ("wi", k)], writes=[("wi", k)])
        p.dma("sp", lambda e: e.dma_start(out=tmpA, in_=g2_d.partition_broadcast(128)), writes=["tmpA"])
        p.dve(lambda e: e.scalar_tensor_tensor(out=s2bc[:], in0=scale_f_bc, scalar=1.0, in1=tmpA, op0=ALU.add, op1=ALU.mult),
              reads=MODALL + ["tmpA"], writes=["s2bc"])
        p.dve(lambda e: e.tensor_tensor(out=bpscol[:], in0=bpoolc, in1=pscalec, op=ALU.mult), reads=["colp"], writes=["bpscol"])
        for k in range(0, 8, 2):
            for kk in (k, k + 1):
                p.dve((lambda kk: lambda e: e.tensor_tensor(out=wo[:, kk, :], in0=wo[:, kk, :], in1=gate_a_bc, op=ALU.mult))(kk),
                      reads=[("wo", k)] + MODALL, writes=[("wo", k)])
        dump("modB", modB[:], ("mod", 11))
        dump("biasbc", biasbc[:], "biasbc_g")
        dump("biascol", biascol[:], "biascol")

        p.barrier()
        arena.reset()
        A = arena.alloc
        xs_r = Rot("xs", [A([128, D], F32) for _ in range(2)])
        xb_r = Rot("xb", [A([128, D], BF16) for _ in range(2)])
        sqj = A([128, D], BF16)
        xT = A([128, 8, SEG], BF16)
        R1 = A([128, SEG], F32)
        ssq1 = A([128, TPS], F32)
        rstd1 = A([128, TPS], F32)
        diag_r = Rot("diag", [A([128, 128], F32) for _ in range(2)])
        ubuf = A([128, 4, SEG + 3], F32)
        scrA = A([128, 2 * SEG], F32)
        ctmp_r = Rot("ctmp", [scrA[:, 0:SEG], scrA[:, SEG:2 * SEG]])
        CT2 = [("ctmp", 0), ("ctmp", 1)]
        ucT = A([128, 4, SEG], BF16)
        sigoT2 = [A([128, 4, SEG], BF16) for _ in range(2)]
        scrB = A([128, 2 * SEG], F32)
        otmp_r = Rot("otmp", [scrB[:, 0:SEG], scrB[:, SEG:2 * SEG]])
        OT2 = [("otmp", 0), ("otmp", 1)]
        pbuf = A([128, 4, SEG + 16], F32)
        ptmp = [A([128, SEG + 16], F32) for _ in range(2)]
        pooled = A([128, 4, SEG], BF16)
        p16 = A([128, 16], F32)
        yT = A([128, 8, SEG], BF16)
        vaug2 = [A([128, TPS, 4, 129], BF16) for _ in range(2)]
        gat = A([128, TPS, 8], F32)
        gsx = A([128, 8, TPS * 4], F32)
        gtmp = A([128, TPS, 4], F32)
        qT = A([128, 4, SEG], BF16)
        kT = A([128, 4, SEG], BF16)
        ktok = A([128, TPS, 4, 128], BF16)
        wv_r = Rot("wv", [A([128, 129], BF16) for _ in range(4)])
        dst_r = Rot("dst", [A([128, 128], BF16) for _ in range(4)])
        cb_r = Rot("cb", [A([128, 129], BF16) for _ in range(4)])
        sm_r = Rot("sm", [A([128, 8], F32) for _ in range(4)])
        hn_r = Rot("hn", [A([128, 512], BF16) for _ in range(2)])
        ytmp_r = Rot("otmp", [scrB[:, 0:SEG].rearrange("p (a b) -> p a b", b=128), scrB[:, SEG:2 * SEG].rearrange("p (a b) -> p a b", b=128)])
        prod_r = Rot("prod", [A([128, 512], F32) for _ in range(2)])
        h2_r = Rot("h2", [A([128, D], BF16) for _ in range(2)])
        h2T_r = Rot("h2T", [A([128, 8, 128], BF16) for _ in range(1)])
        st2 = A([128, 8], F32)
        rt_r = Rot("rt", [A([128, 768], F32) for _ in range(1)])
        print("phase1 arena used", arena.off)

        p.dve(lambda e: e.memset(vaug2[0], 1.0), writes=[("vaug", 0, j) for j in range(TPS)])
        p.dve(lambda e: e.memset(vaug2[1], 1.0), writes=[("vaug", 1, j) for j in range(TPS)])
        p.dve(lambda e: e.memset(ubuf[:, :, 0:3], 0.0), writes=[("u", c, "halo") for c in range(4)])
        p.dve(lambda e: e.memset(pbuf[:, :, 0:16], 0.0), writes=[("p", c, "halo") for c in range(4)])

        x_v = x_d.rearrange("(n p) c -> n p c", p=128)
        X1_v = X1_d.rearrange("(n p) c -> n p c", p=128)
        H2_v = H2_d.rearrange("(n p) c -> n p c", p=128)
        QCS, QTOT, QINVRS, QG, QWS, QAC, QSQA, QTMP = range(8)

        def stF(sg):
            par = sg % 2
            vaug = vaug2[par]
            sigoT = sigoT2[par]
            for j in range(TPS):
                ti = sg * TPS + j
                xs, xsk = xs_r.next()
                xb, xbk = xb_r.next()
                p.dma("sp", (lambda xs, ti: lambda e: e.dma_start(out=xs, in_=x_v[ti]))(xs, ti), writes=[xsk])
                p.act((lambda xs, j: lambda e: e.activation(out=sqj, in_=xs, func=AF.Square, accum_out=ssq1[:, j:j + 1]))(xs, j),
                      reads=[xsk], writes=["sqj", ("ssq1", j)])
                p.act((lambda j: lambda e: e.activation(out=rstd1[:, j:j + 1], in_=ssq1[:, j:j + 1], func=AF.Sqrt, scale=1.0 / D, bias=EPS))(j),
                      reads=[("ssq1", j)], writes=[("std1", j)])
                p.dve((lambda j: lambda e: e.reciprocal(out=rstd1[:, j:j + 1], in_=rstd1[:, j:j + 1]))(j), reads=[("std1", j)], writes=[("rstd1", j)])
                p.dve((lambda xs, xb, j: lambda e: e.tensor_scalar(out=xb, in0=xs, scalar1=rstd1[:, j:j + 1], scalar2=None, op0=ALU.mult))(xs, xb, j),
                      reads=[xsk, ("rstd1", j)], writes=[xbk])
                bk, bkk = nbank()
                bkb = bk[:].bitcast(BF16).rearrange("p (a b) -> p a b", b=128)

                def tr(e, xb=xb, bkb=bkb):
                    for k in range(8):
                        ins = e.transpose(out=bkb[:, k, :], in_=xb[:, k * 128:(k + 1) * 128], identity=identb[:])
                    return ins
                p.pe(tr, reads=[xbk, "identb"], writes=[bkk])
                p.act((lambda bkb, j: lambda e: e.copy(out=xT[:, :, j * 128:(j + 1) * 128], in_=bkb))(bkb, j),
                      reads=[bkk], writes=[("xT", j)])
                yield
            XTALL = [("xT", j) for j in range(TPS)]

            for grp, col0 in (("U", COL_U), ("O", COL_O), ("P", COL_P)):
                for c in range(4):
                    bk, bkk = nbank()
                    c0 = col0 + c * 128

                    def mm(e, bk=bk, c0=c0):
                        for k in range(8):
                            ins = e.matmul(bk[:], lhsT=wi[:, k, c0:c0 + 128], rhs=xT[:, k, :], start=(k == 0), stop=(k == 7))
                        return ins
                    p.pe(mm, reads=WIALL + XTALL, writes=[bkk])
                    if grp == "U":
                        p.act((lambda bk, c: lambda e: e.activation(out=ubuf[:, c, 3:SEG + 3], in_=bk[:], func=AF.Identity,
                                                                    bias=biascol[:, c:c + 1]))(bk, c),
                              reads=[bkk, "biascol"], writes=[("u", c, "body")])
                    elif grp == "O":
                        p.act((lambda bk, c: lambda e: e.activation(out=sigoT[:, c, :], in_=bk[:], func=AF.Sigmoid,
                                                                    bias=biascol[:, 4 + c:5 + c]))(bk, c),
                              reads=[bkk, "biascol"], writes=[("sigo", par, c)])
                    else:
                        p.dve((lambda bk, c: lambda e: e.tensor_copy(out=pbuf[:, c, 16:SEG + 16], in_=bk[:]))(bk, c),
                              reads=[bkk], writes=[("p", c, "body")])
                    yield
            for j in range(TPS):
                bk, bkk = nbank()

                def mmv(e, bk=bk, j=j):
                    for k in range(8):
                        ins = e.matmul(bk[:], lhsT=xT[:, k, j * 128:(j + 1) * 128], rhs=wi[:, k, COL_V:COL_V + 512],
                                       start=(k == 0), stop=(k == 7))
                    return ins
                p.pe(mmv, reads=WIALL + XTALL, writes=[bkk])
                p.dve((lambda bk, j: lambda e: e.tensor_tensor(
                    out=vaug[:, j, :, 0:128], in0=bk[:].rearrange("p (a b) -> p a b", b=128),
                    in1=biasbc[:, 0:512].rearrange("p (a b) -> p a b", b=128), op=ALU.add))(bk, j),
                    reads=[bkk, "biasbc_v"], writes=[("vaug", par, j)])
                bk, bkk = nbank()

                def mmg(e, bk=bk, j=j):
                    for k in range(8):
                        ins = e.matmul(bk[:, 0:8], lhsT=xT[:, k, j * 128:(j + 1) * 128], rhs=wi[:, k, COL_I:COL_I + 8],
                                       start=(k == 0), stop=(k == 7))
                    return ins
                p.pe(mmg, reads=WIALL + XTALL, writes=[bkk])
                p.dve((lambda bk, j: lambda e: e.tensor_tensor(out=gat[:, j, :], in0=bk[:, 0:8], in1=biasbc[:, 512:520], op=ALU.add))(bk, j),
                      reads=[bkk, "biasbc_g"], writes=[("gat", j)])
                yield

        def stM(sg):
            GATALL = [("gat", j) for j in range(TPS)]

            for c in range(4):
                eng = "dve"
                ct, ctk = ctmp_r.next()
                UR = [("u", c, "halo"), ("u", c, "body")]
                p.ve(eng, (lambda ct, c: lambda e: e.tensor_scalar(out=ct, in0=ubuf[:, c, 0:SEG], scalar1=convw[:, c * 4:c * 4 + 1],
                                                                    scalar2=None, op0=ALU.mult))(ct, c),
                     reads=UR + ["colp"], writes=[ctk])
                for k in range(1, 4):
                    p.ve(eng, (lambda ct, c, k: lambda e: e.scalar_tensor_tensor(
                        out=ct, in0=ubuf[:, c, k:k + SEG], scalar=convw[:, c * 4 + k:c * 4 + k + 1], in1=ct,
                        op0=ALU.mult, op1=ALU.add))(ct, c, k), reads=UR + ["colp", ctk], writes=[ctk])
                p.act((lambda ct, c: lambda e: e.activation(out=ucT[:, c, :], in_=ct, func=AF.Silu, bias=convb[:, c:c + 1]))(ct, c),
                      reads=[ctk, "colp"], writes=[("ucT", c)])
                p.ve(eng, (lambda c: lambda e: e.tensor_copy(out=ubuf[:, c, 0:3], in_=ubuf[:, c, SEG:SEG + 3]))(c),
                     reads=[("u", c, "body")], writes=[("u", c, "halo")])
            for g in range(4):
                eng = "pool" if g % 2 == 0 else "dve"
                PR = [("p", g, "halo"), ("p", g, "body")]
                W = SEG + 16
                src = pbuf[:, g, :]
                srck = PR
                sh = 1
                for lvl in range(g + 1):
                    dstt = ptmp[lvl % 2]
                    dk = ("ptmp", lvl % 2)
                    lo = 2 * sh
                    p.ve(eng, (lambda dstt, src, sh, lo: lambda e: e.tensor_tensor(
                        out=dstt[:, lo:W], in0=src[:, lo:W], in1=src[:, lo - sh:W - sh], op=ALU.add))(dstt, src, sh, lo),
                        reads=list(srck), writes=[dk])
                    src, srck, sh = dstt, [dk], sh * 2
                wg_ = float(2 ** (g + 1))
                p.ve("dve", (lambda src, g, wg_: lambda e: e.scalar_tensor_tensor(
                    out=pooled[:, g, :], in0=src[:, 16:W], scalar=1.0 / wg_, in1=pbuf[:, g, 16:W],
                    op0=ALU.mult, op1=ALU.subtract))(src, g, wg_), reads=list(srck) + PR, writes=[("pooled", g)])
                if sg == 0:
                    p.ve(eng, (lambda src, g: lambda e: e.tensor_tensor(out=p16, in0=src[:, 16:32], in1=inv0_bc[:, g * 16:(g + 1) * 16],
                                                                        op=ALU.mult))(src, g),
                         reads=list(srck) + ["rowb"], writes=["p16"])
                    p.ve(eng, (lambda g: lambda e: e.tensor_tensor(out=pooled[:, g, 0:16], in0=p16, in1=pbuf[:, g, 16:32],
                                                                   op=ALU.subtract))(g),
                         reads=["p16"] + PR, writes=[("pooled", g)])
                p.ve(eng, (lambda g: lambda e: e.tensor_copy(out=pbuf[:, g, 0:16], in_=pbuf[:, g, SEG:SEG + 16]))(g),
                     reads=[("p", g, "body")], writes=[("p", g, "halo")])
                bk, bkk = nbank()
                p.pe((lambda bk, g: lambda e: e.matmul(bk[:], lhsT=wpl[:, g, :], rhs=pooled[:, g, :], start=True, stop=True))(bk, g),
                     reads=["wpl", ("pooled", g)], writes=[bkk])
                p.act((lambda bk, g: lambda e: e.activation(out=yT[:, 4 + g, :], in_=bk[:], func=AF.Identity,
                                                            scale=pscalec[:, g:g + 1], bias=bpscol[:, g:g + 1]))(bk, g),
                      reads=[bkk, "colp", "bpscol"], writes=[("yT", 4 + g)])

            gi = gat[:, :, 0:4]
            gf = gat[:, :, 4:8]
            q = lambda n: gsx[:, n, :].rearrange("p (a b) -> p a b", b=4)
            p.act(lambda e: e.activation(out=gtmp, in_=gf, func=AF.Exp, scale=-1.0), reads=GATALL, writes=["gtmp"])
            p.act(lambda e: e.activation(out=gtmp, in_=gtmp, func=AF.Ln, bias=1.0), reads=["gtmp"], writes=["gtmp"])
            bk, bkk = nbank()

            def mmcs(e, bk=bk):
                for j in range(TPS):
                    e.matmul(bk[:, j * 4:(j + 1) * 4], lhsT=tri[:], rhs=gtmp[:, j, :], start=True, stop=True)
                for j in range(TPS):
                    ins = e.matmul(bk[:, 64 + j * 4:64 + (j + 1) * 4], lhsT=onesf[:], rhs=gtmp[:, j, :], start=True, stop=True)
                return ins
            p.pe(mmcs, reads=["gtmp", "tri", "onesf"], writes=[bkk])
            p.dve((lambda bk: lambda e: e.tensor_copy(out=gsx[:, QCS, :], in_=bk[:, 0:TPS * 4]))(bk), reads=[bkk], writes=["q_cs"])
            p.dve((lambda bk: lambda e: e.tensor_copy(out=gsx[:, QTOT, :], in_=bk[:, 64:64 + TPS * 4]))(bk), reads=[bkk], writes=["q_tot"])
            p.dve(lambda e: e.scalar_tensor_tensor(out=gsx[:, QTMP, :], in0=gsx[:, QTOT, :], scalar=-0.5, in1=gsx[:, QCS, :],
                                                   op0=ALU.mult, op1=ALU.add), reads=["q_cs", "q_tot"], writes=["q_tmp"])
            p.act(lambda e: e.activation(out=gsx[:, QINVRS, :], in_=gsx[:, QTMP, :], func=AF.Exp), reads=["q_tmp"], writes=["q_invrs"])
            p.dve(lambda e: e.tensor_tensor(out=q(QG), in0=q(QTMP), in1=gi, op=ALU.add), reads=["q_tmp"] + GATALL, writes=["q_g"])
            p.act(lambda e: e.activation(out=gsx[:, QG, :], in_=gsx[:, QG, :], func=AF.Exp), reads=["q_g"], writes=["q_g"])
            p.dve(lambda e: e.tensor_tensor(out=gsx[:, QWS, :], in0=gsx[:, QCS, :], in1=gsx[:, QTOT, :], op=ALU.subtract),
                  reads=["q_cs", "q_tot"], writes=["q_ws"])
            p.dve(lambda e: e.tensor_tensor(out=q(QWS), in0=q(QWS), in1=gi, op=ALU.add), reads=["q_ws"] + GATALL, writes=["q_ws"])
            p.act(lambda e: e.activation(out=gsx[:, QWS, :], in_=gsx[:, QWS, :], func=AF.Exp), reads=["q_ws"], writes=["q_ws"])
            p.act(lambda e: e.activation(out=gsx[:, QAC, :], in_=gsx[:, QTOT, :], func=AF.Exp, scale=-1.0), reads=["q_tot"], writes=["q_ac"])
            p.act(lambda e: e.activation(out=gsx[:, QSQA, :], in_=gsx[:, QTOT, :], func=AF.Exp, scale=-0.5), reads=["q_tot"], writes=["q_sqa"])

            for h in range(4):
                for wsel, dstT, sc, nm in ((0, qT, 128.0 ** -0.5, "qT"), (1, kT, 1.0, "kT")):
                    bk, bkk = nbank()
                    p.pe((lambda bk, h, wsel: lambda e: e.matmul(bk[:], lhsT=wqk[:, wsel, h, :], rhs=ucT[:, h, :], start=True, stop=True))(bk, h, wsel),
                         reads=["wq", "wk", ("ucT", h)], writes=[bkk])
                    p.act((lambda bk, h, dstT, sc: lambda e: e.activation(out=dstT[:, h, :], in_=bk[:], func=AF.Copy, scale=sc))(bk, h, dstT, sc),
                          reads=[bkk], writes=[(nm, h)])
            for j in range(TPS):
                bk, bkk = nbank()

                def mmk(e, bk=bk, j=j):
                    for h in range(4):
                        ins = e.matmul(bk[:, h * 128:(h + 1) * 128], lhsT=ucT[:, h, j * 128:(j + 1) * 128], rhs=wqk[:, 1, h, :],
                                       start=True, stop=True)
                    return ins
                p.pe(mmk, reads=["wk"] + [("ucT", h) for h in range(4)], writes=[bkk])
                p.dve((lambda bk, j: lambda e: e.tensor_copy(out=ktok[:, j, :, :], in_=bk[:].rearrange("p (a b) -> p a b", b=128)))(bk, j),
                      reads=[bkk], writes=[("ktok", j)])

        def stK(sg, pump, after_tile):
            par = sg % 2
            vaug = vaug2[par]
            sigoT = sigoT2[par]
            ctxs = {}
            hns = {}

            def S1(n):
                j, h = divmod(n, 4)
                jh = n
                if h == 0:
                    hns[j] = hn_r.next()
                c = {}
                eng2 = "pool" if h % 2 == 0 else "dve"
                wv, wvk = wv_r.next()
                p.ve(eng2, (lambda wv, j, h, jh: lambda e: e.tensor_scalar(out=wv, in0=vaug[:, j, h, :], scalar1=gsx[:, QWS, jh:jh + 1],
                                                                          scalar2=None, op0=ALU.mult))(wv, j, h, jh),
                     reads=[("vaug", par, j), "q_ws"], writes=[wvk])
                bkA, bkAk = nbank()
                p.pe((lambda bkA, wv, j, h: lambda e: e.matmul(bkA[:, 0:129], lhsT=ktok[:, j, h, :], rhs=wv, start=True, stop=True))(bkA, wv, j, h),
                     reads=[("ktok", j), wvk], writes=[bkAk])
                p.pe((lambda bkA, j, h: lambda e: e.matmul(bkA[:, 256:384], lhsT=kT[:, h, j * 128:(j + 1) * 128],
                                                           rhs=qT[:, h, j * 128:(j + 1) * 128], start=True, stop=True))(bkA, j, h),
                     reads=[("kT", h), ("qT", h)], writes=[bkAk])
                ds, dsk = dst_r.next()
                p.dve((lambda ds, bkA, jh: lambda e: e.scalar_tensor_tensor(out=ds, in0=bkA[:, 256:384], scalar=gsx[:, QG, jh:jh + 1],
                                                                           in1=trib[:], op0=ALU.mult, op1=ALU.mult))(ds, bkA, jh),
                      reads=[bkAk, "q_g", "trib"], writes=[dsk])
                cb, cbk = cb_r.next()
                p.ve(eng2, (lambda cb, h, jh: lambda e: e.tensor_scalar(out=cb, in0=Cst[:, h, :], scalar1=gsx[:, QSQA, jh:jh + 1],
                                                                       scalar2=None, op0=ALU.mult))(cb, h, jh),
                     reads=[("C", h), "q_sqa"], writes=[cbk])
                p.dve((lambda bkA, h, jh: lambda e: e.scalar_tensor_tensor(out=Cst[:, h, :], in0=Cst[:, h, :], scalar=gsx[:, QAC, jh:jh + 1],
                                                                          in1=bkA[:, 0:129], op0=ALU.mult, op1=ALU.add))(bkA, h, jh),
                      reads=[("C", h), "q_ac", bkAk], writes=[("C", h)])
                c.update(ds=ds, dsk=dsk, cb=cb, cbk=cbk)
                ctxs[n] = c

            def S2(n):
                j, h = divmod(n, 4)
                jh = n
                c = ctxs[n]
                ds, dsk, cb, cbk = c["ds"], c["dsk"], c["cb"], c["cbk"]
                bkO, bkOk = obank()

                def mmo(e, bkO=bkO, ds=ds, cb=cb, j=j, h=h):
                    e.matmul(bkO[:, 0:129], lhsT=ds, rhs=vaug[:, j, h, :], start=True, stop=False)
                    return e.matmul(bkO[:, 0:129], lhsT=qT[:, h, j * 128:(j + 1) * 128], rhs=cb, start=False, stop=True)
                p.pe(mmo, reads=[dsk, ("vaug", par, j), ("qT", h), cbk], writes=[bkOk])
                sm, smk = sm_r.next()
                p.act((lambda sm, bkO: lambda e: e.activation(out=sm[:, 0:1], in_=bkO[:, 128:129], func=AF.Abs))(sm, bkO),
                      reads=[bkOk], writes=[(smk, 0)])
                p.dve((lambda sm, jh: lambda e: e.tensor_tensor(out=sm[:, 1:2], in0=sm[:, 0:1], in1=gsx[:, QINVRS, jh:jh + 1], op=ALU.max))(sm, jh),
                      reads=[(smk, 0), "q_invrs"], writes=[(smk, 1)])
                p.dve((lambda sm: lambda e: e.reciprocal(out=sm[:, 2:3], in_=sm[:, 1:2]))(sm), reads=[(smk, 1)], writes=[(smk, 2)])
                p.act((lambda sm, bkO: lambda e: e.activation(out=sqj[:, 0:128], in_=bkO[:, 0:128], func=AF.Square, scale=sm[:, 2:3],
                                                              accum_out=sm[:, 3:4]))(sm, bkO),
                      reads=[bkOk, (smk, 2)], writes=["sqj", (smk, 3)])
                p.act((lambda sm: lambda e: e.activation(out=sm[:, 4:5], in_=sm[:, 3:4], func=AF.Sqrt, scale=1.0 / 128, bias=EPS))(sm),
                      reads=[(smk, 3)], writes=[(smk, 4)])
                c.update(bkO=bkO, bkOk=bkOk, sm=sm, smk=smk)

            def S3(n):
                j, h = divmod(n, 4)
                c = ctxs[n]
                bkO, bkOk, sm, smk = c["bkO"], c["bkOk"], c["sm"], c["smk"]
                hn, hnk = hns[j]
                p.dve((lambda sm: lambda e: e.reciprocal(out=sm[:, 5:6], in_=sm[:, 4:5]))(sm), reads=[(smk, 4)], writes=[(smk, 5)])
                p.dve((lambda sm: lambda e: e.tensor_tensor(out=sm[:, 6:7], in0=sm[:, 5:6], in1=sm[:, 2:3], op=ALU.mult))(sm),
                      reads=[(smk, 5), (smk, 2)], writes=[(smk, 6)])
                p.dve((lambda sm, bkO, hn, h: lambda e: e.scalar_tensor_tensor(
                    out=hn[:, h * 128:(h + 1) * 128], in0=bkO[:, 0:128], scalar=sm[:, 6:7], in1=normg_bc[:, h * 128:(h + 1) * 128],
                    op0=ALU.mult, op1=ALU.mult))(sm, bkO, hn, h), reads=[bkOk, (smk, 6), "rowb"], writes=[(hnk, h)])
                if h == 3:
                    bk, bkk = nbank()
                    bkb = bk[:].bitcast(BF16).rearrange("p (a b) -> p a b", b=128)

                    def trh(e, hn=hn, bkb=bkb):
                        for hh in range(4):
                            ins = e.transpose(out=bkb[:, hh, :], in_=hn[:, hh * 128:(hh + 1) * 128], identity=identb[:])
                        return ins
                    p.pe(trh, reads=[(hnk, hh) for hh in range(4)] + ["identb"], writes=[bkk])
                    yt, ytk = ytmp_r.next()
                    for hh in range(4):
                        p.dve((lambda yt, bkb, j, hh: lambda e: e.scalar_tensor_tensor(
                            out=yt[:, hh, :], in0=ucT[:, hh, j * 128:(j + 1) * 128], scalar=skipc[:, hh:hh + 1], in1=bkb[:, hh, :],
                            op0=ALU.mult, op1=ALU.add))(yt, bkb, j, hh),
                            reads=[bkk, ("ucT", hh), "colp"], writes=[ytk])
                    p.dve((lambda yt, j: lambda e: e.tensor_tensor(out=yT[:, 0:4, j * 128:(j + 1) * 128], in0=yt, in1=sigoT[:, :, j * 128:(j + 1) * 128],
                                                                    op=ALU.mult))(yt, j),
                           reads=[ytk] + [("sigo", par, cc_) for cc_ in range(4)], writes=[("yTm", j)])
                    after_tile(j)

            NIT = TPS * 4
            for step in range(NIT + 2):
                if step < NIT:
                    S1(step)
                if 0 <= step - 1 < NIT:
                    S2(step - 1)
                if 0 <= step - 2 < NIT:
                    S3(step - 2)
                pump(2 if step % 2 == 0 else 1)

        def stE(sg):
            YTALL = [("yT", 4 + g) for g in range(4)] + [("yTm", j) for j in range(TPS)]

            ectx = {}

            def EW(j):
                ti = sg * TPS + j
                xs, xsk = xs_r.next()
                p.dma("sp", (lambda xs, ti: lambda e: e.dma_start(out=xs, in_=x_v[ti]))(xs, ti), writes=[xsk])
                x1, x1k = xs, xsk
                X1K = [xsk]
                for half in range(2):
                    bk, bkk = nbank()

                    def mmw(e, bk=bk, j=j, half=half):
                        for k in range(8):
                            ins = e.matmul(bk[:], lhsT=yT[:, k, j * 128:(j + 1) * 128], rhs=wo[:, k, half * 512:(half + 1) * 512],
                                           start=(k == 0), stop=(k == 7))
                        return ins
                    p.pe(mmw, reads=[("yT", 4 + g) for g in range(4)] + [("yTm", j)] + WOALL, writes=[bkk])
                    p.dve((lambda bk, x1, half: lambda e: e.tensor_tensor(out=x1[:, half * 512:(half + 1) * 512], in0=bk[:],
                                                                         in1=x1[:, half * 512:(half + 1) * 512], op=ALU.add))(bk, x1, half),
                          reads=[bkk, xsk], writes=[xsk])
                ectx[j] = (x1, x1k, X1K, ti)

            def EP(j):
                x1, x1k, X1K, ti = ectx[j]
                p.dma("act", (lambda x1, ti: lambda e: e.dma_start(out=X1_v[ti], in_=x1))(x1, ti), reads=X1K, writes=[("X1", ti)])
                p.act((lambda x1, j: lambda e: e.activation(out=sqj, in_=x1, func=AF.Square, accum_out=st2[:, j:j + 1]))(x1, j),
                      reads=X1K, writes=["sqj", ("st2a", j)])
                p.act((lambda j: lambda e: e.activation(out=st2[:, 4 + j:5 + j], in_=st2[:, j:j + 1], func=AF.Sqrt, scale=1.0 / D, bias=EPS))(j),
                      reads=[("st2a", j)], writes=[("st2b", j)])
                p.dve((lambda j: lambda e: e.reciprocal(out=st2[:, 4 + j:5 + j], in_=st2[:, 4 + j:5 + j]))(j), reads=[("st2b", j)], writes=[("st2c", j)])
                h2f, h2fk = scrA, "h2f"
                h2, h2k = h2_r.next()
                p.dve((lambda h2f, x1, j: lambda e: e.scalar_tensor_tensor(out=h2f, in0=x1, scalar=st2[:, 4 + j:5 + j], in1=s2bc[:],
                                                                          op0=ALU.mult, op1=ALU.mult))(h2f, x1, j),
                      reads=X1K + [("st2c", j), "s2bc"], writes=[h2fk] + CT2)
                p.dve((lambda h2, h2f: lambda e: e.tensor_tensor(out=h2, in0=h2f, in1=shift_f_bc, op=ALU.add))(h2, h2f),
                       reads=[h2fk] + CT2 + MODALL, writes=[h2k])
                p.dma("act", (lambda h2, ti: lambda e: e.dma_start(out=H2_v[ti], in_=h2))(h2, ti), reads=[h2k], writes=[("H2", ti)])
                bk, bkk = nbank()
                bkb = bk[:].bitcast(BF16).rearrange("p (a b) -> p a b", b=128)

                def tr2(e, h2=h2, bkb=bkb):
                    for k in range(8):
                        ins = e.transpose(out=bkb[:, k, :], in_=h2[:, k * 128:(k + 1) * 128], identity=identb[:])
                    return ins
                p.pe(tr2, reads=[h2k, "identb"], writes=[bkk])
                h2T, h2Tk = h2T_r.next()
                p.act((lambda h2T, bkb: lambda e: e.copy(out=h2T, in_=bkb))(h2T, bkb), reads=[bkk], writes=[h2Tk])

                def mmr(e, j=j, h2T=h2T):
                    for k in range(8):
                        ins = e.matmul(banks[7][:, j * 36:(j + 1) * 36], lhsT=h2T[:, k, :], rhs=wr[:, k, :], start=(k == 0), stop=(k == 7))
                    return ins
                p.pe(mmr, reads=[h2Tk, "wr"], writes=[("rbank", j)])
                if j < TPS - 1:
                    return
                t0 = sg * TPS
                rt, rtk = rt_r.next()
                RB = [("rbank", jj) for jj in range(TPS)]
                v3 = lambda a, n_: rt[:, a:a + TPS * n_].rearrange("p (t n) -> p t n", n=n_)
                lg = v3(0, 36)
                elm = v3(144, 32)
                elm2 = v3(272, 32)
                oh1f = v3(400, 32)
                oh2f = v3(528, 32)
                ohg = v3(656, 4)
                pen = v3(672, 4)
                egj = v3(688, 4)
                s4 = lambda a: rt[:, 704 + a * 4:708 + a * 4]
                R = lambda *a: [(rtk, x) for x in a]
                bc3 = lambda ap2, n_: ap2.unsqueeze(2).to_broadcast([128, TPS, n_])
                p.dve(lambda e: e.tensor_tensor(out=lg, in0=banks[7][:, 0:TPS * 36].rearrange("p (t n) -> p t n", n=36),
                                                in1=br_bc.unsqueeze(1).to_broadcast([128, TPS, 36]), op=ALU.add),
                      reads=RB + ["rowb"], writes=R("lg"))
                p.dve(lambda e: e.reduce_max(out=s4(0), in_=lg[:, :, 0:4], axis=AX.X), reads=R("lg"), writes=R("gmax"))
                p.dve(lambda e: e.tensor_tensor(out=ohg, in0=lg[:, :, 0:4], in1=bc3(s4(0), 4), op=ALU.is_equal), reads=R("lg", "gmax"), writes=R("ohg"))
                p.dve(lambda e: e.tensor_tensor(out=egj, in0=lg[:, :, 0:4], in1=bc3(s4(0), 4), op=ALU.subtract), reads=R("lg", "gmax"), writes=R("egj"))
                p.act(lambda e: e.activation(out=egj, in_=egj, func=AF.Exp), reads=R("egj"), writes=R("egj"))
                p.dve(lambda e: e.reduce_sum(out=s4(1), in_=egj, axis=AX.X), reads=R("egj"), writes=R("sumg"))
                p.dve(lambda e: e.reciprocal(out=s4(2), in_=s4(1)), reads=R("sumg"), writes=R("ggate"))
                p.dve(lambda e: e.tensor_scalar(out=pen, in0=ohg, scalar1=-1.0, scalar2=BIG, op0=ALU.add, op1=ALU.mult), reads=R("ohg"), writes=R("pen"))
                p.dve(lambda e: e.tensor_tensor(out=elm.rearrange("p t (a b) -> p t a b", b=8),
                                                in0=lg[:, :, 4:36].rearrange("p t (a b) -> p t a b", b=8),
                                                in1=pen.unsqueeze(3).to_broadcast([128, TPS, 4, 8]), op=ALU.add),
                      reads=R("lg", "pen"), writes=R("elm"))
                p.dve(lambda e: e.reduce_max(out=s4(3), in_=elm, axis=AX.X), reads=R("elm"), writes=R("m1"))
                p.dve(lambda e: e.tensor_tensor(out=oh1f, in0=elm, in1=bc3(s4(3), 32), op=ALU.is_equal), reads=R("elm", "m1"), writes=R("oh1"))
                p.dve(lambda e: e.scalar_tensor_tensor(out=elm2, in0=oh1f, scalar=-BIG, in1=elm, op0=ALU.mult, op1=ALU.add),
                      reads=R("elm", "oh1"), writes=R("elm2"))
                p.dve(lambda e: e.reduce_max(out=s4(4), in_=elm2, axis=AX.X), reads=R("elm2"), writes=R("m2"))
                p.dve(lambda e: e.tensor_tensor(out=oh2f, in0=elm2, in1=bc3(s4(4), 32), op=ALU.is_equal), reads=R("elm2", "m2"), writes=R("oh2"))
                p.dve(lambda e: e.tensor_tensor(out=s4(5), in0=s4(3), in1=s4(4), op=ALU.subtract), reads=R("m1", "m2"), writes=R("d12"))
                p.act(lambda e: e.activation(out=s4(6), in_=s4(5), func=AF.Sigmoid), reads=R("d12"), writes=R("p1"))
                p.act(lambda e: e.activation(out=s4(7), in_=s4(5), func=AF.Sigmoid, scale=-1.0), reads=R("d12"), writes=R("p2"))
                p.dve(lambda e: e.tensor_tensor(out=gts[:, t0:t0 + TPS, 0], in0=s4(6), in1=s4(2), op=ALU.mult), reads=R("p1", "ggate"), writes=[("gts", t0, 0)])
                p.dve(lambda e: e.tensor_tensor(out=gts[:, t0:t0 + TPS, 1], in0=s4(7), in1=s4(2), op=ALU.mult), reads=R("p2", "ggate"), writes=[("gts", t0, 1)])
                p.dve(lambda e: e.tensor_copy(out=ohs[:, t0:t0 + TPS, 0, :], in_=oh1f), reads=R("oh1"), writes=[("ohs", t0, 0)])
                p.dve(lambda e: e.tensor_copy(out=ohs[:, t0:t0 + TPS, 1, :], in_=oh2f), reads=R("oh2"), writes=[("ohs", t0, 1)])
                p.dve(lambda e: e.tensor_tensor(out=ohsum[:, t0:t0 + TPS, :], in0=oh1f, in1=oh2f, op=ALU.add), reads=R("oh1", "oh2"), writes=[("ohsum", t0)])
            return EW, EP

        for _ in stF(0):
            pass
        for sg in range(NSEG):
            stM(sg)
            gen = stF(sg + 1) if sg + 1 < NSEG else iter(())

            def pump(n, gen=gen):
                for _ in range(n):
                    next(gen, None)
            EW, EP = stE(sg)

            def after_tile(j, EW=EW, EP=EP):
                EW(j)
                if j >= 1:
                    EP(j - 1)
            stK(sg, pump, after_tile)
            EP(TPS - 1)
            for _ in gen:
                pass
            pump_zero((NB + NSEG - 1) // NSEG)

        pump_zero(NB)
        p.barrier()
        arena.reset()
        cnt = A([128, 32], F32)
        cmpJ = A([128, 32, JMAX], F32)
        nbk = A([128, 32], F32)
        cs_a = A([128, 32], F32)
        cs_b = A([128, 32], F32)
        pstart = A([128, 32], F32)
        cmpB = A([128, NB, 32], F32)
        bef = A([128, NB], F32)
        widxf = A([128, NB, 4], F32)
        gidxf = A([128, NB], F32)
        pos_r = Rot("pos", [A([128, 96], F32) for _ in range(2)])
        hsb_r = Rot("hsb", [A([128, D], BF16) for _ in range(3)])
        OHSUM = [("ohsum", i) for i in range(0, NT, TPS)]
        bk, bkk = nbank()

        def mmcnt(e, bk=bk):
            for i in range(NT):
                ins = e.matmul(bk[:, 0:32], lhsT=onesb[:], rhs=ohsum[:, i, :], start=(i == 0), stop=(i == NT - 1))
            return ins
        p.pe(mmcnt, reads=OHSUM + ["onesb"], writes=[bkk])
        p.dve((lambda bk: lambda e: e.tensor_copy(out=cnt, in_=bk[:, 0:32]))(bk), reads=[bkk], writes=["cnt"])
        p.dve(lambda e: e.tensor_tensor(out=cmpJ, in0=cnt.unsqueeze(2).to_broadcast([128, 32, JMAX]),
                                        in1=thr_bc.unsqueeze(1).to_broadcast([128, 32, JMAX]), op=ALU.is_gt),
              reads=["cnt", "rowb"], writes=["cmpJ"])
        p.dve(lambda e: e.reduce_sum(out=nbk, in_=cmpJ, axis=AX.X), reads=["cmpJ"], writes=["nbk"])
        p.dve(lambda e: e.tensor_scalar(out=nbk, in0=nbk, scalar1=float(B), scalar2=None, op0=ALU.mult), reads=["nbk"], writes=["padded"])
        src, srck = nbk, "padded"
        bufs = [(cs_a, "cs_a"), (cs_b, "cs_b")]
        for li, sh in enumerate((1, 2, 4, 8, 16)):
            dstt, dk = bufs[li % 2]
            p.dve((lambda dstt, src, sh: lambda e: e.tensor_copy(out=dstt[:, 0:sh], in_=src[:, 0:sh]))(dstt, src, sh), reads=[srck], writes=[(dk, 0)])
            p.dve((lambda dstt, src, sh: lambda e: e.tensor_tensor(out=dstt[:, sh:32], in0=src[:, sh:32], in1=src[:, 0:32 - sh], op=ALU.add))(dstt, src, sh),
                  reads=[srck], writes=[(dk, 1)])
            p.dve((lambda dstt: lambda e: e.tensor_copy(out=dstt[:, 0:1], in_=dstt[:, 0:1]))(dstt), reads=[(dk, 0), (dk, 1)], writes=[dk])
            src, srck = dstt, dk
        pend, pendk = src, srck
        p.dve(lambda e: e.tensor_tensor(out=pstart, in0=pend, in1=nbk, op=ALU.subtract), reads=[pendk, "padded"], writes=["pstart"])
        p.dve(lambda e: e.tensor_tensor(out=cmpB, in0=pend.unsqueeze(1).to_broadcast([128, NB, 32]),
                                        in1=bpos_bc.unsqueeze(2).to_broadcast([128, NB, 32]), op=ALU.is_le),
              reads=[pendk, "rowb"], writes=["cmpB"])
        p.dve(lambda e: e.reduce_sum(out=bef, in_=cmpB, axis=AX.X), reads=["cmpB"], writes=["bef0"])
        p.dve(lambda e: e.tensor_scalar(out=gidxf, in0=bef, scalar1=128.0, scalar2=None, op0=ALU.mult), reads=["bef0"], writes=["gidxf0"])
        p.dve(lambda e: e.tensor_tensor(out=gidxf, in0=gidxf, in1=rowoff[:, 0:1].to_broadcast([128, NB]), op=ALU.add),
              reads=["gidxf0", "rowoff"], writes=["gidxf"])
        p.dve(lambda e: e.tensor_copy(out=widx[:], in_=gidxf), reads=["gidxf"], writes=["widx"])
        p.dve(lambda e: e.tensor_scalar(out=gidxf, in0=gidxf, scalar1=float(NE * 128 - 1), scalar2=None, op0=ALU.min),
              reads=["gidxf", "widx"], writes=["gidxc"])
        p.dve(lambda e: e.tensor_copy(out=widxc[:], in_=gidxf), reads=["gidxc"], writes=["widxc"])
        wg_r = Rot("wg", [A([128, 8, 512], BF16) for _ in range(2)])
        wu_r = Rot("wu", [A([128, 8, 512], BF16) for _ in range(2)])
        wd_r = Rot("wd", [A([128, 4, D], BF16) for _ in range(2)])
        NSKIP = 15

        def gather_kw(b):
            if b >= NB - NSKIP:
                return dict(in_offset=bass.IndirectOffsetOnAxis(ap=widx[:, b:b + 1], axis=0), bounds_check=NE * 128 - 1, oob_is_err=False)
            return dict(in_offset=bass.IndirectOffsetOnAxis(ap=widxc[:, b:b + 1], axis=0))
        order = []
        lo, hi = 0, NB - 1
        while lo <= hi:
            order.append(lo); lo += 1
            if lo <= hi:
                order.append(lo); lo += 1
            if lo <= hi and hi >= NB - NSKIP:
                order.append(hi); hi -= 1
        assert sorted(order) == list(range(NB))
        nxt = {order[i]: (order[i + 2] if i + 2 < NB else None) for i in range(NB)}
        wbufs = {}

        def issue_gathers(b):
            wg_, wgk = wg_r.next()
            wu_, wuk = wu_r.next()
            wd_, wdk = wd_r.next()
            p.dma("pool", (lambda wg_, b: lambda e: e.indirect_dma_start(
                out=wg_.rearrange("p a b -> p (a b)"), out_offset=None, in_=wgate_d,
                **gather_kw(b)))(wg_, b),
                reads=["widx", "widxc"], writes=[(wgk, k) for k in range(8)])
            p.dma("pool", (lambda wu_, b: lambda e: e.indirect_dma_start(
                out=wu_.rearrange("p a b -> p (a b)"), out_offset=None, in_=wup_d,
                **gather_kw(b)))(wu_, b),
                reads=["widx", "widxc"], writes=[(wuk, k) for k in range(8)])
            p.dma("pool", (lambda wd_, b: lambda e: e.indirect_dma_start(
                out=wd_.rearrange("p a b -> p (a b)"), out_offset=None, in_=wdown_d,
                **gather_kw(b)))(wd_, b),
                reads=["widx", "widxc"], writes=[(wdk, k) for k in range(4)])
            wbufs[b] = (wg_, wgk, wu_, wuk, wd_, wdk)
        issue_gathers(order[0])
        issue_gathers(order[1])
        for i in range(NT):
            bk, bkk = nbank()

            def mmrk(e, bk=bk, i=i):
                ins = e.matmul(bk[:, 0:32], lhsT=striub[:], rhs=ohsum[:, i, :], start=True, stop=(i == 0))
                for j2 in range(i):
                    ins = e.matmul(bk[:, 0:32], lhsT=onesb[:], rhs=ohsum[:, j2, :], start=False, stop=(j2 == i - 1))
                return ins
            p.pe(mmrk, reads=OHSUM + ["onesb", "striub"], writes=[bkk])
            ps, psk = pos_r.next()
            p.dve((lambda bk, ps: lambda e: e.tensor_tensor(out=ps[:, 0:32], in0=bk[:, 0:32], in1=pstart, op=ALU.add))(bk, ps),
                  reads=[bkk, "pstart"], writes=[(psk, 0)])
            p.dve((lambda ps, i: lambda e: e.tensor_tensor(out=ps[:, 32:96].rearrange("p (a b) -> p a b", b=32), in0=ohs[:, i, :, :],
                                                           in1=ps[:, 0:32].unsqueeze(1).to_broadcast([128, 2, 32]), op=ALU.mult))(ps, i),
                  reads=[(psk, 0), ("ohs", i // TPS * TPS, 0), ("ohs", i // TPS * TPS, 1)], writes=[(psk, 1)])
            p.dve((lambda ps, i: lambda e: e.reduce_sum(out=destf[:, i, :], in_=ps[:, 32:96].rearrange("p (a b) -> p a b", b=32), axis=AX.X))(ps, i),
                  reads=[(psk, 1)], writes=[("destf", i)])
        p.dve(lambda e: e.tensor_copy(out=desti[:], in_=destf[:]), reads=[("destf", i) for i in range(NT)], writes=["desti"])
        dump("destf", destf[:], "desti")
        HSZ = [("Hs", "z", n, a_) for n in range(nz) for a_ in range(BT)]
        for i in range(NT):
            hb, hbk = hsb_r.next()
            p.dma("sp", (lambda hb, i: lambda e: e.dma_start(out=hb, in_=H2_v[i]))(hb, i), reads=[("H2", i)], writes=[hbk])
            for k in range(2):
                p.dma("pool", (lambda hb, i, k: lambda e: e.indirect_dma_start(
                    out=Hs_d, out_offset=bass.IndirectOffsetOnAxis(ap=desti[:, i, k:k + 1], axis=0), in_=hb, in_offset=None))(hb, i, k),
                    reads=[hbk, "desti"] + HSZ, writes=[("Hs", "s", i, k)])
        HSALL = [("Hs", "s", i, k) for i in range(NT) for k in range(2)]

        hsl_r = Rot("hsl", [A([128, BT, D], BF16) for _ in range(2)])
        hT_r = Rot("hT", [A([128, 8, B], BF16) for _ in range(2)])
        aT_r = Rot("aT", [A([128, 4, B], BF16) for _ in range(2)])
        sg_r = Rot("sg", [A([128, B], F32) for _ in range(2)])
        ysb_r = Rot("ysb", [A([128, D], F32) for _ in range(3)])
        print("phase3 arena used", arena.off)
        Hs_b = Hs_d.rearrange("(n a p) c -> n p a c", p=128, a=BT)
        Y_v = Y_d.rearrange("(n p) c -> n p c", p=128)
        hsl_bufs = {}

        def load_hsl(b):
            hsl, hslk = hsl_r.next()
            p.dma("sp", (lambda hsl, b: lambda e: e.dma_start(out=hsl, in_=Hs_b[b]))(hsl, b), reads=HSALL + HSZ, writes=[hslk])
            hsl_bufs[b] = (hsl, hslk)
        load_hsl(order[0])
        load_hsl(order[1])
        for b in order:
            if b not in wbufs:
                issue_gathers(b)
            wg_, wgk, wu_, wuk, wd_, wdk = wbufs[b]
            WG = [(wgk, k) for k in range(8)]
            WU = [(wuk, k) for k in range(8)]
            WD = [(wdk, k) for k in range(4)]
            hsl, hslk = hsl_bufs[b]
            hT, hTk = hT_r.next()
            for a in range(BT):
                bk, bkk = nbank()
                bkb = bk[:].bitcast(BF16).rearrange("p (a b) -> p a b", b=128)

                def tr3(e, hsl=hsl, bkb=bkb, a=a):
                    for k in range(8):
                        ins = e.transpose(out=bkb[:, k, :], in_=hsl[:, a, :].rearrange("s (p k) -> s k p", k=8)[:, k, :], identity=identb[:])
                    return ins
                p.pe(tr3, reads=[hslk, "identb"], writes=[bkk])
                eng = "act" if a % 2 == 0 else "dve"
                if eng == "act":
                    p.act((lambda hT, bkb, a: lambda e: e.copy(out=hT[:, :, a * 128:(a + 1) * 128], in_=bkb))(hT, bkb, a),
                          reads=[bkk], writes=[(hTk, a)])
                else:
                    p.dve((lambda hT, bkb, a: lambda e: e.tensor_copy(out=hT[:, :, a * 128:(a + 1) * 128], in_=bkb))(hT, bkb, a),
                          reads=[bkk], writes=[(hTk, a)])
            HT = [(hTk, a) for a in range(BT)]
            if nxt[b] is not None:
                load_hsl(nxt[b])
            aT, aTk = aT_r.next()
            for hc in range(4):
                bkG, bkGk = nbank()
                bkU, bkUk = nbank()

                def mmg2(e, bkG=bkG, wg_=wg_, hT=hT, hc=hc):
                    for k in range(8):
                        ins = e.matmul(bkG[:, 0:B], lhsT=wg_[:, k, :].rearrange("d (p c) -> d c p", c=4)[:, hc, :], rhs=hT[:, k, :], start=(k == 0), stop=(k == 7))
                    return ins
                p.pe(mmg2, reads=WG + HT, writes=[bkGk])

                def mmu2(e, bkU=bkU, wu_=wu_, hT=hT, hc=hc):
                    for k in range(8):
                        ins = e.matmul(bkU[:, 0:B], lhsT=wu_[:, k, :].rearrange("d (p c) -> d c p", c=4)[:, hc, :], rhs=hT[:, k, :], start=(k == 0), stop=(k == 7))
                    return ins
                p.pe(mmu2, reads=WU + HT, writes=[bkUk])
                sgt, sgk = sg_r.next()
                p.act((lambda sgt, bkG: lambda e: e.activation(out=sgt, in_=bkG[:, 0:B], func=AF.Silu))(sgt, bkG), reads=[bkGk], writes=[sgk])
                p.dve((lambda aT, sgt, bkU, hc: lambda e: e.tensor_tensor(out=aT[:, hc, :], in0=bkU[:, 0:B], in1=sgt, op=ALU.mult))(aT, sgt, bkU, hc),
                      reads=[bkUk, sgk], writes=[(aTk, hc)])
            AT = [(aTk, hc) for hc in range(4)]
            for a in range(BT):
                ysb, ysbk = ysb_r.next()
                for half in range(2):
                    bk, bkk = nbank()

                    def mmd(e, bk=bk, aT=aT, wd_=wd_, a=a, half=half):
                        for hc in range(4):
                            ins = e.matmul(bk[:], lhsT=aT[:, hc, a * 128:(a + 1) * 128], rhs=wd_[:, hc, half * 512:(half + 1) * 512],
                                           start=(hc == 0), stop=(hc == 3))
                        return ins
                    p.pe(mmd, reads=AT + WD, writes=[bkk])
                    if half == 0:
                        p.act((lambda ysb, bk: lambda e: e.copy(out=ysb[:, 0:512], in_=bk[:]))(ysb, bk), reads=[bkk], writes=[(ysbk, 0)])
                    else:
                        p.dve((lambda ysb, bk: lambda e: e.tensor_copy(out=ysb[:, 512:1024], in_=bk[:]))(ysb, bk), reads=[bkk], writes=[(ysbk, 1)])
                p.dma("act", (lambda ysb, b, a: lambda e: e.dma_start(out=Y_v[b * BT + a], in_=ysb))(ysb, b, a),
                      reads=[(ysbk, 0), (ysbk, 1)], writes=[("Y", b, a)])
        YALL = [("Y", b, a) for b in range(NB) for a in range(BT)]

        p.barrier()
        arena.reset()
        fgbc = A([128, D], F32)
        y1_r = Rot("y1", [A([128, D], F32) for _ in range(4)])
        y2_r = Rot("y2", [A([128, D], F32) for _ in range(4)])
        xr_r = Rot("xr", [A([128, D], F32) for _ in range(4)])
        ob_r = Rot("ob", [A([128, D], F32) for _ in range(3)])
        sq4 = A([128, D], BF16)
        st4 = A([128, 2 * NT], F32)
        p.dma("sp", lambda e: e.dma_start(out=fgbc, in_=fg_d.partition_broadcast(128)), writes=["fgbc"])
        out_v = out_d.rearrange("(n p) c -> n p c", p=128)
        for i in range(NT):
            y1, y1k = y1_r.next()
            y2, y2k = y2_r.next()
            xr, xrk = xr_r.next()
            ob, obk = ob_r.next()
            p.dma("pool", (lambda y1, i: lambda e: e.indirect_dma_start(
                out=y1, out_offset=None, in_=Y_d, in_offset=bass.IndirectOffsetOnAxis(ap=desti[:, i, 0:1], axis=0)))(y1, i),
                reads=["desti"] + YALL, writes=[y1k])
            p.dma("pool", (lambda y2, i: lambda e: e.indirect_dma_start(
                out=y2, out_offset=None, in_=Y_d, in_offset=bass.IndirectOffsetOnAxis(ap=desti[:, i, 1:2], axis=0)))(y2, i),
                reads=["desti"] + YALL, writes=[y2k])
            p.dma("sp", (lambda xr, i: lambda e: e.dma_start(out=xr, in_=X1_v[i]))(xr, i), reads=[("X1", i)], writes=[xrk])
            p.dve((lambda y1, i: lambda e: e.tensor_scalar(out=y1, in0=y1, scalar1=gts[:, i, 0:1], scalar2=None, op0=ALU.mult))(y1, i),
                  reads=[y1k, ("gts", i // TPS * TPS, 0)], writes=[y1k])
            p.dve((lambda y1, y2, i: lambda e: e.scalar_tensor_tensor(out=y1, in0=y2, scalar=gts[:, i, 1:2], in1=y1, op0=ALU.mult, op1=ALU.add))(y1, y2, i),
                  reads=[y1k, y2k, ("gts", i // TPS * TPS, 1)], writes=[y1k])
            p.dve((lambda y1: lambda e: e.tensor_tensor(out=y1, in0=y1, in1=gate_f_bc, op=ALU.mult))(y1), reads=[y1k], writes=[y1k])
            p.dve((lambda y1, xr: lambda e: e.tensor_tensor(out=xr, in0=y1, in1=xr, op=ALU.add))(y1, xr), reads=[y1k, xrk], writes=[xrk])
            p.act((lambda xr, i: lambda e: e.activation(out=sq4, in_=xr, func=AF.Square, accum_out=st4[:, i:i + 1]))(xr, i),
                  reads=[xrk], writes=["sq4", ("st4a", i)])
            p.act((lambda i: lambda e: e.activation(out=st4[:, NT + i:NT + i + 1], in_=st4[:, i:i + 1], func=AF.Sqrt, scale=1.0 / D, bias=EPS))(i),
                  reads=[("st4a", i)], writes=[("st4b", i)])
            p.dve((lambda i: lambda e: e.reciprocal(out=st4[:, NT + i:NT + i + 1], in_=st4[:, NT + i:NT + i + 1]))(i),
                  reads=[("st4b", i)], writes=[("st4c", i)])
            p.dve((lambda ob, xr, i: lambda e: e.scalar_tensor_tensor(out=ob, in0=xr, scalar=st4[:, NT + i:NT + i + 1], in1=fgbc,
                                                                     op0=ALU.mult, op1=ALU.mult))(ob, xr, i),
                  reads=[xrk, ("st4c", i), "fgbc"], writes=[obk])
            p.dma("act", (lambda ob, i: lambda e: e.dma_start(out=out_v[i], in_=ob))(ob, i), reads=[obk], writes=[("out", i)])
        p.finish()
        p.emit(es)
        print("ops per engine", p.stats)
    return nc, dbg_out


def _host_consts():
    ident = np.eye(128, dtype=np.float32)
    tri = np.triu(np.ones((128, 128), np.float32))
    rowoff = (np.arange(8)[None, :] * 128 + np.arange(128)[:, None]).astype(np.float32)
    inv0 = np.zeros((4, 16), np.float32)
    for g in range(4):
        w = 2 ** (g + 1)
        inv0[g] = 1.0 / np.minimum(np.arange(16) + 1, w)
    iota = np.arange(32, dtype=np.float32)
    thr = (np.arange(JMAX) * B).astype(np.float32)
    bpos = (np.arange(NB) * B).astype(np.float32)
    return ident, tri, rowoff, inv0, iota, thr, bpos


_CACHE = {}


def _get_nc(debug=False):
    if debug not in _CACHE:
        _CACHE[debug] = build(debug)
    return _CACHE[debug]


def make_in_maps(inp):
    f = lambda a: np.ascontiguousarray(np.asarray(a, dtype=np.float32))
    ident, tri, rowoff, inv0, iota, thr, bpos = _host_consts()
    x = f(inp["x"]); c = f(inp["c"])
    colv = lambda v: v.reshape(-1, 128).T
    colp = np.concatenate([
        colv(f(inp["norm1_g"])[0]),
        f(inp["conv_w"])[0].T.reshape(4, 128, 4).transpose(1, 0, 2).reshape(128, 16),
        colv(f(inp["conv_b"])[0]), colv(f(inp["mlstm_skip"])[0]),
        colv(f(inp["b_pool"])[0]), colv(f(inp["pool_scale"])[0])], axis=1).astype(np.float32)
    rowp = np.concatenate([
        f(inp["mlstm_norm_g"])[0], f(inp["b_router_group"])[0], f(inp["b_router_expert"])[0],
        f(inp["b_igate"])[0], f(inp["b_fgate"])[0], inv0.reshape(-1), iota, thr, bpos])[None, :].astype(np.float32)
    w_r = np.concatenate([f(inp["w_router_group"])[0], f(inp["w_router_expert"])[0]], axis=1)
    shared = dict(
        ada_w=f(inp["ada_w"])[0], ada_b=f(inp["ada_b"]), w_in=f(inp["w_in"])[0], w_q=f(inp["w_q"])[0], w_k=f(inp["w_k"])[0],
        w_pool=f(inp["w_pool"])[0], w_out=f(inp["w_out"])[0], w_r=np.ascontiguousarray(w_r),
        w_gate=f(inp["w_expert_gate"])[0].reshape(NE * 128, 8 * 512), w_up=f(inp["w_expert_up"])[0].reshape(NE * 128, 8 * 512),
        w_down=f(inp["w_expert_down"])[0].reshape(NE * 128, 4 * D),
        colp=np.ascontiguousarray(colp), rowp=np.ascontiguousarray(rowp), norm2_g=f(inp["norm2_g"]),
        final_g=f(inp["final_g"]).reshape(1, D), ident=ident, tri=tri, rowoff=rowoff)
    maps = []
    for b in range(8):
        m = dict(shared)
        m["x"] = np.ascontiguousarray(x[b])
        m["ccol"] = np.ascontiguousarray(c[b].reshape(8, 128).T)
        maps.append(m)
    return maps


def kernel(**inputs):
    nc, _ = _get_nc(False)
    in_maps = make_in_maps(inputs)
    res = run_bass_kernel_spmd(nc, in_maps, core_ids=list(range(8)))
    out = np.stack([np.asarray(r["out"], dtype=np.float32) for r in res.results], axis=0)
    return out
```

```python
import numpy as np
from contextlib import ExitStack
import concourse.bass as bass
import concourse.mybir as mybir
from concourse.bass_utils import run_bass_kernel_spmd

F32 = mybir.dt.float32
BF16 = mybir.dt.bfloat16
I32 = mybir.dt.int32
AF = mybir.ActivationFunctionType
ALU = mybir.AluOpType
AX = mybir.AxisListType

T = 4096
D = 1024
NT = T // 128
SEG = 512
NSEG = T // SEG
TPS = SEG // 128
INC = 2056
COL_U, COL_V, COL_O, COL_I, COL_F, COL_P = 0, 512, 1024, 1536, 1540, 1544
NE = 32
B = 384
BT = B // 128
NB = (2 * T) // B + NE
NSLOT = NB * B
JMAX = (T + B - 1) // B + 1
EPS = 1e-6
BIG = 30000.0

ENGS = ("pe", "act", "dve", "pool", "sp")
DMAQ = ("sp", "act", "pool")
NDSEM = 16


class Op:
    __slots__ = ("eng", "fn", "reads", "writes", "dma", "deps", "sig", "sem", "val", "name")

    def __init__(self, eng, fn, reads, writes, dma, name):
        self.eng, self.fn, self.reads, self.writes, self.dma, self.name = eng, fn, reads, writes, dma, name
        self.deps = []
        self.sig = False
        self.sem = None
        self.val = 0


class Prog:
    def __init__(self, nc):
        self.nc = nc
        self.ops = []
        self.last_w = {}
        self.readers = {}

    def op(self, eng, fn, reads=(), writes=(), dma=False, name=""):
        o = Op(eng, fn, tuple(reads), tuple(writes), dma, name)
        deps = set()
        for r in o.reads:
            w = self.last_w.get(r)
            if w is not None:
                deps.add(w)
        for w_ in o.writes:
            w = self.last_w.get(w_)
            if w is not None:
                deps.add(w)
            for rd in self.readers.get(w_, ()):
                deps.add(rd)
        for d in deps:
            if d is o:
                continue
            if d.eng == o.eng and not d.dma and not o.dma:
                if o.eng == "pe":
                    continue
                if not any(self.last_w.get(r) is d for r in o.reads):
                    continue
            o.deps.append(d)
            d.sig = True
        for w_ in o.writes:
            self.last_w[w_] = o
            self.readers[w_] = []
        for r in o.reads:
            if r not in o.writes:
                self.readers.setdefault(r, []).append(o)
        self.ops.append(o)
        return o

    def pe(self, fn, reads=(), writes=(), name=""):
        return self.op("pe", fn, reads, writes, name=name)

    def act(self, fn, reads=(), writes=(), name=""):
        return self.op("act", fn, reads, writes, name=name)

    def dve(self, fn, reads=(), writes=(), name=""):
        return self.op("dve", fn, reads, writes, name=name)

    def pool(self, fn, reads=(), writes=(), name=""):
        return self.op("pool", fn, reads, writes, name=name)

    def ve(self, eng, fn, reads=(), writes=(), name=""):
        return self.op(eng, fn, reads, writes, name=name)

    def dma(self, q, fn, reads=(), writes=(), name=""):
        return self.op(q, fn, reads, writes, dma=True, name=name)

    def _sync_all(self, engines):
        last = {}
        dmas = {q: [] for q in DMAQ}
        for o in self.ops:
            if o.fn is None:
                continue
            if o.dma:
                dmas[o.eng].append(o)
            else:
                last[o.eng] = o
        deps = list(last.values())
        for q in DMAQ:
            deps += dmas[q][-NDSEM:]
        for e in engines:
            o = Op(e, None, (), (), False, "sync_all")
            for d in deps:
                if d.eng == e and not d.dma:
                    continue
                o.deps.append(d)
                d.sig = True
            self.ops.append(o)

    def barrier(self):
        self._sync_all(ENGS)

    def finish(self):
        self._sync_all(("sp",))

    def emit(self, es):
        nc = self.nc
        engsem = {e: es.enter_context(nc.semaphore("s_" + e)) for e in ENGS}
        dsem = {e: [es.enter_context(nc.semaphore(f"d_{e}{i}")) for i in range(NDSEM)] for e in DMAQ}
        cnt = {e: 0 for e in ENGS}
        hist = {e: [] for e in DMAQ}
        per_eng = {e: [] for e in ENGS}
        for o in self.ops:
            if o.dma:
                h = hist[o.eng]
                i = len(h)
                o.sem = dsem[o.eng][i % NDSEM]
                o.val = 16 * (i // NDSEM + 1)
                o.sig = True
                if i >= NDSEM and h[i - NDSEM] not in o.deps:
                    o.deps.append(h[i - NDSEM])
                h.append(o)
            elif o.sig and o.fn is not None:
                cnt[o.eng] += 1
                o.sem = engsem[o.eng]
                o.val = cnt[o.eng]
            per_eng[o.eng].append(o)
        block = es.enter_context(nc.Block())
        handles = {"pe": "tensor", "act": "scalar", "dve": "vector", "pool": "gpsimd", "sp": "sync"}

        def make(e):
            def body(eng):
                seen = {}
                for o in per_eng[e]:
                    for d in o.deps:
                        k = d.sem.name
                        if seen.get(k, 0) >= d.val:
                            continue
                        seen[k] = d.val
                        eng.wait_ge(d.sem, d.val)
                    if o.fn is None:
                        continue
                    ins = o.fn(eng)
                    if o.sig:
                        ins.then_inc(o.sem, 16 if o.dma else 1)
            return body

        for e in ENGS:
            getattr(block, handles[e])(make(e))
        self.stats = {e: len(per_eng[e]) for e in ENGS}


class Arena:
    def __init__(self, ap_bf16, nbytes):
        self.ap = ap_bf16
        self.nbytes = nbytes
        self.off = 0

    def reset(self):
        self.off = 0

    def alloc(self, shape, dt):
        esz = {F32: 4, BF16: 2, I32: 4}[dt]
        n = int(np.prod(shape[1:]))
        nb = (n * esz + 31) // 32 * 32
        assert self.off + nb <= self.nbytes, f"arena overflow {self.off + nb} > {self.nbytes}"
        v = self.ap[:, self.off // 2:(self.off + n * esz) // 2]
        self.off += nb
        if dt != BF16:
            v = v.bitcast(dt)
        if len(shape) == 3:
            v = v.rearrange("p (a b) -> p a b", b=shape[2])
        elif len(shape) == 4:
            v = v.rearrange("p (a b c) -> p a b c", b=shape[2], c=shape[3])
        return v


class Rot:
    def __init__(self, name, aps):
        self.name, self.aps, self.i = name, aps, 0

    def next(self):
        k = self.i % len(self.aps)
        self.i += 1
        return self.aps[k], (self.name, k)


def build(debug=False):
    nc = bass.Bass("TRN2", target_bir_lowering=False)
    dbg_out = {}

    def DT(name, shape, dt, kind="ExternalInput"):
        return nc.dram_tensor(name, list(shape), dt, kind=kind).ap()

    x_d = DT("x", [T, D], F32)
    ccol_d = DT("ccol", [128, 8], F32)
    adaw_d = DT("ada_w", [D, 6 * D], F32)
    adab_d = DT("ada_b", [1, 6 * D], F32)
    win_d = DT("w_in", [D, INC], F32)
    wq_d = DT("w_q", [4, 128, 128], F32)
    wk_d = DT("w_k", [4, 128, 128], F32)
    wpool_d = DT("w_pool", [4, 128, 128], F32)
    wout_d = DT("w_out", [D, D], F32)
    wr_d = DT("w_r", [D, 36], F32)
    wgate_d = DT("w_gate", [NE * 128, 8 * 512], F32)
    wup_d = DT("w_up", [NE * 128, 8 * 512], F32)
    wdown_d = DT("w_down", [NE * 128, 4 * D], F32)
    colp_d = DT("colp", [128, 40], F32)
    NROW = 512 + 36 + 8 + 64 + 32 + JMAX + NB
    rowp_d = DT("rowp", [1, NROW], F32)
    g2_d = DT("norm2_g", [1, D], F32)
    fg_d = DT("final_g", [1, D], F32)
    ident_d = DT("ident", [128, 128], F32)
    tri_d = DT("tri", [128, 128], F32)
    rowoff_d = DT("rowoff", [128, 8], F32)
    out_d = DT("out", [T, D], F32, "ExternalOutput")
    scr = "ExternalOutput"
    X1_d = DT("X1", [T, D], F32, scr)
    H2_d = DT("H2", [T, D], BF16, scr)
    Hs_d = DT("Hs", [NSLOT, D], BF16, "Internal")
    Y_d = DT("Y", [NSLOT, D], F32, "Internal")

    es = ExitStack()
    with es:
        def S(name, shape, dt):
            return es.enter_context(nc.sbuf_tensor("s_" + name, list(shape), dt))

        p = Prog(nc)
        banks = [es.enter_context(nc.psum_tensor(f"bank{i}", [128, 512], F32)) for i in range(8)]
        bank_i = [0]

        def nbank():
            k = bank_i[0] % 5
            bank_i[0] += 1
            return banks[k], ("bank", k)

        obank_i = [0]

        def obank():
            k = 5 + obank_i[0] % 2
            obank_i[0] += 1
            return banks[k], ("bank", k)

        ident = S("ident", [128, 128], F32)
        identb = S("identb", [128, 128], BF16)
        tri = S("tri", [128, 128], F32)
        trib = S("trib", [128, 128], BF16)
        striub = S("striub", [128, 128], BF16)
        onesf = S("onesf", [128, 128], F32)
        onesb = S("onesb", [128, 128], BF16)
        colp = S("colp", [128, 40], F32)
        rowb = S("rowb", [128, NROW], F32)
        rowoff = S("rowoff", [128, 8], F32)
        modB = S("modB", [128, 4 * D], F32)
        s2bc = S("s2bc", [128, D], F32)
        wi = S("wi", [128, 8, INC], BF16)
        wo = S("wo", [128, 8, D], BF16)
        wqk = S("wqk", [128, 2, 4, 128], BF16)
        wpl = S("wpl", [128, 4, 128], BF16)
        wr = S("wr", [128, 8, 36], BF16)
        biasbc = S("biasbc", [128, 520], F32)
        biascol = S("biascol", [128, 8], F32)
        bpscol = S("bpscol", [128, 4], F32)
        s1col = S("s1col", [128, 8], F32)
        Cst = S("Cst", [128, 4, 129], F32)
        ohs = S("ohs", [128, NT, 2, 32], BF16)
        ohsum = S("ohsum", [128, NT, 32], BF16)
        gts = S("gts", [128, NT, 2], F32)
        destf = S("destf", [128, NT, 2], F32)
        desti = S("desti", [128, NT, 2], I32)
        widx = S("widx", [128, NB], I32)
        widxc = S("widxc", [128, NB], I32)
        ztile = S("ztile", [128, D], BF16)
        ARENA_BYTES = 119808
        arena_t = S("arena", [128, ARENA_BYTES // 2], BF16)
        arena = Arena(arena_t, ARENA_BYTES)

        g1col = colp[:, 0:8]
        convw = colp[:, 8:24]
        convb = colp[:, 24:28]
        skipc = colp[:, 28:32]
        bpoolc = colp[:, 32:36]
        pscalec = colp[:, 36:40]
        r0 = 0
        normg_bc = rowb[:, r0:r0 + 512]; r0 += 512
        br_bc = rowb[:, r0:r0 + 36]; r0 += 36
        bif_bc = rowb[:, r0:r0 + 8]; r0 += 8
        inv0_bc = rowb[:, r0:r0 + 64]; r0 += 64
        iota_bc = rowb[:, r0:r0 + 32]; r0 += 32
        thr_bc = rowb[:, r0:r0 + JMAX]; r0 += JMAX
        bpos_bc = rowb[:, r0:r0 + NB]; r0 += NB
        gate_a_bc = modB[:, 0:D]
        shift_f_bc = modB[:, D:2 * D]
        scale_f_bc = modB[:, 2 * D:3 * D]
        gate_f_bc = modB[:, 3 * D:4 * D]

        def dump(name, ap, key, dt=F32):
            if not debug:
                return
            shp = list(ap.shape)
            t = DT("dbg_" + name, shp, dt, "ExternalOutput")
            dbg_out["dbg_" + name] = shp
            p.dma("sp", lambda e: e.dma_start(out=t, in_=ap), reads=[key], name="dump")

        p.dma("sp", lambda e: e.dma_start(out=ident[:], in_=ident_d), writes=["ident"])
        p.dma("sp", lambda e: e.dma_start(out=tri[:], in_=tri_d), writes=["tri"])
        p.dma("sp", lambda e: e.dma_start(out=colp[:], in_=colp_d), writes=["colp"])
        p.dma("sp", lambda e: e.dma_start(out=rowb[:], in_=rowp_d.partition_broadcast(128)), writes=["rowb"])
        p.dma("sp", lambda e: e.dma_start(out=rowoff[:], in_=rowoff_d), writes=["rowoff"])
        p.dve(lambda e: e.tensor_copy(out=identb[:], in_=ident[:]), reads=["ident"], writes=["identb"])
        p.dve(lambda e: e.tensor_copy(out=trib[:], in_=tri[:]), reads=["tri"], writes=["trib"])
        p.dve(lambda e: e.tensor_tensor(out=striub[:], in0=tri[:], in1=ident[:], op=ALU.subtract),
              reads=["tri", "ident"], writes=["striub"])
        p.dve(lambda e: e.memset(onesf[:], 1.0), writes=["onesf"])
        p.dve(lambda e: e.memset(onesb[:], 1.0), writes=["onesb"])
        p.dve(lambda e: e.memset(Cst[:], 0.0), writes=[("C", h) for h in range(4)])

        p.pool(lambda e: e.memset(ztile[:], 0.0), writes=["ztile"])
        hs_v = Hs_d.rearrange("(n a p) c -> n p a c", p=128, a=BT)
        nz = NB
        rem = 0
        zero_todo = list(range(nz))

        def pump_zero(n):
            for _ in range(n):
                if not zero_todo:
                    return
                nn = zero_todo.pop(0)
                for a_ in range(BT):
                    p.dma("pool", (lambda nn, a_: lambda e: e.dma_start(out=hs_v[nn][:, a_, :], in_=ztile[:]))(nn, a_),
                          reads=["ztile"], writes=[("Hs", "z", nn, a_)])

        modA = arena.alloc([128, 2 * D], F32)
        shift_a_bc = modA[:, 0:D]
        scale_a_bc = modA[:, D:2 * D]

        def modslice(j):
            return modA[:, j * 512:(j + 1) * 512] if j < 4 else modB[:, (j - 4) * 512:(j - 3) * 512]
        cc = arena.alloc([128, 8], F32)
        scb = arena.alloc([128, 8], BF16)
        screp = arena.alloc([128, 8, 128], BF16)
        p.dma("sp", lambda e: e.dma_start(out=cc, in_=ccol_d), writes=["cc"])
        p.act(lambda e: e.activation(out=scb, in_=cc, func=AF.Silu), reads=["cc"], writes=["scb"])
        p.dve(lambda e: e.tensor_copy(out=screp, in_=scb.unsqueeze(2).to_broadcast([128, 8, 128])),
              reads=["scb"], writes=["screp"])
        wa_r = Rot("wa", [arena.alloc([128, 8, 512], BF16) for _ in range(2)])
        ab_r = Rot("ab", [arena.alloc([128, 512], F32) for _ in range(2)])
        adaw_v = adaw_d.rearrange("(k p) n -> p k n", p=128)
        for j in range(12):
            wa, wak = wa_r.next()
            ab, abk = ab_r.next()
            p.dma("pool", (lambda wa, j: lambda e: e.dma_start(out=wa, in_=adaw_v[:, :, j * 512:(j + 1) * 512]))(wa, j),
                  writes=[wak])
            p.dma("sp", (lambda ab, j: lambda e: e.dma_start(
                out=ab, in_=adab_d[0:1, j * 512:(j + 1) * 512].partition_broadcast(128)))(ab, j), writes=[abk])
            bk, bkk = nbank()

            def mm(e, wa=wa, bk=bk):
                for k in range(8):
                    ins = e.matmul(bk[:], lhsT=screp[:, k, :], rhs=wa[:, k, :], start=(k == 0), stop=(k == 7))
                return ins
            p.pe(mm, reads=["screp", wak], writes=[bkk])
            p.dve((lambda ab, bk, j: lambda e: e.tensor_tensor(out=modslice(j), in0=bk[:], in1=ab,
                                                               op=ALU.add))(ab, bk, j),
                  reads=[bkk, abk], writes=[("mod", j)])
        MODALL = [("mod", j) for j in range(12)]

        win_v = win_d.rearrange("(k p) n -> p k n", p=128)
        for k in range(8):
            p.dma("pool", (lambda k: lambda e: e.dma_start(out=wi[:, k, :], in_=win_v[:, k, :]))(k), writes=[("wi", k)])
        WIALL = [("wi", k) for k in range(8)]
        wout_v = wout_d.rearrange("(k p) n -> p k n", p=128)
        for k in range(0, 8, 2):
            p.dma("pool", (lambda k: lambda e: e.dma_start(out=wo[:, k:k + 2, :], in_=wout_v[:, k:k + 2, :]))(k),
                  writes=[("wo", k)])
        WOALL = [("wo", k) for k in range(0, 8, 2)]
        WOSC = True
        p.dma("pool", lambda e: e.dma_start(out=wqk[:, 0, :, :], in_=wq_d.rearrange("h d e -> d h e")), writes=["wq"])
        p.dma("pool", lambda e: e.dma_start(out=wqk[:, 1, :, :], in_=wk_d.rearrange("h d e -> d h e")), writes=["wk"])
        p.dma("pool", lambda e: e.dma_start(out=wpl[:], in_=wpool_d.rearrange("g c d -> c g d")), writes=["wpl"])
        p.dma("pool", lambda e: e.dma_start(out=wr[:], in_=wr_d.rearrange("(k p) n -> p k n", p=128)), writes=["wr"])

        tmpA = arena.alloc([128, D], F32)
        tmp3 = arena.alloc([128, 8, 128], F32)
        scl = arena.alloc([128, 8], F32)
        shc = arena.alloc([128, 8], F32)
        shcb = arena.alloc([128, 8], BF16)
        shrep = arena.alloc([128, 8, 128], BF16)
        idb3 = ident[:].unsqueeze(1).to_broadcast([128, 8, 128])
        p.dve(lambda e: e.tensor_tensor(out=tmp3, in0=scale_a_bc.rearrange("p (a b) -> p a b", b=128), in1=idb3, op=ALU.mult),
              reads=MODALL + ["ident"], writes=["tmp3"])
        p.dve(lambda e: e.reduce_sum(out=scl, in_=tmp3, axis=AX.X), reads=["tmp3"], writes=["scl"])
        p.dve(lambda e: e.scalar_tensor_tensor(out=s1col[:], in0=scl, scalar=1.0, in1=g1col, op0=ALU.add, op1=ALU.mult),
              reads=["scl", "colp"], writes=["s1col"])
        p.dve(lambda e: e.tensor_tensor(out=tmp3, in0=shift_a_bc.rearrange("p (a b) -> p a b", b=128), in1=idb3, op=ALU.mult),
              reads=MODALL + ["ident", "scl"], writes=["tmp3"])
        p.dve(lambda e: e.reduce_sum(out=shc, in_=tmp3, axis=AX.X), reads=["tmp3"], writes=["shc"])
        p.dve(lambda e: e.tensor_copy(out=shcb, in_=shc), reads=["shc"], writes=["shcb"])
        p.dve(lambda e: e.tensor_copy(out=shrep, in_=shcb.unsqueeze(2).to_broadcast([128, 8, 128])),
              reads=["shcb"], writes=["shrep"])
        bk, bkk = nbank()

        def mmbv(e, bk=bk):
            for k in range(8):
                ins = e.matmul(bk[:], lhsT=shrep[:, k, :], rhs=wi[:, k, COL_V:COL_V + 512], start=(k == 0), stop=(k == 7))
            return ins
        p.pe(mmbv, reads=["shrep"] + WIALL, writes=[bkk])
        p.dve((lambda bk: lambda e: e.tensor_copy(out=biasbc[:, 0:512], in_=bk[:]))(bk), reads=[bkk], writes=["biasbc_v"])
        bk, bkk = nbank()

        def mmbg(e, bk=bk):
            for k in range(8):
                ins = e.matmul(bk[:, 0:8], lhsT=shrep[:, k, :], rhs=wi[:, k, COL_I:COL_I + 8], start=(k == 0), stop=(k == 7))
            return ins
        p.pe(mmbg, reads=["shrep"] + WIALL, writes=[bkk])
        p.dve((lambda bk: lambda e: e.tensor_tensor(out=biasbc[:, 512:520], in0=bk[:, 0:8], in1=bif_bc, op=ALU.add))(bk),
              reads=[bkk, "rowb"], writes=["biasbc_g"])
        bk, bkk = nbank()

        def mmbc(e, bk=bk):
            for c in range(8):
                c0 = (COL_U if c < 4 else COL_O) + (c % 4) * 128
                for k in range(8):
                    ins = e.matmul(bk[:, c:c + 1], lhsT=wi[:, k, c0:c0 + 128], rhs=shcb[:, k:k + 1], start=(k == 0), stop=(k == 7))
            return ins
        p.pe(mmbc, reads=["shcb"] + WIALL, writes=[bkk])
        p.dve((lambda bk: lambda e: e.tensor_copy(out=biascol[:], in_=bk[:, 0:8]))(bk), reads=[bkk], writes=["biascol"])
        for k in range(8):
            p.dve((lambda k: lambda e: e.tensor_scalar(out=wi[:, k, :], in0=wi[:, k, :], scalar1=s1col[:, k:k + 1], scalar2=None,
                                                       op0=ALU.mult))(k),
                  reads=["s1col", ("wi", k)], writes=[("wi", k)])
        p.dma("sp", lambda e: e.dma_start(out=tmpA, in_=g2_d.partition_broadcast(128)), writes=["tmpA"])
        p.dve(lambda e: e.scalar_tensor_tensor(out=s2bc[:], in0=scale_f_bc, scalar=1.0, in1=tmpA, op0=ALU.add, op1=ALU.mult),
              reads=MODALL + ["tmpA"], writes=["s2bc"])
        p.dve(lambda e: e.tensor_tensor(out=bpscol[:], in0=bpoolc, in1=pscalec, op=ALU.mult), reads=["colp"], writes=["bpscol"])
        for k in range(0, 8, 2):
            for kk in (k, k + 1):
                p.dve((lambda kk: lambda e: e.tensor_tensor(out=wo[:, kk, :], in0=wo[:, kk, :], in1=gate_a_bc, op=ALU.mult))(kk),
                      reads=[("wo", k)] + MODALL, writes=[("wo", k)])
        dump("modB", modB[:], ("mod", 11))
        dump("biasbc", biasbc[:], "biasbc_g")
        dump("biascol", biascol[:], "biascol")

        p.barrier()
        arena.reset()
        A = arena.alloc
        xs_r = Rot("xs", [A([128, D], F32) for _ in range(2)])
        xb_r = Rot("xb", [A([128, D], BF16) for _ in range(2)])
        sqj = A([128, D], BF16)
        xT = A([128, 8, SEG], BF16)
        R1 = A([128, SEG], F32)
        ssq1 = A([128, TPS], F32)
        rstd1 = A([128, TPS], F32)
        diag_r = Rot("diag", [A([128, 128], F32) for _ in range(2)])
        ubuf = A([128, 4, SEG + 3], F32)
        scrA = A([128, 2 * SEG], F32)
        ctmp_r = Rot("ctmp", [scrA[:, 0:SEG], scrA[:, SEG:2 * SEG]])
        CT2 = [("ctmp", 0), ("ctmp", 1)]
        ucT = A([128, 4, SEG], BF16)
        sigoT2 = [A([128, 4, SEG], BF16) for _ in range(2)]
        scrB = A([128, 2 * SEG], F32)
        otmp_r = Rot("otmp", [scrB[:, 0:SEG], scrB[:, SEG:2 * SEG]])
        OT2 = [("otmp", 0), ("otmp", 1)]
        pbuf = A([128, 4, SEG + 16], F32)
        ptmp = [A([128, SEG + 16], F32) for _ in range(2)]
        pooled = A([128, 4, SEG], BF16)
        p16 = A([128, 16], F32)
        yT = A([128, 8, SEG], BF16)
        vaug2 = [A([128, TPS, 4, 129], BF16) for _ in range(2)]
        gat = A([128, TPS, 8], F32)
        gsx = A([128, 8, TPS * 4], F32)
        gtmp = A([128, TPS, 4], F32)
        qT = A([128, 4, SEG], BF16)
        kT = A([128, 4, SEG], BF16)
        ktok = A([128, TPS, 4, 128], BF16)
        wv_r = Rot("wv", [A([128, 129], BF16) for _ in range(4)])
        dst_r = Rot("dst", [A([128, 128], BF16) for _ in range(4)])
        cb_r = Rot("cb", [A([128, 129], BF16) for _ in range(4)])
        sm_r = Rot("sm", [A([128, 8], F32) for _ in range(4)])
        hn_r = Rot("hn", [A([128, 512], BF16) for _ in range(2)])
        ytmp_r = Rot("otmp", [scrB[:, 0:SEG].rearrange("p (a b) -> p a b", b=128), scrB[:, SEG:2 * SEG].rearrange("p (a b) -> p a b", b=128)])
        prod_r = Rot("prod", [A([128, 512], F32) for _ in range(2)])
        h2_r = Rot("h2", [A([128, D], BF16) for _ in range(2)])
        h2T_r = Rot("h2T", [A([128, 8, 128], BF16) for _ in range(1)])
        st2 = A([128, 8], F32)
        rt_r = Rot("rt", [A([128, 768], F32) for _ in range(1)])
        print("phase1 arena used", arena.off)

        p.dve(lambda e: e.memset(vaug2[0], 1.0), writes=[("vaug", 0, j) for j in range(TPS)])
        p.dve(lambda e: e.memset(vaug2[1], 1.0), writes=[("vaug", 1, j) for j in range(TPS)])
        p.dve(lambda e: e.memset(ubuf[:, :, 0:3], 0.0), writes=[("u", c, "halo") for c in range(4)])
        p.dve(lambda e: e.memset(pbuf[:, :, 0:16], 0.0), writes=[("p", c, "halo") for c in range(4)])

        x_v = x_d.rearrange("(n p) c -> n p c", p=128)
        X1_v = X1_d.rearrange("(n p) c -> n p c", p=128)
        H2_v = H2_d.rearrange("(n p) c -> n p c", p=128)
        QCS, QTOT, QINVRS, QG, QWS, QAC, QSQA, QTMP = range(8)

        def stF(sg):
            par = sg % 2
            vaug = vaug2[par]
            sigoT = sigoT2[par]
            for j in range(TPS):
                ti = sg * TPS + j
                xs, xsk = xs_r.next()
                xb, xbk = xb_r.next()
                p.dma("sp", (lambda xs, ti: lambda e: e.dma_start(out=xs, in_=x_v[ti]))(xs, ti), writes=[xsk])
                p.act((lambda xs, j: lambda e: e.activation(out=sqj, in_=xs, func=AF.Square, accum_out=ssq1[:, j:j + 1]))(xs, j),
                      reads=[xsk], writes=["sqj", ("ssq1", j)])
                p.act((lambda j: lambda e: e.activation(out=rstd1[:, j:j + 1], in_=ssq1[:, j:j + 1], func=AF.Sqrt, scale=1.0 / D, bias=EPS))(j),
                      reads=[("ssq1", j)], writes=[("std1", j)])
                p.dve((lambda j: lambda e: e.reciprocal(out=rstd1[:, j:j + 1], in_=rstd1[:, j:j + 1]))(j), reads=[("std1", j)], writes=[("rstd1", j)])
                p.dve((lambda xs, xb, j: lambda e: e.tensor_scalar(out=xb, in0=xs, scalar1=rstd1[:, j:j + 1], scalar2=None, op0=ALU.mult))(xs, xb, j),
                      reads=[xsk, ("rstd1", j)], writes=[xbk])
                bk, bkk = nbank()
                bkb = bk[:].bitcast(BF16).rearrange("p (a b) -> p a b", b=128)

                def tr(e, xb=xb, bkb=bkb):
                    for k in range(8):
                        ins = e.transpose(out=bkb[:, k, :], in_=xb[:, k * 128:(k + 1) * 128], identity=identb[:])
                    return ins
                p.pe(tr, reads=[xbk, "identb"], writes=[bkk])
                p.act((lambda bkb, j: lambda e: e.copy(out=xT[:, :, j * 128:(j + 1) * 128], in_=bkb))(bkb, j),
                      reads=[bkk], writes=[("xT", j)])
                yield
            XTALL = [("xT", j) for j in range(TPS)]

            for grp, col0 in (("U", COL_U), ("O", COL_O), ("P", COL_P)):
                for c in range(4):
                    bk, bkk = nbank()
                    c0 = col0 + c * 128

                    def mm(e, bk=bk, c0=c0):
                        for k in range(8):
                            ins = e.matmul(bk[:], lhsT=wi[:, k, c0:c0 + 128], rhs=xT[:, k, :], start=(k == 0), stop=(k == 7))
                        return ins
                    p.pe(mm, reads=WIALL + XTALL, writes=[bkk])
                    if grp == "U":
                        p.act((lambda bk, c: lambda e: e.activation(out=ubuf[:, c, 3:SEG + 3], in_=bk[:], func=AF.Identity,
                                                                    bias=biascol[:, c:c + 1]))(bk, c),
                              reads=[bkk, "biascol"], writes=[("u", c, "body")])
                    elif grp == "O":
                        p.act((lambda bk, c: lambda e: e.activation(out=sigoT[:, c, :], in_=bk[:], func=AF.Sigmoid,
                                                                    bias=biascol[:, 4 + c:5 + c]))(bk, c),
                              reads=[bkk, "biascol"], writes=[("sigo", par, c)])
                    else:
                        p.dve((lambda bk, c: lambda e: e.tensor_copy(out=pbuf[:, c, 16:SEG + 16], in_=bk[:]))(bk, c),
                              reads=[bkk], writes=[("p", c, "body")])
                    yield
            for j in range(TPS):
                bk, bkk = nbank()

                def mmv(e, bk=bk, j=j):
                    for k in range(8):
                        ins = e.matmul(bk[:], lhsT=xT[:, k, j * 128:(j + 1) * 128], rhs=wi[:, k, COL_V:COL_V + 512],
                                       start=(k == 0), stop=(k == 7))
                    return ins
                p.pe(mmv, reads=WIALL + XTALL, writes=[bkk])
                p.dve((lambda bk, j: lambda e: e.tensor_tensor(
                    out=vaug[:, j, :, 0:128], in0=bk[:].rearrange("p (a b) -> p a b", b=128),
                    in1=biasbc[:, 0:512].rearrange("p (a b) -> p a b", b=128), op=ALU.add))(bk, j),
                    reads=[bkk, "biasbc_v"], writes=[("vaug", par, j)])
                bk, bkk = nbank()

                def mmg(e, bk=bk, j=j):
                    for k in range(8):
                        ins = e.matmul(bk[:, 0:8], lhsT=xT[:, k, j * 128:(j + 1) * 128], rhs=wi[:, k, COL_I:COL_I + 8],
                                       start=(k == 0), stop=(k == 7))
                    return ins
                p.pe(mmg, reads=WIALL + XTALL, writes=[bkk])
                p.dve((lambda bk, j: lambda e: e.tensor_tensor(out=gat[:, j, :], in0=bk[:, 0:8], in1=biasbc[:, 512:520], op=ALU.add))(bk, j),
                      reads=[bkk, "biasbc_g"], writes=[("gat", j)])
                yield

        def stM(sg):
            GATALL = [("gat", j) for j in range(TPS)]

            for c in range(4):
                eng = "dve"
                ct, ctk = ctmp_r.next()
                UR = [("u", c, "halo"), ("u", c, "body")]
                p.ve(eng, (lambda ct, c: lambda e: e.tensor_scalar(out=ct, in0=ubuf[:, c, 0:SEG], scalar1=convw[:, c * 4:c * 4 + 1],
                                                                    scalar2=None, op0=ALU.mult))(ct, c),
                     reads=UR + ["colp"], writes=[ctk])
                for k in range(1, 4):
                    p.ve(eng, (lambda ct, c, k: lambda e: e.scalar_tensor_tensor(
                        out=ct, in0=ubuf[:, c, k:k + SEG], scalar=convw[:, c * 4 + k:c * 4 + k + 1], in1=ct,
                        op0=ALU.mult, op1=ALU.add))(ct, c, k), reads=UR + ["colp", ctk], writes=[ctk])
                p.act((lambda ct, c: lambda e: e.activation(out=ucT[:, c, :], in_=ct, func=AF.Silu, bias=convb[:, c:c + 1]))(ct, c),
                      reads=[ctk, "colp"], writes=[("ucT", c)])
                p.ve(eng, (lambda c: lambda e: e.tensor_copy(out=ubuf[:, c, 0:3], in_=ubuf[:, c, SEG:SEG + 3]))(c),
                     reads=[("u", c, "body")], writes=[("u", c, "halo")])
            for g in range(4):
                eng = "pool" if g % 2 == 0 else "dve"
                PR = [("p", g, "halo"), ("p", g, "body")]
                W = SEG + 16
                src = pbuf[:, g, :]
                srck = PR
                sh = 1
                for lvl in range(g + 1):
                    dstt = ptmp[lvl % 2]
                    dk = ("ptmp", lvl % 2)
                    lo = 2 * sh
                    p.ve(eng, (lambda dstt, src, sh, lo: lambda e: e.tensor_tensor(
                        out=dstt[:, lo:W], in0=src[:, lo:W], in1=src[:, lo - sh:W - sh], op=ALU.add))(dstt, src, sh, lo),
                        reads=list(srck), writes=[dk])
                    src, srck, sh = dstt, [dk], sh * 2
                wg_ = float(2 ** (g + 1))
                p.ve("dve", (lambda src, g, wg_: lambda e: e.scalar_tensor_tensor(
                    out=pooled[:, g, :], in0=src[:, 16:W], scalar=1.0 / wg_, in1=pbuf[:, g, 16:W],
                    op0=ALU.mult, op1=ALU.subtract))(src, g, wg_), reads=list(srck) + PR, writes=[("pooled", g)])
                if sg == 0:
                    p.ve(eng, (lambda src, g: lambda e: e.tensor_tensor(out=p16, in0=src[:, 16:32], in1=inv0_bc[:, g * 16:(g + 1) * 16],
                                                                        op=ALU.mult))(src, g),
                         reads=list(srck) + ["rowb"], writes=["p16"])
                    p.ve(eng, (lambda g: lambda e: e.tensor_tensor(out=pooled[:, g, 0:16], in0=p16, in1=pbuf[:, g, 16:32],
                                                                   op=ALU.subtract))(g),
                         reads=["p16"] + PR, writes=[("pooled", g)])
                p.ve(eng, (lambda g: lambda e: e.tensor_copy(out=pbuf[:, g, 0:16], in_=pbuf[:, g, SEG:SEG + 16]))(g),
                     reads=[("p", g, "body")], writes=[("p", g, "halo")])
                bk, bkk = nbank()
                p.pe((lambda bk, g: lambda e: e.matmul(bk[:], lhsT=wpl[:, g, :], rhs=pooled[:, g, :], start=True, stop=True))(bk, g),
                     reads=["wpl", ("pooled", g)], writes=[bkk])
                p.act((lambda bk, g: lambda e: e.activation(out=yT[:, 4 + g, :], in_=bk[:], func=AF.Identity,
                                                            scale=pscalec[:, g:g + 1], bias=bpscol[:, g:g + 1]))(bk, g),
                      reads=[bkk, "colp", "bpscol"], writes=[("yT", 4 + g)])

            gi = gat[:, :, 0:4]
            gf = gat[:, :, 4:8]
            q = lambda n: gsx[:, n, :].rearrange("p (a b) -> p a b", b=4)
            p.act(lambda e: e.activation(out=gtmp, in_=gf, func=AF.Exp, scale=-1.0), reads=GATALL, writes=["gtmp"])
            p.act(lambda e: e.activation(out=gtmp, in_=gtmp, func=AF.Ln, bias=1.0), reads=["gtmp"], writes=["gtmp"])
            bk, bkk = nbank()

            def mmcs(e, bk=bk):
                for j in range(TPS):
                    e.matmul(bk[:, j * 4:(j + 1) * 4], lhsT=tri[:], rhs=gtmp[:, j, :], start=True, stop=True)
                for j in range(TPS):
                    ins = e.matmul(bk[:, 64 + j * 4:64 + (j + 1) * 4], lhsT=onesf[:], rhs=gtmp[:, j, :], start=True, stop=True)
                return ins
            p.pe(mmcs, reads=["gtmp", "tri", "onesf"], writes=[bkk])
            p.dve((lambda bk: lambda e: e.tensor_copy(out=gsx[:, QCS, :], in_=bk[:, 0:TPS * 4]))(bk), reads=[bkk], writes=["q_cs"])
            p.dve((lambda bk: lambda e: e.tensor_copy(out=gsx[:, QTOT, :], in_=bk[:, 64:64 + TPS * 4]))(bk), reads=[bkk], writes=["q_tot"])
            p.dve(lambda e: e.scalar_tensor_tensor(out=gsx[:, QTMP, :], in0=gsx[:, QTOT, :], scalar=-0.5, in1=gsx[:, QCS, :],
                                                   op0=ALU.mult, op1=ALU.add), reads=["q_cs", "q_tot"], writes=["q_tmp"])
            p.act(lambda e: e.activation(out=gsx[:, QINVRS, :], in_=gsx[:, QTMP, :], func=AF.Exp), reads=["q_tmp"], writes=["q_invrs"])
            p.dve(lambda e: e.tensor_tensor(out=q(QG), in0=q(QTMP), in1=gi, op=ALU.add), reads=["q_tmp"] + GATALL, writes=["q_g"])
            p.act(lambda e: e.activation(out=gsx[:, QG, :], in_=gsx[:, QG, :], func=AF.Exp), reads=["q_g"], writes=["q_g"])
            p.dve(lambda e: e.tensor_tensor(out=gsx[:, QWS, :], in0=gsx[:, QCS, :], in1=gsx[:, QTOT, :], op=ALU.subtract),
                  reads=["q_cs", "q_tot"], writes=["q_ws"])
            p.dve(lambda e: e.tensor_tensor(out=q(QWS), in0=q(QWS), in1=gi, op=ALU.add), reads=["q_ws"] + GATALL, writes=["q_ws"])
            p.act(lambda e: e.activation(out=gsx[:, QWS, :], in_=gsx[:, QWS, :], func=AF.Exp), reads=["q_ws"], writes=["q_ws"])
            p.act(lambda e: e.activation(out=gsx[:, QAC, :], in_=gsx[:, QTOT, :], func=AF.Exp, scale=-1.0), reads=["q_tot"], writes=["q_ac"])
            p.act(lambda e: e.activation(out=gsx[:, QSQA, :], in_=gsx[:, QTOT, :], func=AF.Exp, scale=-0.5), reads=["q_tot"], writes=["q_sqa"])

            for h in range(4):
                for wsel, dstT, sc, nm in ((0, qT, 128.0 ** -0.5, "qT"), (1, kT, 1.0, "kT")):
                    bk, bkk = nbank()
                    p.pe((lambda bk, h, wsel: lambda e: e.matmul(bk[:], lhsT=wqk[:, wsel, h, :], rhs=ucT[:, h, :], start=True, stop=True))(bk, h, wsel),
                         reads=["wq", "wk", ("ucT", h)], writes=[bkk])
                    p.act((lambda bk, h, dstT, sc: lambda e: e.activation(out=dstT[:, h, :], in_=bk[:], func=AF.Copy, scale=sc))(bk, h, dstT, sc),
                          reads=[bkk], writes=[(nm, h)])
            for j in range(TPS):
                bk, bkk = nbank()

                def mmk(e, bk=bk, j=j):
                    for h in range(4):
                        ins = e.matmul(bk[:, h * 128:(h + 1) * 128], lhsT=ucT[:, h, j * 128:(j + 1) * 128], rhs=wqk[:, 1, h, :],
                                       start=True, stop=True)
                    return ins
                p.pe(mmk, reads=["wk"] + [("ucT", h) for h in range(4)], writes=[bkk])
                p.dve((lambda bk, j: lambda e: e.tensor_copy(out=ktok[:, j, :, :], in_=bk[:].rearrange("p (a b) -> p a b", b=128)))(bk, j),
                      reads=[bkk], writes=[("ktok", j)])

        def stK(sg, pump, after_tile):
            par = sg % 2
            vaug = vaug2[par]
            sigoT = sigoT2[par]
            ctxs = {}
            hns = {}

            def S1(n):
                j, h = divmod(n, 4)
                jh = n
                if h == 0:
                    hns[j] = hn_r.next()
                c = {}
                eng2 = "pool" if h % 2 == 0 else "dve"
                wv, wvk = wv_r.next()
                p.ve(eng2, (lambda wv, j, h, jh: lambda e: e.tensor_scalar(out=wv, in0=vaug[:, j, h, :], scalar1=gsx[:, QWS, jh:jh + 1],
                                                                          scalar2=None, op0=ALU.mult))(wv, j, h, jh),
                     reads=[("vaug", par, j), "q_ws"], writes=[wvk])
                bkA, bkAk = nbank()
                p.pe((lambda bkA, wv, j, h: lambda e: e.matmul(bkA[:, 0:129], lhsT=ktok[:, j, h, :], rhs=wv, start=True, stop=True))(bkA, wv, j, h),
                     reads=[("ktok", j), wvk], writes=[bkAk])
                p.pe((lambda bkA, j, h: lambda e: e.matmul(bkA[:, 256:384], lhsT=kT[:, h, j * 128:(j + 1) * 128],
                                                           rhs=qT[:, h, j * 128:(j + 1) * 128], start=True, stop=True))(bkA, j, h),
                     reads=[("kT", h), ("qT", h)], writes=[bkAk])
                ds, dsk = dst_r.next()
                p.dve((lambda ds, bkA, jh: lambda e: e.scalar_tensor_tensor(out=ds, in0=bkA[:, 256:384], scalar=gsx[:, QG, jh:jh + 1],
                                                                           in1=trib[:], op0=ALU.mult, op1=ALU.mult))(ds, bkA, jh),
                      reads=[bkAk, "q_g", "trib"], writes=[dsk])
                cb, cbk = cb_r.next()
                p.ve(eng2, (lambda cb, h, jh: lambda e: e.tensor_scalar(out=cb, in0=Cst[:, h, :], scalar1=gsx[:, QSQA, jh:jh + 1],
                                                                       scalar2=None, op0=ALU.mult))(cb, h, jh),
                     reads=[("C", h), "q_sqa"], writes=[cbk])
                p.dve((lambda bkA, h, jh: lambda e: e.scalar_tensor_tensor(out=Cst[:, h, :], in0=Cst[:, h, :], scalar=gsx[:, QAC, jh:jh + 1],
                                                                          in1=bkA[:, 0:129], op0=ALU.mult, op1=ALU.add))(bkA, h, jh),
                      reads=[("C", h), "q_ac", bkAk], writes=[("C", h)])
                c.update(ds=ds, dsk=dsk, cb=cb, cbk=cbk)
                ctxs[n] = c

            def S2(n):
                j, h = divmod(n, 4)
                jh = n
                c = ctxs[n]
                ds, dsk, cb, cbk = c["ds"], c["dsk"], c["cb"], c["cbk"]
                bkO, bkOk = obank()

                def mmo(e, bkO=bkO, ds=ds, cb=cb, j=j, h=h):
                    e.matmul(bkO[:, 0:129], lhsT=ds, rhs=vaug[:, j, h, :], start=True, stop=False)
                    return e.matmul(bkO[:, 0:129], lhsT=qT[:, h, j * 128:(j + 1) * 128], rhs=cb, start=False, stop=True)
                p.pe(mmo, reads=[dsk, ("vaug", par, j), ("qT", h), cbk], writes=[bkOk])
                sm, smk = sm_r.next()
                p.act((lambda sm, bkO: lambda e: e.activation(out=sm[:, 0:1], in_=bkO[:, 128:129], func=AF.Abs))(sm, bkO),
                      reads=[bkOk], writes=[(smk, 0)])
                p.dve((lambda sm, jh: lambda e: e.tensor_tensor(out=sm[:, 1:2], in0=sm[:, 0:1], in1=gsx[:, QINVRS, jh:jh + 1], op=ALU.max))(sm, jh),
                      reads=[(smk, 0), "q_invrs"], writes=[(smk, 1)])
                p.dve((lambda sm: lambda e: e.reciprocal(out=sm[:, 2:3], in_=sm[:, 1:2]))(sm), reads=[(smk, 1)], writes=[(smk, 2)])
                p.act((lambda sm, bkO: lambda e: e.activation(out=sqj[:, 0:128], in_=bkO[:, 0:128], func=AF.Square, scale=sm[:, 2:3],
                                                              accum_out=sm[:, 3:4]))(sm, bkO),
                      reads=[bkOk, (smk, 2)], writes=["sqj", (smk, 3)])
                p.act((lambda sm: lambda e: e.activation(out=sm[:, 4:5], in_=sm[:, 3:4], func=AF.Sqrt, scale=1.0 / 128, bias=EPS))(sm),
                      reads=[(smk, 3)], writes=[(smk, 4)])
                c.update(bkO=bkO, bkOk=bkOk, sm=sm, smk=smk)

            def S3(n):
                j, h = divmod(n, 4)
                c = ctxs[n]
                bkO, bkOk, sm, smk = c["bkO"], c["bkOk"], c["sm"], c["smk"]
                hn, hnk = hns[j]
                p.dve((lambda sm: lambda e: e.reciprocal(out=sm[:, 5:6], in_=sm[:, 4:5]))(sm), reads=[(smk, 4)], writes=[(smk, 5)])
                p.dve((lambda sm: lambda e: e.tensor_tensor(out=sm[:, 6:7], in0=sm[:, 5:6], in1=sm[:, 2:3], op=ALU.mult))(sm),
                      reads=[(smk, 5), (smk, 2)], writes=[(smk, 6)])
                p.dve((lambda sm, bkO, hn, h: lambda e: e.scalar_tensor_tensor(
                    out=hn[:, h * 128:(h + 1) * 128], in0=bkO[:, 0:128], scalar=sm[:, 6:7], in1=normg_bc[:, h * 128:(h + 1) * 128],
                    op0=ALU.mult, op1=ALU.mult))(sm, bkO, hn, h), reads=[bkOk, (smk, 6), "rowb"], writes=[(hnk, h)])
                if h == 3:
                    bk, bkk = nbank()
                    bkb = bk[:].bitcast(BF16).rearrange("p (a b) -> p a b", b=128)

                    def trh(e, hn=hn, bkb=bkb):
                        for hh in range(4):
                            ins = e.transpose(out=bkb[:, hh, :], in_=hn[:, hh * 128:(hh + 1) * 128], identity=identb[:])
                        return ins
                    p.pe(trh, reads=[(hnk, hh) for hh in range(4)] + ["identb"], writes=[bkk])
                    yt, ytk = ytmp_r.next()
                    for hh in range(4):
                        p.dve((lambda yt, bkb, j, hh: lambda e: e.scalar_tensor_tensor(
                            out=yt[:, hh, :], in0=ucT[:, hh, j * 128:(j + 1) * 128], scalar=skipc[:, hh:hh + 1], in1=bkb[:, hh, :],
                            op0=ALU.mult, op1=ALU.add))(yt, bkb, j, hh),
                            reads=[bkk, ("ucT", hh), "colp"], writes=[ytk])
                    p.dve((lambda yt, j: lambda e: e.tensor_tensor(out=yT[:, 0:4, j * 128:(j + 1) * 128], in0=yt, in1=sigoT[:, :, j * 128:(j + 1) * 128],
                                                                    op=ALU.mult))(yt, j),
                           reads=[ytk] + [("sigo", par, cc_) for cc_ in range(4)], writes=[("yTm", j)])
                    after_tile(j)

            NIT = TPS * 4
            for step in range(NIT + 2):
                if step < NIT:
                    S1(step)
                if 0 <= step - 1 < NIT:
                    S2(step - 1)
                if 0 <= step - 2 < NIT:
                    S3(step - 2)
                pump(2 if step % 2 == 0 else 1)

        def stE(sg):
            YTALL = [("yT", 4 + g) for g in range(4)] + [("yTm", j) for j in range(TPS)]

            ectx = {}

            def EW(j):
                ti = sg * TPS + j
                xs, xsk = xs_r.next()
                p.dma("sp", (lambda xs, ti: lambda e: e.dma_start(out=xs, in_=x_v[ti]))(xs, ti), writes=[xsk])
                x1, x1k = xs, xsk
                X1K = [xsk]
                for half in range(2):
                    bk, bkk = nbank()

                    def mmw(e, bk=bk, j=j, half=half):
                        for k in range(8):
                            ins = e.matmul(bk[:], lhsT=yT[:, k, j * 128:(j + 1) * 128], rhs=wo[:, k, half * 512:(half + 1) * 512],
                                           start=(k == 0), stop=(k == 7))
                        return ins
                    p.pe(mmw, reads=[("yT", 4 + g) for g in range(4)] + [("yTm", j)] + WOALL, writes=[bkk])
                    p.dve((lambda bk, x1, half: lambda e: e.tensor_tensor(out=x1[:, half * 512:(half + 1) * 512], in0=bk[:],
                                                                         in1=x1[:, half * 512:(half + 1) * 512], op=ALU.add))(bk, x1, half),
                          reads=[bkk, xsk], writes=[xsk])
                ectx[j] = (x1, x1k, X1K, ti)

            def EP(j):
                x1, x1k, X1K, ti = ectx[j]
                p.dma("act", (lambda x1, ti: lambda e: e.dma_start(out=X1_v[ti], in_=x1))(x1, ti), reads=X1K, writes=[("X1", ti)])
                p.act((lambda x1, j: lambda e: e.activation(out=sqj, in_=x1, func=AF.Square, accum_out=st2[:, j:j + 1]))(x1, j),
                      reads=X1K, writes=["sqj", ("st2a", j)])
                p.act((lambda j: lambda e: e.activation(out=st2[:, 4 + j:5 + j], in_=st2[:, j:j + 1], func=AF.Sqrt, scale=1.0 / D, bias=EPS))(j),
                      reads=[("st2a", j)], writes=[("st2b", j)])
                p.dve((lambda j: lambda e: e.reciprocal(out=st2[:, 4 + j:5 + j], in_=st2[:, 4 + j:5 + j]))(j), reads=[("st2b", j)], writes=[("st2c", j)])
                h2f, h2fk = scrA, "h2f"
                h2, h2k = h2_r.next()
                p.dve((lambda h2f, x1, j: lambda e: e.scalar_tensor_tensor(out=h2f, in0=x1, scalar=st2[:, 4 + j:5 + j], in1=s2bc[:],
                                                                          op0=ALU.mult, op1=ALU.mult))(h2f, x1, j),
                      reads=X1K + [("st2c", j), "s2bc"], writes=[h2fk] + CT2)
                p.dve((lambda h2, h2f: lambda e: e.tensor_tensor(out=h2, in0=h2f, in1=shift_f_bc, op=ALU.add))(h2, h2f),
                       reads=[h2fk] + CT2 + MODALL, writes=[h2k])
                p.dma("act", (lambda h2, ti: lambda e: e.dma_start(out=H2_v[ti], in_=h2))(h2, ti), reads=[h2k], writes=[("H2", ti)])
                bk, bkk = nbank()
                bkb = bk[:].bitcast(BF16).rearrange("p (a b) -> p a b", b=128)

                def tr2(e, h2=h2, bkb=bkb):
                    for k in range(8):
                        ins = e.transpose(out=bkb[:, k, :], in_=h2[:, k * 128:(k + 1) * 128], identity=identb[:])
                    return ins
                p.pe(tr2, reads=[h2k, "identb"], writes=[bkk])
                h2T, h2Tk = h2T_r.next()
                p.act((lambda h2T, bkb: lambda e: e.copy(out=h2T, in_=bkb))(h2T, bkb), reads=[bkk], writes=[h2Tk])

                def mmr(e, j=j, h2T=h2T):
                    for k in range(8):
                        ins = e.matmul(banks[7][:, j * 36:(j + 1) * 36], lhsT=h2T[:, k, :], rhs=wr[:, k, :], start=(k == 0), stop=(k == 7))
                    return ins
                p.pe(mmr, reads=[h2Tk, "wr"], writes=[("rbank", j)])
                if j < TPS - 1:
                    return
                t0 = sg * TPS
                rt, rtk = rt_r.next()
                RB = [("rbank", jj) for jj in range(TPS)]
                v3 = lambda a, n_: rt[:, a:a + TPS * n_].rearrange("p (t n) -> p t n", n=n_)
                lg = v3(0, 36)
                elm = v3(144, 32)
                elm2 = v3(272, 32)
                oh1f = v3(400, 32)
                oh2f = v3(528, 32)
                ohg = v3(656, 4)
                pen = v3(672, 4)
                egj = v3(688, 4)
                s4 = lambda a: rt[:, 704 + a * 4:708 + a * 4]
                R = lambda *a: [(rtk, x) for x in a]
                bc3 = lambda ap2, n_: ap2.unsqueeze(2).to_broadcast([128, TPS, n_])
                p.dve(lambda e: e.tensor_tensor(out=lg, in0=banks[7][:, 0:TPS * 36].rearrange("p (t n) -> p t n", n=36),
                                                in1=br_bc.unsqueeze(1).to_broadcast([128, TPS, 36]), op=ALU.add),
                      reads=RB + ["rowb"], writes=R("lg"))
                p.dve(lambda e: e.reduce_max(out=s4(0), in_=lg[:, :, 0:4], axis=AX.X), reads=R("lg"), writes=R("gmax"))
                p.dve(lambda e: e.tensor_tensor(out=ohg, in0=lg[:, :, 0:4], in1=bc3(s4(0), 4), op=ALU.is_equal), reads=R("lg", "gmax"), writes=R("ohg"))
                p.dve(lambda e: e.tensor_tensor(out=egj, in0=lg[:, :, 0:4], in1=bc3(s4(0), 4), op=ALU.subtract), reads=R("lg", "gmax"), writes=R("egj"))
                p.act(lambda e: e.activation(out=egj, in_=egj, func=AF.Exp), reads=R("egj"), writes=R("egj"))
                p.dve(lambda e: e.reduce_sum(out=s4(1), in_=egj, axis=AX.X), reads=R("egj"), writes=R("sumg"))
                p.dve(lambda e: e.reciprocal(out=s4(2), in_=s4(1)), reads=R("sumg"), writes=R("ggate"))
                p.dve(lambda e: e.tensor_scalar(out=pen, in0=ohg, scalar1=-1.0, scalar2=BIG, op0=ALU.add, op1=ALU.mult), reads=R("ohg"), writes=R("pen"))
                p.dve(lambda e: e.tensor_tensor(out=elm.rearrange("p t (a b) -> p t a b", b=8),
                                                in0=lg[:, :, 4:36].rearrange("p t (a b) -> p t a b", b=8),
                                                in1=pen.unsqueeze(3).to_broadcast([128, TPS, 4, 8]), op=ALU.add),
                      reads=R("lg", "pen"), writes=R("elm"))
                p.dve(lambda e: e.reduce_max(out=s4(3), in_=elm, axis=AX.X), reads=R("elm"), writes=R("m1"))
                p.dve(lambda e: e.tensor_tensor(out=oh1f, in0=elm, in1=bc3(s4(3), 32), op=ALU.is_equal), reads=R("elm", "m1"), writes=R("oh1"))
                p.dve(lambda e: e.scalar_tensor_tensor(out=elm2, in0=oh1f, scalar=-BIG, in1=elm, op0=ALU.mult, op1=ALU.add),
                      reads=R("elm", "oh1"), writes=R("elm2"))
                p.dve(lambda e: e.reduce_max(out=s4(4), in_=elm2, axis=AX.X), reads=R("elm2"), writes=R("m2"))
                p.dve(lambda e: e.tensor_tensor(out=oh2f, in0=elm2, in1=bc3(s4(4), 32), op=ALU.is_equal), reads=R("elm2", "m2"), writes=R("oh2"))
                p.dve(lambda e: e.tensor_tensor(out=s4(5), in0=s4(3), in1=s4(4), op=ALU.subtract), reads=R("m1", "m2"), writes=R("d12"))
                p.act(lambda e: e.activation(out=s4(6), in_=s4(5), func=AF.Sigmoid), reads=R("d12"), writes=R("p1"))
                p.act(lambda e: e.activation(out=s4(7), in_=s4(5), func=AF.Sigmoid, scale=-1.0), reads=R("d12"), writes=R("p2"))
                p.dve(lambda e: e.tensor_tensor(out=gts[:, t0:t0 + TPS, 0], in0=s4(6), in1=s4(2), op=ALU.mult), reads=R("p1", "ggate"), writes=[("gts", t0, 0)])
                p.dve(lambda e: e.tensor_tensor(out=gts[:, t0:t0 + TPS, 1], in0=s4(7), in1=s4(2), op=ALU.mult), reads=R("p2", "ggate"), writes=[("gts", t0, 1)])
                p.dve(lambda e: e.tensor_copy(out=ohs[:, t0:t0 + TPS, 0, :], in_=oh1f), reads=R("oh1"), writes=[("ohs", t0, 0)])
                p.dve(lambda e: e.tensor_copy(out=ohs[:, t0:t0 + TPS, 1, :], in_=oh2f), reads=R("oh2"), writes=[("ohs", t0, 1)])
                p.dve(lambda e: e.tensor_tensor(out=ohsum[:, t0:t0 + TPS, :], in0=oh1f, in1=oh2f, op=ALU.add), reads=R("oh1", "oh2"), writes=[("ohsum", t0)])
            return EW, EP

        for _ in stF(0):
            pass
        for sg in range(NSEG):
            stM(sg)
            gen = stF(sg + 1) if sg + 1 < NSEG else iter(())

            def pump(n, gen=gen):
                for _ in range(n):
                    next(gen, None)
            EW, EP = stE(sg)

            def after_tile(j, EW=EW, EP=EP):
                EW(j)
                if j >= 1:
                    EP(j - 1)
            stK(sg, pump, after_tile)
            EP(TPS - 1)
            for _ in gen:
                pass
            pump_zero((NB + NSEG - 1) // NSEG)

        pump_zero(NB)
        p.barrier()
        arena.reset()
        cnt = A([128, 32], F32)
        cmpJ = A([128, 32, JMAX], F32)
        nbk = A([128, 32], F32)
        cs_a = A([128, 32], F32)
        cs_b = A([128, 32], F32)
        pstart = A([128, 32], F32)
        cmpB = A([128, NB, 32], F32)
        bef = A([128, NB], F32)
        widxf = A([128, NB, 4], F32)
        gidxf = A([128, NB], F32)
        pos_r = Rot("pos", [A([128, 96], F32) for _ in range(2)])
        hsb_r = Rot("hsb", [A([128, D], BF16) for _ in range(3)])
        OHSUM = [("ohsum", i) for i in range(0, NT, TPS)]
        bk, bkk = nbank()

        def mmcnt(e, bk=bk):
            for i in range(NT):
                ins = e.matmul(bk[:, 0:32], lhsT=onesb[:], rhs=ohsum[:, i, :], start=(i == 0), stop=(i == NT - 1))
            return ins
        p.pe(mmcnt, reads=OHSUM + ["onesb"], writes=[bkk])
        p.dve((lambda bk: lambda e: e.tensor_copy(out=cnt, in_=bk[:, 0:32]))(bk), reads=[bkk], writes=["cnt"])
        p.dve(lambda e: e.tensor_tensor(out=cmpJ, in0=cnt.unsqueeze(2).to_broadcast([128, 32, JMAX]),
                                        in1=thr_bc.unsqueeze(1).to_broadcast([128, 32, JMAX]), op=ALU.is_gt),
              reads=["cnt", "rowb"], writes=["cmpJ"])
        p.dve(lambda e: e.reduce_sum(out=nbk, in_=cmpJ, axis=AX.X), reads=["cmpJ"], writes=["nbk"])
        p.dve(lambda e: e.tensor_scalar(out=nbk, in0=nbk, scalar1=float(B), scalar2=None, op0=ALU.mult), reads=["nbk"], writes=["padded"])
        src, srck = nbk, "padded"
        bufs = [(cs_a, "cs_a"), (cs_b, "cs_b")]
        for li, sh in enumerate((1, 2, 4, 8, 16)):
            dstt, dk = bufs[li % 2]
            p.dve((lambda dstt, src, sh: lambda e: e.tensor_copy(out=dstt[:, 0:sh], in_=src[:, 0:sh]))(dstt, src, sh), reads=[srck], writes=[(dk, 0)])
            p.dve((lambda dstt, src, sh: lambda e: e.tensor_tensor(out=dstt[:, sh:32], in0=src[:, sh:32], in1=src[:, 0:32 - sh], op=ALU.add))(dstt, src, sh),
                  reads=[srck], writes=[(dk, 1)])
            p.dve((lambda dstt: lambda e: e.tensor_copy(out=dstt[:, 0:1], in_=dstt[:, 0:1]))(dstt), reads=[(dk, 0), (dk, 1)], writes=[dk])
            src, srck = dstt, dk
        pend, pendk = src, srck
        p.dve(lambda e: e.tensor_tensor(out=pstart, in0=pend, in1=nbk, op=ALU.subtract), reads=[pendk, "padded"], writes=["pstart"])
        p.dve(lambda e: e.tensor_tensor(out=cmpB, in0=pend.unsqueeze(1).to_broadcast([128, NB, 32]),
                                        in1=bpos_bc.unsqueeze(2).to_broadcast([128, NB, 32]), op=ALU.is_le),
              reads=[pendk, "rowb"], writes=["cmpB"])
        p.dve(lambda e: e.reduce_sum(out=bef, in_=cmpB, axis=AX.X), reads=["cmpB"], writes=["bef0"])
        p.dve(lambda e: e.tensor_scalar(out=gidxf, in0=bef, scalar1=128.0, scalar2=None, op0=ALU.mult), reads=["bef0"], writes=["gidxf0"])
        p.dve(lambda e: e.tensor_tensor(out=gidxf, in0=gidxf, in1=rowoff[:, 0:1].to_broadcast([128, NB]), op=ALU.add),
              reads=["gidxf0", "rowoff"], writes=["gidxf"])
        p.dve(lambda e: e.tensor_copy(out=widx[:], in_=gidxf), reads=["gidxf"], writes=["widx"])
        p.dve(lambda e: e.tensor_scalar(out=gidxf, in0=gidxf, scalar1=float(NE * 128 - 1), scalar2=None, op0=ALU.min),
              reads=["gidxf", "widx"], writes=["gidxc"])
        p.dve(lambda e: e.tensor_copy(out=widxc[:], in_=gidxf), reads=["gidxc"], writes=["widxc"])
        wg_r = Rot("wg", [A([128, 8, 512], BF16) for _ in range(2)])
        wu_r = Rot("wu", [A([128, 8, 512], BF16) for _ in range(2)])
        wd_r = Rot("wd", [A([128, 4, D], BF16) for _ in range(2)])
        NSKIP = 15

        def gather_kw(b):
            if b >= NB - NSKIP:
                return dict(in_offset=bass.IndirectOffsetOnAxis(ap=widx[:, b:b + 1], axis=0), bounds_check=NE * 128 - 1, oob_is_err=False)
            return dict(in_offset=bass.IndirectOffsetOnAxis(ap=widxc[:, b:b + 1], axis=0))
        order = []
        lo, hi = 0, NB - 1
        while lo <= hi:
            order.append(lo); lo += 1
            if lo <= hi:
                order.append(lo); lo += 1
            if lo <= hi and hi >= NB - NSKIP:
                order.append(hi); hi -= 1
        assert sorted(order) == list(range(NB))
        nxt = {order[i]: (order[i + 2] if i + 2 < NB else None) for i in range(NB)}
        wbufs = {}

        def issue_gathers(b):
            wg_, wgk = wg_r.next()
            wu_, wuk = wu_r.next()
            wd_, wdk = wd_r.next()
            p.dma("pool", (lambda wg_, b: lambda e: e.indirect_dma_start(
                out=wg_.rearrange("p a b -> p (a b)"), out_offset=None, in_=wgate_d,
                **gather_kw(b)))(wg_, b),
                reads=["widx", "widxc"], writes=[(wgk, k) for k in range(8)])
            p.dma("pool", (lambda wu_, b: lambda e: e.indirect_dma_start(
                out=wu_.rearrange("p a b -> p (a b)"), out_offset=None, in_=wup_d,
                **gather_kw(b)))(wu_, b),
                reads=["widx", "widxc"], writes=[(wuk, k) for k in range(8)])
            p.dma("pool", (lambda wd_, b: lambda e: e.indirect_dma_start(
                out=wd_.rearrange("p a b -> p (a b)"), out_offset=None, in_=wdown_d,
                **gather_kw(b)))(wd_, b),
                reads=["widx", "widxc"], writes=[(wdk, k) for k in range(4)])
            wbufs[b] = (wg_, wgk, wu_, wuk, wd_, wdk)
        issue_gathers(order[0])
        issue_gathers(order[1])
        for i in range(NT):
            bk, bkk = nbank()

            def mmrk(e, bk=bk, i=i):
                ins = e.matmul(bk[:, 0:32], lhsT=striub[:], rhs=ohsum[:, i, :], start=True, stop=(i == 0))
                for j2 in range(i):
                    ins = e.matmul(bk[:, 0:32], lhsT=onesb[:], rhs=ohsum[:, j2, :], start=False, stop=(j2 == i - 1))
                return ins
            p.pe(mmrk, reads=OHSUM + ["onesb", "striub"], writes=[bkk])
            ps, psk = pos_r.next()
            p.dve((lambda bk, ps: lambda e: e.tensor_tensor(out=ps[:, 0:32], in0=bk[:, 0:32], in1=pstart, op=ALU.add))(bk, ps),
                  reads=[bkk, "pstart"], writes=[(psk, 0)])
            p.dve((lambda ps, i: lambda e: e.tensor_tensor(out=ps[:, 32:96].rearrange("p (a b) -> p a b", b=32), in0=ohs[:, i, :, :],
                                                           in1=ps[:, 0:32].unsqueeze(1).to_broadcast([128, 2, 32]), op=ALU.mult))(ps, i),
                  reads=[(psk, 0), ("ohs", i // TPS * TPS, 0), ("ohs", i // TPS * TPS, 1)], writes=[(psk, 1)])
            p.dve((lambda ps, i: lambda e: e.reduce_sum(out=destf[:, i, :], in_=ps[:, 32:96].rearrange("p (a b) -> p a b", b=32), axis=AX.X))(ps, i),
                  reads=[(psk, 1)], writes=[("destf", i)])
        p.dve(lambda e: e.tensor_copy(out=desti[:], in_=destf[:]), reads=[("destf", i) for i in range(NT)], writes=["desti"])
        dump("destf", destf[:], "desti")
        HSZ = [("Hs", "z", n, a_) for n in range(nz) for a_ in range(BT)]
        for i in range(NT):
            hb, hbk = hsb_r.next()
            p.dma("sp", (lambda hb, i: lambda e: e.dma_start(out=hb, in_=H2_v[i]))(hb, i), reads=[("H2", i)], writes=[hbk])
            for k in range(2):
                p.dma("pool", (lambda hb, i, k: lambda e: e.indirect_dma_start(
                    out=Hs_d, out_offset=bass.IndirectOffsetOnAxis(ap=desti[:, i, k:k + 1], axis=0), in_=hb, in_offset=None))(hb, i, k),
                    reads=[hbk, "desti"] + HSZ, writes=[("Hs", "s", i, k)])
        HSALL = [("Hs", "s", i, k) for i in range(NT) for k in range(2)]

        hsl_r = Rot("hsl", [A([128, BT, D], BF16) for _ in range(2)])
        hT_r = Rot("hT", [A([128, 8, B], BF16) for _ in range(2)])
        aT_r = Rot("aT", [A([128, 4, B], BF16) for _ in range(2)])
        sg_r = Rot("sg", [A([128, B], F32) for _ in range(2)])
        ysb_r = Rot("ysb", [A([128, D], F32) for _ in range(3)])
        print("phase3 arena used", arena.off)
        Hs_b = Hs_d.rearrange("(n a p) c -> n p a c", p=128, a=BT)
        Y_v = Y_d.rearrange("(n p) c -> n p c", p=128)
        hsl_bufs = {}

        def load_hsl(b):
            hsl, hslk = hsl_r.next()
            p.dma("sp", (lambda hsl, b: lambda e: e.dma_start(out=hsl, in_=Hs_b[b]))(hsl, b), reads=HSALL + HSZ, writes=[hslk])
            hsl_bufs[b] = (hsl, hslk)
        load_hsl(order[0])
        load_hsl(order[1])
        for b in order:
            if b not in wbufs:
                issue_gathers(b)
            wg_, wgk, wu_, wuk, wd_, wdk = wbufs[b]
            WG = [(wgk, k) for k in range(8)]
            WU = [(wuk, k) for k in range(8)]
            WD = [(wdk, k) for k in range(4)]
            hsl, hslk = hsl_bufs[b]
            hT, hTk = hT_r.next()
            for a in range(BT):
                bk, bkk = nbank()
                bkb = bk[:].bitcast(BF16).rearrange("p (a b) -> p a b", b=128)

                def tr3(e, hsl=hsl, bkb=bkb, a=a):
                    for k in range(8):
                        ins = e.transpose(out=bkb[:, k, :], in_=hsl[:, a, :].rearrange("s (p k) -> s k p", k=8)[:, k, :], identity=identb[:])
                    return ins
                p.pe(tr3, reads=[hslk, "identb"], writes=[bkk])
                eng = "act" if a % 2 == 0 else "dve"
                if eng == "act":
                    p.act((lambda hT, bkb, a: lambda e: e.copy(out=hT[:, :, a * 128:(a + 1) * 128], in_=bkb))(hT, bkb, a),
                          reads=[bkk], writes=[(hTk, a)])
                else:
                    p.dve((lambda hT, bkb, a: lambda e: e.tensor_copy(out=hT[:, :, a * 128:(a + 1) * 128], in_=bkb))(hT, bkb, a),
                          reads=[bkk], writes=[(hTk, a)])
            HT = [(hTk, a) for a in range(BT)]
            if nxt[b] is not None:
                load_hsl(nxt[b])
            aT, aTk = aT_r.next()
            for hc in range(4):
                bkG, bkGk = nbank()
                bkU, bkUk = nbank()

                def mmg2(e, bkG=bkG, wg_=wg_, hT=hT, hc=hc):
                    for k in range(8):
                        ins = e.matmul(bkG[:, 0:B], lhsT=wg_[:, k, :].rearrange("d (p c) -> d c p", c=4)[:, hc, :], rhs=hT[:, k, :], start=(k == 0), stop=(k == 7))
                    return ins
                p.pe(mmg2, reads=WG + HT, writes=[bkGk])

                def mmu2(e, bkU=bkU, wu_=wu_, hT=hT, hc=hc):
                    for k in range(8):
                        ins = e.matmul(bkU[:, 0:B], lhsT=wu_[:, k, :].rearrange("d (p c) -> d c p", c=4)[:, hc, :], rhs=hT[:, k, :], start=(k == 0), stop=(k == 7))
                    return ins
                p.pe(mmu2, reads=WU + HT, writes=[bkUk])
                sgt, sgk = sg_r.next()
                p.act((lambda sgt, bkG: lambda e: e.activation(out=sgt, in_=bkG[:, 0:B], func=AF.Silu))(sgt, bkG), reads=[bkGk], writes=[sgk])
                p.dve((lambda aT, sgt, bkU, hc: lambda e: e.tensor_tensor(out=aT[:, hc, :], in0=bkU[:, 0:B], in1=sgt, op=ALU.mult))(aT, sgt, bkU, hc),
                      reads=[bkUk, sgk], writes=[(aTk, hc)])
            AT = [(aTk, hc) for hc in range(4)]
            for a in range(BT):
                ysb, ysbk = ysb_r.next()
                for half in range(2):
                    bk, bkk = nbank()

                    def mmd(e, bk=bk, aT=aT, wd_=wd_, a=a, half=half):
                        for hc in range(4):
                            ins = e.matmul(bk[:], lhsT=aT[:, hc, a * 128:(a + 1) * 128], rhs=wd_[:, hc, half * 512:(half + 1) * 512],
                                           start=(hc == 0), stop=(hc == 3))
                        return ins
                    p.pe(mmd, reads=AT + WD, writes=[bkk])
                    if half == 0:
                        p.act((lambda ysb, bk: lambda e: e.copy(out=ysb[:, 0:512], in_=bk[:]))(ysb, bk), reads=[bkk], writes=[(ysbk, 0)])
                    else:
                        p.dve((lambda ysb, bk: lambda e: e.tensor_copy(out=ysb[:, 512:1024], in_=bk[:]))(ysb, bk), reads=[bkk], writes=[(ysbk, 1)])
                p.dma("act", (lambda ysb, b, a: lambda e: e.dma_start(out=Y_v[b * BT + a], in_=ysb))(ysb, b, a),
                      reads=[(ysbk, 0), (ysbk, 1)], writes=[("Y", b, a)])
        YALL = [("Y", b, a) for b in range(NB) for a in range(BT)]

        p.barrier()
        arena.reset()
        fgbc = A([128, D], F32)
        y1_r = Rot("y1", [A([128, D], F32) for _ in range(4)])
        y2_r = Rot("y2", [A([128, D], F32) for _ in range(4)])
        xr_r = Rot("xr", [A([128, D], F32) for _ in range(4)])
        ob_r = Rot("ob", [A([128, D], F32) for _ in range(3)])
        sq4 = A([128, D], BF16)
        st4 = A([128, 2 * NT], F32)
        p.dma("sp", lambda e: e.dma_start(out=fgbc, in_=fg_d.partition_broadcast(128)), writes=["fgbc"])
        out_v = out_d.rearrange("(n p) c -> n p c", p=128)
        for i in range(NT):
            y1, y1k = y1_r.next()
            y2, y2k = y2_r.next()
            xr, xrk = xr_r.next()
            ob, obk = ob_r.next()
            p.dma("pool", (lambda y1, i: lambda e: e.indirect_dma_start(
                out=y1, out_offset=None, in_=Y_d, in_offset=bass.IndirectOffsetOnAxis(ap=desti[:, i, 0:1], axis=0)))(y1, i),
                reads=["desti"] + YALL, writes=[y1k])
            p.dma("pool", (lambda y2, i: lambda e: e.indirect_dma_start(
                out=y2, out_offset=None, in_=Y_d, in_offset=bass.IndirectOffsetOnAxis(ap=desti[:, i, 1:2], axis=0)))(y2, i),
                reads=["desti"] + YALL, writes=[y2k])
            p.dma("sp", (lambda xr, i: lambda e: e.dma_start(out=xr, in_=X1_v[i]))(xr, i), reads=[("X1", i)], writes=[xrk])
            p.dve((lambda y1, i: lambda e: e.tensor_scalar(out=y1, in0=y1, scalar1=gts[:, i, 0:1], scalar2=None, op0=ALU.mult))(y1, i),
                  reads=[y1k, ("gts", i // TPS * TPS, 0)], writes=[y1k])
            p.dve((lambda y1, y2, i: lambda e: e.scalar_tensor_tensor(out=y1, in0=y2, scalar=gts[:, i, 1:2], in1=y1, op0=ALU.mult, op1=ALU.add))(y1, y2, i),
                  reads=[y1k, y2k, ("gts", i // TPS * TPS, 1)], writes=[y1k])
            p.dve((lambda y1: lambda e: e.tensor_tensor(out=y1, in0=y1, in1=gate_f_bc, op=ALU.mult))(y1), reads=[y1k], writes=[y1k])
            p.dve((lambda y1, xr: lambda e: e.tensor_tensor(out=xr, in0=y1, in1=xr, op=ALU.add))(y1, xr), reads=[y1k, xrk], writes=[xrk])
            p.act((lambda xr, i: lambda e: e.activation(out=sq4, in_=xr, func=AF.Square, accum_out=st4[:, i:i + 1]))(xr, i),
                  reads=[xrk], writes=["sq4", ("st4a", i)])
            p.act((lambda i: lambda e: e.activation(out=st4[:, NT + i:NT + i + 1], in_=st4[:, i:i + 1], func=AF.Sqrt, scale=1.0 / D, bias=EPS))(i),
                  reads=[("st4a", i)], writes=[("st4b", i)])
            p.dve((lambda i: lambda e: e.reciprocal(out=st4[:, NT + i:NT + i + 1], in_=st4[:, NT + i:NT + i + 1]))(i),
                  reads=[("st4b", i)], writes=[("st4c", i)])
            p.dve((lambda ob, xr, i: lambda e: e.scalar_tensor_tensor(out=ob, in0=xr, scalar=st4[:, NT + i:NT + i + 1], in1=fgbc,
                                                                     op0=ALU.mult, op1=ALU.mult))(ob, xr, i),
                  reads=[xrk, ("st4c", i), "fgbc"], writes=[obk])
            p.dma("act", (lambda ob, i: lambda e: e.dma_start(out=out_v[i], in_=ob))(ob, i), reads=[obk], writes=[("out", i)])
        p.finish()
        p.emit(es)
        print("ops per engine", p.stats)
    return nc, dbg_out


def _host_consts():
    ident = np.eye(128, dtype=np.float32)
    tri = np.triu(np.ones((128, 128), np.float32))
    rowoff = (np.arange(8)[None, :] * 128 + np.arange(128)[:, None]).astype(np.float32)
    inv0 = np.zeros((4, 16), np.float32)
    for g in range(4):
        w = 2 ** (g + 1)
        inv0[g] = 1.0 / np.minimum(np.arange(16) + 1, w)
    iota = np.arange(32, dtype=np.float32)
    thr = (np.arange(JMAX) * B).astype(np.float32)
    bpos = (np.arange(NB) * B).astype(np.float32)
    return ident, tri, rowoff, inv0, iota, thr, bpos


_CACHE = {}


def _get_nc(debug=False):
    if debug not in _CACHE:
        _CACHE[debug] = build(debug)
    return _CACHE[debug]


def make_in_maps(inp):
    f = lambda a: np.ascontiguousarray(np.asarray(a, dtype=np.float32))
    ident, tri, rowoff, inv0, iota, thr, bpos = _host_consts()
    x = f(inp["x"]); c = f(inp["c"])
    colv = lambda v: v.reshape(-1, 128).T
    colp = np.concatenate([
        colv(f(inp["norm1_g"])[0]),
        f(inp["conv_w"])[0].T.reshape(4, 128, 4).transpose(1, 0, 2).reshape(128, 16),
        colv(f(inp["conv_b"])[0]), colv(f(inp["mlstm_skip"])[0]),
        colv(f(inp["b_pool"])[0]), colv(f(inp["pool_scale"])[0])], axis=1).astype(np.float32)
    rowp = np.concatenate([
        f(inp["mlstm_norm_g"])[0], f(inp["b_router_group"])[0], f(inp["b_router_expert"])[0],
        f(inp["b_igate"])[0], f(inp["b_fgate"])[0], inv0.reshape(-1), iota, thr, bpos])[None, :].astype(np.float32)
    w_r = np.concatenate([f(inp["w_router_group"])[0], f(inp["w_router_expert"])[0]], axis=1)
    shared = dict(
        ada_w=f(inp["ada_w"])[0], ada_b=f(inp["ada_b"]), w_in=f(inp["w_in"])[0], w_q=f(inp["w_q"])[0], w_k=f(inp["w_k"])[0],
        w_pool=f(inp["w_pool"])[0], w_out=f(inp["w_out"])[0], w_r=np.ascontiguousarray(w_r),
        w_gate=f(inp["w_expert_gate"])[0].reshape(NE * 128, 8 * 512), w_up=f(inp["w_expert_up"])[0].reshape(NE * 128, 8 * 512),
        w_down=f(inp["w_expert_down"])[0].reshape(NE * 128, 4 * D),
        colp=np.ascontiguousarray(colp), rowp=np.ascontiguousarray(rowp), norm2_g=f(inp["norm2_g"]),
        final_g=f(inp["final_g"]).reshape(1, D), ident=ident, tri=tri, rowoff=rowoff)
    maps = []
    for b in range(8):
        m = dict(shared)
        m["x"] = np.ascontiguousarray(x[b])
        m["ccol"] = np.ascontiguousarray(c[b].reshape(8, 128).T)
        maps.append(m)
    return maps


def kernel(**inputs):
    nc, _ = _get_nc(False)
    in_maps = make_in_maps(inputs)
    res = run_bass_kernel_spmd(nc, in_maps, core_ids=list(range(8)))
    out = np.stack([np.asarray(r["out"], dtype=np.float32) for r in res.results], axis=0)
    return out
```
